# Optimizing a Trainium2 kernel written in Bass

```python
import jax, jax.numpy as jnp
from jax import lax
import numpy as np

D_MODEL = 2048
BATCH = 4
SEQ = 8192
DEPTH = 2

GRID_W = 64
CTX_LEN = 256
N_MIXERS = 2
N_ADA = 6
RMS_EPS = 1e-6
N_HEADS = 16
Q_LORA = 512
KV_LORA = 512
QK_NOPE = 128
QK_ROPE = 64
V_DIM = 128
ROPE_AXIS = QK_ROPE // 2
ROPE_THETA = 10000.0
Q_BLOCK = 128
ATTN_SCALE = (QK_NOPE + QK_ROPE) ** -0.5
CONV_WIDTH = 3
N_EXPERTS = 32
TOP_K = 4
D_EXPERT = D_MODEL
SWIGLU_LIMIT = 7.0
SWIGLU_ALPHA = 1.702
EXPERT_BLOCK = 256

kernel_name = "hybrid_mla_shortconv_moe_dit"


def rms_norm(x, g):
    xf = x.astype(jnp.float32)
    y = xf * lax.rsqrt(jnp.mean(xf * xf, axis=-1, keepdims=True) + RMS_EPS)
    return (y * g.astype(jnp.float32)).astype(x.dtype)


def modulate(x, g, shift, scale):
    return rms_norm(x, g) * (1 + scale) + shift


def ada_chunks(cvec, w, b, n):
    d = w.shape[0]
    m = jax.nn.silu(cvec) @ w[:, : n * d] + b[: n * d]
    return jnp.split(m, n, axis=-1)


def grid_rope_tables(length):
    rows = length // GRID_W
    row = jnp.repeat(jnp.arange(rows), GRID_W).astype(jnp.float32)
    col = jnp.tile(jnp.arange(GRID_W), rows).astype(jnp.float32)
    inv = 1.0 / (ROPE_THETA ** (jnp.arange(0, ROPE_AXIS, 2, dtype=jnp.float32) / ROPE_AXIS))
    ang_r = row[:, None] * inv[None, :]
    ang_c = col[:, None] * inv[None, :]
    return jnp.cos(ang_r), jnp.sin(ang_r), jnp.cos(ang_c), jnp.sin(ang_c)


def _rotate(x, cos, sin):
    x1, x2 = jnp.split(x, 2, axis=-1)
    return jnp.concatenate([x1 * cos - x2 * sin, x2 * cos + x1 * sin], axis=-1)


def apply_rope2d(x, tables):
    cos_r, sin_r, cos_c, sin_c = tables

    def bc(t):
        return t.reshape((1, t.shape[0]) + (1,) * (x.ndim - 3) + (t.shape[1],)).astype(x.dtype)

    x_r, x_c = jnp.split(x, 2, axis=-1)
    return jnp.concatenate([_rotate(x_r, bc(cos_r), bc(sin_r)), _rotate(x_c, bc(cos_c), bc(sin_c))], axis=-1)


def mla_q(cq, g_q, w_q_up):
    b, l, _ = cq.shape
    q = (rms_norm(cq, g_q) @ w_q_up).reshape(b, l, N_HEADS, QK_NOPE + QK_ROPE)
    return q[..., :QK_NOPE], q[..., QK_NOPE:]


def mla_kv(ckv, g_kv, w_kv_up):
    b, l, _ = ckv.shape
    kv = (rms_norm(ckv, g_kv) @ w_kv_up).reshape(b, l, N_HEADS, QK_NOPE + V_DIM)
    return kv[..., :QK_NOPE], kv[..., QK_NOPE:]


def attend(q_nope, q_rope, k_nope, k_rope, v):
    b, l, h, _ = q_nope.shape
    nb = l // Q_BLOCK
    qn = q_nope.reshape(b, nb, Q_BLOCK, h, QK_NOPE).transpose(1, 0, 2, 3, 4)
    qr = q_rope.reshape(b, nb, Q_BLOCK, h, QK_ROPE).transpose(1, 0, 2, 3, 4)

    def one_block(args):
        qn_b, qr_b = args
        s = (jnp.einsum('bqhd,bkhd->bhqk', qn_b, k_nope).astype(jnp.float32)
             + jnp.einsum('bqhr,bkr->bhqk', qr_b, k_rope).astype(jnp.float32)) * ATTN_SCALE
        p = jax.nn.softmax(s, axis=-1).astype(v.dtype)
        return jnp.einsum('bhqk,bkhd->bqhd', p, v)

    o = lax.map(one_block, (qn, qr))
    return o.transpose(1, 0, 2, 3, 4).reshape(b, l, h * V_DIM)


def conv3_centred(z, w):
    zp = jnp.pad(z, ((0, 0), (1, 1), (0, 0)))
    return w[0] * zp[:, :-2] + w[1] * zp[:, 1:-1] + w[2] * zp[:, 2:]


def short_gated_conv(h, w_in, w_conv, w_out):
    gate_b, gate_c, u = jnp.split(h @ w_in, 3, axis=-1)
    return (gate_b * conv3_centred(gate_c * u, w_conv)) @ w_out


def expert_ffn(h, w_r, b_r, w_gu, b_gu, w_down, b_down):
    shp = h.shape
    t = h.reshape(-1, shp[-1])
    n_tok = t.shape[0]
    n_assign = n_tok * TOP_K
    logits = (t @ w_r + b_r).astype(jnp.float32)
    top_val, top_idx = lax.top_k(logits, TOP_K)
    gate = jax.nn.softmax(top_val, axis=-1).astype(h.dtype)
    flat_e = top_idx.reshape(-1)
    flat_tok = jnp.arange(n_assign, dtype=jnp.int32) // TOP_K
    flat_w = gate.reshape(-1)
    counts = jnp.bincount(flat_e, length=N_EXPERTS)
    padded = (counts + EXPERT_BLOCK - 1) // EXPERT_BLOCK * EXPERT_BLOCK
    padded_end = jnp.cumsum(padded)
    padded_start = padded_end - padded
    group_start = jnp.cumsum(counts) - counts
    order = jnp.argsort(flat_e)
    sorted_e = flat_e[order]
    dest = padded_start[sorted_e] + jnp.arange(n_assign, dtype=jnp.int32) - group_start[sorted_e]
    n_blocks = -(-(n_assign + N_EXPERTS * (EXPERT_BLOCK - 1)) // EXPERT_BLOCK)
    n_slots = n_blocks * EXPERT_BLOCK
    slot_tok = jnp.zeros((n_slots,), jnp.int32).at[dest].set(flat_tok[order])
    slot_w = jnp.zeros((n_slots,), h.dtype).at[dest].set(flat_w[order])
    block_start = jnp.arange(n_blocks, dtype=jnp.int32) * EXPERT_BLOCK
    block_e = jnp.minimum(jnp.sum(block_start[:, None] >= padded_end[None, :], axis=1), N_EXPERTS - 1)

    def run_block(args):
        tok, wgt, e = args
        xb = t[tok]
        gu = xb @ w_gu[e] + b_gu[e]
        glu, lin = jnp.split(gu, 2, axis=-1)
        glu = jnp.minimum(glu, SWIGLU_LIMIT)
        lin = jnp.clip(lin, -SWIGLU_LIMIT, SWIGLU_LIMIT)
        act = glu * jax.nn.sigmoid(SWIGLU_ALPHA * glu) * (lin + 1)
        return (act @ w_down[e] + b_down[e]) * wgt[:, None]

    y = lax.map(run_block, (slot_tok.reshape(n_blocks, EXPERT_BLOCK),
                            slot_w.reshape(n_blocks, EXPERT_BLOCK), block_e))
    out = jax.ops.segment_sum(y.reshape(n_slots, shp[-1]), slot_tok, num_segments=n_tok)
    return out.reshape(shp)


def setup_inputs(seed: int = 0) -> dict:
    key = jax.random.key(seed)
    ks = jax.random.split(key, 24)
    D, F = D_MODEL, D_EXPERT
    n_mla = (DEPTH + N_MIXERS - 1) // N_MIXERS
    n_conv = DEPTH // N_MIXERS

    def nrm(k, shape, scale):
        return jax.random.normal(k, shape, jnp.float32) * scale

    return {
        "x": nrm(ks[0], (BATCH, SEQ, D), 1.0),
        "c": nrm(ks[1], (BATCH, D), 1.0),
        "ctx": nrm(ks[2], (BATCH, CTX_LEN, D), 1.0),
        "c_ctx": nrm(ks[3], (D,), 1.0),
        "ada_w": nrm(ks[4], (DEPTH, D, N_ADA * D), 0.5 * D ** -0.5),
        "ada_b": nrm(ks[5], (DEPTH, N_ADA * D), 0.02),
        "norm_mix_g": 1.0 + nrm(ks[6], (DEPTH, D), 0.02),
        "norm_ffn_g": 1.0 + nrm(ks[7], (DEPTH, D), 0.02),
        "mla_w_in": nrm(ks[8], (n_mla, D, Q_LORA + KV_LORA + QK_ROPE), D ** -0.5),
        "mla_q_norm_g": 1.0 + nrm(ks[9], (n_mla, Q_LORA), 0.02),
        "mla_kv_norm_g": 1.0 + nrm(ks[10], (n_mla, KV_LORA), 0.02),
        "mla_w_q_up": nrm(ks[11], (n_mla, Q_LORA, N_HEADS * (QK_NOPE + QK_ROPE)), Q_LORA ** -0.5),
        "mla_w_kv_up": nrm(ks[12], (n_mla, KV_LORA, N_HEADS * (QK_NOPE + V_DIM)), KV_LORA ** -0.5),
        "mla_w_out": nrm(ks[13], (n_mla, N_HEADS * V_DIM, D), (N_HEADS * V_DIM) ** -0.5),
        "conv_w_in": nrm(ks[14], (n_conv, D, 3 * D), D ** -0.5),
        "conv_w": nrm(ks[15], (n_conv, CONV_WIDTH, D), CONV_WIDTH ** -0.5),
        "conv_w_out": nrm(ks[16], (n_conv, D, D), D ** -0.5),
        "router_w": nrm(ks[17], (DEPTH, D, N_EXPERTS), D ** -0.5),
        "router_b": nrm(ks[18], (DEPTH, N_EXPERTS), 0.01),
        "expert_w_gu": nrm(ks[19], (DEPTH, N_EXPERTS, D, 2 * F), D ** -0.5),
        "expert_b_gu": nrm(ks[20], (DEPTH, N_EXPERTS, 2 * F), 0.02),
        "expert_w_down": nrm(ks[21], (DEPTH, N_EXPERTS, F, D), F ** -0.5),
        "expert_b_down": nrm(ks[22], (DEPTH, N_EXPERTS, D), 0.02),
        "final_norm_g": 1.0 + nrm(ks[23], (D,), 0.02),
    }


def reference(x, c, ctx, c_ctx, ada_w, ada_b, norm_mix_g, norm_ffn_g, mla_w_in, mla_q_norm_g,
              mla_kv_norm_g, mla_w_q_up, mla_w_kv_up, mla_w_out, conv_w_in, conv_w, conv_w_out,
              router_w, router_b, expert_w_gu, expert_b_gu, expert_w_down, expert_b_down,
              final_norm_g):
    seq_len = x.shape[1]
    rope_tabs = grid_rope_tables(seq_len)
    c_lat = c[:, None, :]
    c_con = c_ctx[None, None, :]
    h_ctx = ctx

    for i in range(DEPTH):
        kind = i % N_MIXERS
        j = i // N_MIXERS
        update_ctx = any(k % N_MIXERS == 0 for k in range(i + 1, DEPTH))
        ctx_read = (kind == 0) or update_ctx

        sh1, sc1, g1, sh2, sc2, g2 = ada_chunks(c_lat, ada_w[i], ada_b[i], N_ADA)
        hx = modulate(x, norm_mix_g[i], sh1, sc1)
        if ctx_read:
            cmod = ada_chunks(c_con, ada_w[i], ada_b[i], N_ADA if update_ctx else 2)
            hc = modulate(h_ctx, norm_mix_g[i], cmod[0], cmod[1])

        if kind == 0:
            w_in = mla_w_in[j]
            if update_ctx:
                cq_c, ckv_c, kr_c = jnp.split(hc @ w_in, [Q_LORA, Q_LORA + KV_LORA], axis=-1)
            else:
                ckv_c, kr_c = jnp.split(hc @ w_in[:, Q_LORA:], [KV_LORA], axis=-1)
            k_nope_c, v_c = mla_kv(ckv_c, mla_kv_norm_g[j], mla_w_kv_up[j])
            cq, ckv, kr = jnp.split(hx @ w_in, [Q_LORA, Q_LORA + KV_LORA], axis=-1)
            q_nope, q_rope = mla_q(cq, mla_q_norm_g[j], mla_w_q_up[j])
            q_rope = apply_rope2d(q_rope, rope_tabs)
            kr = apply_rope2d(kr, rope_tabs)
            k_nope, v = mla_kv(ckv, mla_kv_norm_g[j], mla_w_kv_up[j])
            o = attend(q_nope, q_rope,
                       jnp.concatenate([k_nope_c, k_nope], axis=1),
                       jnp.concatenate([kr_c, kr], axis=1),
                       jnp.concatenate([v_c, v], axis=1))
            mix = o @ mla_w_out[j]
            if update_ctx:
                qn_c, qr_c = mla_q(cq_c, mla_q_norm_g[j], mla_w_q_up[j])
                mix_c = attend(qn_c, qr_c, k_nope_c, kr_c, v_c) @ mla_w_out[j]
        else:
            mix = short_gated_conv(hx, conv_w_in[j], conv_w[j], conv_w_out[j])
            if update_ctx:
                mix_c = short_gated_conv(hc, conv_w_in[j], conv_w[j], conv_w_out[j])

        x = x + g1 * mix
        x = x + g2 * expert_ffn(modulate(x, norm_ffn_g[i], sh2, sc2), router_w[i], router_b[i],
                                expert_w_gu[i], expert_b_gu[i], expert_w_down[i], expert_b_down[i])
        if update_ctx:
            h_ctx = h_ctx + cmod[2] * mix_c
            h_ctx = h_ctx + cmod[5] * expert_ffn(
                modulate(h_ctx, norm_ffn_g[i], cmod[3], cmod[4]), router_w[i], router_b[i],
                expert_w_gu[i], expert_b_gu[i], expert_w_down[i], expert_b_down[i])

    return rms_norm(x, final_norm_g)
```

```python
import numpy as np
import ml_dtypes
from contextlib import ExitStack
import concourse.bass as bass
import concourse.mybir as mybir
from concourse.bass_utils import run_bass_kernel_spmd

F32 = mybir.dt.float32
BF16 = mybir.dt.bfloat16
I32 = mybir.dt.int32
AF = mybir.ActivationFunctionType
ALU = mybir.AluOpType
AX = mybir.AxisListType

D = 2048
KC = D // 128
NH = 16
QL = 512
KVL = 512
ROPE = 64
CTX = 256
TOPK = 4
BLK = 512
EPS = 1e-6
ATT_SCALE = (128 + 64) ** -0.5
LIMIT = 7.0
ALPHA = 1.702
GRID_W = 64
THETA = 10000.0

FULL_CFG = dict(S=8192, NBC=2, E=32, F=2048, NCORES=2, BATCH=4)


class Buf:
    __slots__ = ("w", "r", "name", "ex")

    def __init__(self, name=""):
        self.w = None
        self.r = {}
        self.name = name
        self.ex = False


class Tile:
    def __init__(self, t, name):
        self.t = t
        self.b = Buf(name)

    def __getitem__(self, k):
        return self.t[k]


class K:
    def __init__(self, nc, es):
        self.nc = nc
        self.es = es
        self.eng = {"pe": nc.tensor, "act": nc.scalar, "dve": nc.vector, "pool": nc.gpsimd, "sp": nc.sync}
        self.sem = {}
        self.cnt = {}
        for n in ("pe", "act", "dve", "pool"):
            self.sem[n] = es.enter_context(nc.semaphore("c_" + n))
            self.cnt[n] = 0
        self.waited = {n: {} for n in self.eng}
        self.dsem = {}
        for q, k in (("sp", 20), ("pool", 20), ("act", 8)):
            self.dsem[q] = [[es.enter_context(nc.semaphore("d_%s%d" % (q, i))), 0] for i in range(k)]
        self.drr = {q: 0 for q in self.dsem}
        self.semkey = {}

    def _key(self, sem):
        return id(sem)

    def wait(self, en, tok):
        sem, val, prod = tok
        if en == "pe" and prod == "pe":
            return
        k = self._key(sem)
        if self.waited[en].get(k, 0) >= val:
            return
        self.eng[en].wait_ge(sem, val)
        self.waited[en][k] = val

    def _deps(self, en, R, W):
        for b in R:
            if b.w is not None:
                self.wait(en, b.w)
            if b.ex:
                for key, tok in b.r.items():
                    if key != en:
                        self.wait(en, tok)
        for b in W:
            if b.w is not None:
                self.wait(en, b.w)
            for tok in b.r.values():
                self.wait(en, tok)

    def _post(self, tok, R, W, rkey):
        for b in R:
            b.r[rkey] = tok
        for b in W:
            b.w = tok
            b.r = {}

    def op(self, en, fn, R=(), W=()):
        R = [x.b if isinstance(x, Tile) else x for x in R]
        W = [x.b if isinstance(x, Tile) else x for x in W]
        self._deps(en, R, W)
        ins = fn(self.eng[en])
        self.cnt[en] += 1
        ins.then_inc(self.sem[en], 1)
        tok = (self.sem[en], self.cnt[en], en)
        self._post(tok, R, W, en)
        return tok

    def dma(self, q, fn, R=(), W=()):
        R = [x.b if isinstance(x, Tile) else x for x in R]
        W = [x.b if isinstance(x, Tile) else x for x in W]
        self._deps(q, R, W)
        i = self.drr[q]
        self.drr[q] = (i + 1) % len(self.dsem[q])
        slot = self.dsem[q][i]
        if slot[1] > 0:
            self.wait(q, (slot[0], slot[1], "dma"))
        ins = fn(self.eng[q])
        slot[1] += 16
        ins.then_inc(slot[0], 16)
        tok = (slot[0], slot[1], "dma")
        self._post(tok, R, W, ("d", q, i))
        return tok

    def barrier(self):
        toks = [(self.sem[n], self.cnt[n], n) for n in self.sem if self.cnt[n] > 0]
        for q in self.dsem:
            for s, v in self.dsem[q]:
                if v > 0:
                    toks.append((s, v, "dma"))
        for en in self.eng:
            for tok in toks:
                if en == "pe" and tok[2] == "pe":
                    continue
                self.wait(en, tok)

    def sb(self, es, name, shape, dt):
        self.uid = getattr(self, "uid", 0) + 1
        name = "s%d_%s" % (self.uid, name)
        return Tile(es.enter_context(self.nc.sbuf_tensor(name, list(shape), dt)), name)

    def ps(self, es, name, shape, dt):
        self.uid = getattr(self, "uid", 0) + 1
        name = "p%d_%s" % (self.uid, name)
        t = Tile(es.enter_context(self.nc.psum_tensor(name, list(shape), dt)), name)
        t.b.ex = True
        return t


def build_nc(cfg):
    S, NBC, E, F = cfg["S"], cfg["NBC"], cfg["E"], cfg["F"]
    NFC = F // 128
    T = NBC * S
    NT = T // 128
    NK = S + CTX
    NKT = NK // 128
    A = S * TOPK
    NBLK = -(-(A + E * (BLK - 1)) // BLK)
    NSLOT = NBLK * BLK
    NTB = S // 128
    NCH = S // 512

    nc = bass.Bass("TRN2", target_bir_lowering=False)
    es = ExitStack()
    k = K(nc, es)

    def din(name, shape, dt=F32):
        return nc.dram_tensor(name, list(shape), dt, kind="ExternalInput").ap()

    def dscr(name, shape, dt):
        return nc.dram_tensor(name, list(shape), dt, kind="Internal").ap()

    x_in = din("x", [T, D])
    ctx_in = din("ctx", [NBC * CTX, D])
    cT_in = din("cT", [128, (NBC + 1) * KC])
    out_d = nc.dram_tensor("out", [T, D], F32, kind="ExternalOutput").ap()
    adaw_in = [din("adaw%d" % l, [D, 6 * D]) for l in range(2)]
    adab_in = [din("adab%d" % l, [128, 6 * D]) for l in range(2)]
    gmix_in = [din("gmix%d" % l, [128, D]) for l in range(2)]
    gffn_in = [din("gffn%d" % l, [128, D]) for l in range(2)]
    gfin_in = din("gfin", [128, D])
    win_in = din("mla_win", [D, 1152])
    gq_in = din("gq", [128, 4])
    gkv_in = din("gkv", [128, 4])
    wq_in = din("mla_wq", [QL, NH * 256])
    wkv_in = din("mla_wkv", [KVL, NH * 256])
    wout_in = din("mla_wout", [D, D])
    cwin_in = din("conv_win", [48 * 128, KC * 128])
    cw_in = din("conv_w", [128, KC * 3])
    cwout_in = din("conv_wout", [D, D])
    wr_in = [din("wr%d" % l, [D, E]) for l in range(2)]
    rb_in = [din("rb%d" % l, [128, E]) for l in range(2)]
    wgu_in = [din("wgu%d" % l, [NFC * E * 128, KC * 256]) for l in range(2)]
    wdn_in = [din("wdn%d" % l, [NFC * E * 128, D]) for l in range(2)]
    bgu_in = [din("bgu%d" % l, [E * 128, 2 * NFC]) for l in range(2)]
    bdn_in = [din("bdn%d" % l, [E, D]) for l in range(2)]
    ident_in = din("ident", [128, 128])
    utri_in = din("utri", [128, 128])
    ropec_in = din("ropec", [64, S])
    ropes_in = din("ropes", [64, S])
    blks_in = din("blkstart", [128, NBLK])
    pcol_in = din("pcol", [128, 1])

    adaw_b = [dscr("adaw_b%d" % l, [D, 6 * D], BF16) for l in range(2)]
    win_b = dscr("win_b", [D, 1152], BF16)
    wq_b = dscr("wq_b", [QL, NH * 256], BF16)
    wkv_b = dscr("wkv_b", [KVL, NH * 256], BF16)
    wout_b = dscr("wout_b", [D, D], BF16)
    cwin_b = dscr("cwin_b", [48 * 128, KC * 128], BF16)
    cwout_b = dscr("cwout_b", [D, D], BF16)
    wgu_b = [[dscr("wgu_b%d_%d" % (l, fc), [E * 128, KC * 256], BF16) for fc in range(NFC)] for l in range(2)]
    wdn_b = [[dscr("wdn_b%d_%d" % (l, fc), [E * 128, D], BF16) for fc in range(NFC)] for l in range(2)]
    modv = dscr("modv", [(NBC + 1) * 2 * 6 * 128, D], F32)
    xres = dscr("xres", [T, D], F32)
    hrows = dscr("hrows", [T, D], BF16)
    cqnT = dscr("cqnT", [4 * 128, S], BF16)
    kvT = dscr("kvT", [4 * 128, NK], BF16)
    krT = dscr("krT", [64, NK], BF16)
    oT = dscr("oT", [D, S], BF16)
    zT = dscr("zT", [D, S + 2], BF16)
    gbT = dscr("gbT", [D, S], BF16)
    xs = dscr("xs", [NSLOT, D], BF16)
    ys = [dscr("ys%d" % j, [NSLOT, D // 2], F32) for j in range(2)]

    def modv_ap(i, l, which):
        r0 = ((i * 2 + l) * 6 + which) * 128
        return modv[r0:r0 + 128, :]

    ident_f = k.sb(es, "ident_f", [128, 128], F32)
    ident_bf = k.sb(es, "ident_bf", [128, 128], BF16)
    ones_bf = k.sb(es, "ones_bf", [128, 128], BF16)
    ones_f = k.sb(es, "ones_f", [128, 128], F32)
    utri_bf = k.sb(es, "utri_bf", [128, 128], BF16)
    utri_f = k.sb(es, "utri_f", [128, 128], F32)
    pcol = k.sb(es, "pcol", [128, 1], F32)
    psA = [k.ps(es, "psA%d" % i, [128, 512], F32) for i in range(6)]
    psT = [k.ps(es, "psT%d" % i, [128, 1024], BF16) for i in range(2)]

    k.dma("sp", lambda e: e.dma_start(out=ident_f[:, :], in_=ident_in), W=[ident_f])
    k.dma("sp", lambda e: e.dma_start(out=utri_f[:, :], in_=utri_in), W=[utri_f])
    k.dma("sp", lambda e: e.dma_start(out=pcol[:, :], in_=pcol_in), W=[pcol])
    k.op("dve", lambda e: e.tensor_copy(out=ident_bf[:, :], in_=ident_f[:, :]), R=[ident_f], W=[ident_bf])
    k.op("dve", lambda e: e.tensor_copy(out=utri_bf[:, :], in_=utri_f[:, :]), R=[utri_f], W=[utri_bf])
    k.op("dve", lambda e: e.memset(ones_bf[:, :], 1.0), W=[ones_bf])
    k.op("dve", lambda e: e.memset(ones_f[:, :], 1.0), W=[ones_f])

    def cast_phase():
        pes = ExitStack()
        CHW = 4096
        NB_ = 3
        ibuf = [k.sb(pes, "cw_i%d" % i, [128, CHW], F32) for i in range(NB_)]
        obuf = [k.sb(pes, "cw_o%d" % i, [128, CHW], BF16) for i in range(NB_)]
        jobs = []
        pairs = [(adaw_in[0], adaw_b[0]), (adaw_in[1], adaw_b[1]), (win_in, win_b), (wq_in, wq_b),
                 (wkv_in, wkv_b), (wout_in, wout_b), (cwin_in, cwin_b), (cwout_in, cwout_b)]
        for l in range(2):
            for fc in range(NFC):
                pairs.append((wgu_in[l][fc * E * 128:(fc + 1) * E * 128, :], wgu_b[l][fc]))
                pairs.append((wdn_in[l][fc * E * 128:(fc + 1) * E * 128, :], wdn_b[l][fc]))
        for src, dst in pairs:
            R_, C_ = src.shape
            assert R_ % 128 == 0
            sv = src.rearrange("(p a) c -> p (a c)", p=128)
            dv = dst.rearrange("(p a) c -> p (a c)", p=128)
            tot = (R_ // 128) * C_
            for c0 in range(0, tot, CHW):
                w = min(CHW, tot - c0)
                jobs.append((sv[:, c0:c0 + w], dv[:, c0:c0 + w], w))
        engs = ["dve", "pool", "act"]
        PRE = 2
        for j in range(min(PRE, len(jobs))):
            s_, d_, w = jobs[j]
            k.dma("sp", lambda e, s_=s_, w=w, j=j: e.dma_start(out=ibuf[j % NB_][:, 0:w], in_=s_), W=[ibuf[j % NB_]])
        for j, (s_, d_, w) in enumerate(jobs):
            if j + PRE < len(jobs):
                s2, d2, w2 = jobs[j + PRE]
                bi = (j + PRE) % NB_
                k.dma("sp", lambda e, s2=s2, w2=w2, bi=bi: e.dma_start(out=ibuf[bi][:, 0:w2], in_=s2), W=[ibuf[bi]])
            b = j % NB_
            en = engs[j % 3]
            if en == "act":
                k.op("act", lambda e, b=b, w=w: e.copy(out=obuf[b][:, 0:w], in_=ibuf[b][:, 0:w]), R=[ibuf[b]], W=[obuf[b]])
            else:
                k.op(en, lambda e, b=b, w=w: e.tensor_copy(out=obuf[b][:, 0:w], in_=ibuf[b][:, 0:w]), R=[ibuf[b]], W=[obuf[b]])
            k.dma("act", lambda e, b=b, w=w, d_=d_: e.dma_start(out=d_, in_=obuf[b][:, 0:w]), R=[obuf[b]])
        k.barrier()
        pes.close()

    def ada_phase():
        pes = ExitStack()
        cT = k.sb(pes, "cT", [128, (NBC + 1) * KC], F32)
        sT = k.sb(pes, "sT", [128, (NBC + 1) * KC], F32)
        scb = k.sb(pes, "scb", [128, (NBC + 1) * KC, 128], BF16)
        k.dma("sp", lambda e: e.dma_start(out=cT[:, :], in_=cT_in), W=[cT])
        k.op("act", lambda e: e.activation(out=sT[:, :], in_=cT[:, :], func=AF.Silu), R=[cT], W=[sT])
        for j in range((NBC + 1) * KC):
            k.op("dve", lambda e, j=j: e.tensor_scalar(out=scb[:, j, :], in0=ones_f[:, :], scalar1=sT[:, j:j + 1],
                                                       scalar2=None, op0=ALU.mult), R=[sT, ones_f], W=[scb])
        wsl = [k.sb(pes, "ada_w%d" % i, [128, KC, 512], BF16) for i in range(2)]
        bsl = [k.sb(pes, "ada_b%d" % i, [128, 512], F32) for i in range(2)]
        gsl = [k.sb(pes, "ada_g%d" % i, [128, 512], F32) for i in range(2)]
        osl = [k.sb(pes, "ada_o%d" % i, [128, 512], F32) for i in range(3)]
        it = 0
        oi = 0
        for l in range(2):
            for j in range(24):
                which, sub = j // 4, j % 4
                b = it % 2
                it += 1
                k.dma("sp", lambda e, l=l, j=j, b=b: e.dma_start(
                    out=wsl[b][:, :, :], in_=adaw_b[l][:, j * 512:(j + 1) * 512].rearrange("(kc p) n -> p kc n", p=128)),
                    W=[wsl[b]])
                k.dma("sp", lambda e, l=l, j=j, b=b: e.dma_start(out=bsl[b][:, :], in_=adab_in[l][:, j * 512:(j + 1) * 512]),
                      W=[bsl[b]])
                if which in (1, 4):
                    gsrc = gmix_in[l] if which == 1 else gffn_in[l]
                    k.dma("sp", lambda e, gsrc=gsrc, sub=sub, b=b: e.dma_start(out=gsl[b][:, :], in_=gsrc[:, sub * 512:(sub + 1) * 512]),
                          W=[gsl[b]])
                rows = list(range(NBC))
                if l == 0 and which < 2:
                    rows.append(NBC)
                for i in rows:
                    pz = psA[i % 4]
                    for kc in range(KC):
                        k.op("pe", lambda e, pz=pz, i=i, kc=kc, b=b: e.matmul(
                            pz[:, :], scb[:, i * KC + kc, :], wsl[b][:, kc, :], start=(kc == 0), stop=(kc == KC - 1)),
                            R=[scb, wsl[b]], W=[pz])
                    o = osl[oi % 3]
                    oi += 1
                    k.op("dve", lambda e, o=o, pz=pz, b=b: e.tensor_tensor(out=o[:, :], in0=pz[:, :], in1=bsl[b][:, :], op=ALU.add),
                         R=[pz, bsl[b]], W=[o])
                    if which in (1, 4):
                        k.op("dve", lambda e, o=o, b=b: e.scalar_tensor_tensor(out=o[:, :], in0=o[:, :], scalar=1.0, in1=gsl[b][:, :],
                                                                             op0=ALU.add, op1=ALU.mult), R=[o, gsl[b]], W=[o])
                    k.dma("act", lambda e, o=o, i=i, l=l, which=which, sub=sub: e.dma_start(
                        out=modv_ap(i, l, which)[:, sub * 512:(sub + 1) * 512], in_=o[:, :]), R=[o])
        k.barrier()
        pes.close()

    def rstd_from_ssq(ssq, rstd, n, width=1):
        k.op("dve", lambda e: e.tensor_scalar(out=rstd[:, 0:width], in0=ssq[:, 0:width], scalar1=1.0 / n, scalar2=EPS,
                                              op0=ALU.mult, op1=ALU.add), R=[ssq], W=[rstd])
        k.op("act", lambda e: e.activation(out=rstd[:, 0:width], in_=rstd[:, 0:width], func=AF.Sqrt), R=[rstd], W=[rstd])
        k.op("dve", lambda e: e.reciprocal(out=rstd[:, 0:width], in_=rstd[:, 0:width]), R=[rstd], W=[rstd])

    class NormState:
        pass

    def norm_tiles(pes, tag):
        ns = NormState()
        ns.junk = k.sb(pes, tag + "_junk", [128, D], BF16)
        ns.ssq = [k.sb(pes, tag + "_ssq%d" % i, [128, 1], F32) for i in range(2)]
        ns.rstd = [k.sb(pes, tag + "_rstd%d" % i, [128, 1], F32) for i in range(2)]
        ns.tmp = k.sb(pes, tag + "_tmp", [128, D], F32)
        ns.i = 0
        return ns

    def norm_mod(ns, xt, A_t, B_t, out_t, out_f32=None):
        i = ns.i % 2
        ns.i += 1
        ssq, rstd = ns.ssq[i], ns.rstd[i]
        k.op("act", lambda e: e.activation(out=ns.junk[:, :], in_=xt[:, :], func=AF.Square, accum_out=ssq[:, 0:1]),
             R=[xt], W=[ns.junk, ssq])
        rstd_from_ssq(ssq, rstd, D)
        k.op("dve", lambda e: e.scalar_tensor_tensor(out=ns.tmp[:, :], in0=xt[:, :], scalar=rstd[:, 0:1], in1=A_t[:, :],
                                                     op0=ALU.mult, op1=ALU.mult), R=[xt, rstd, A_t], W=[ns.tmp])
        if B_t is None:
            return
        if out_f32 is not None:
            k.op("pool", lambda e: e.tensor_tensor(out=out_f32[:, :], in0=ns.tmp[:, :], in1=B_t[:, :], op=ALU.add),
                 R=[ns.tmp, B_t], W=[out_f32])
            k.op("act", lambda e: e.copy(out=out_t[:, :], in_=out_f32[:, :]), R=[out_f32], W=[out_t])
        else:
            k.op("pool", lambda e: e.tensor_tensor(out=out_t[:, :], in0=ns.tmp[:, :], in1=B_t[:, :], op=ALU.add),
                 R=[ns.tmp, B_t], W=[out_t])

    def transpose_bf(h_t, hT, col0, ntile_cols=128):
        for half in range(2):
            pt = psT[half]
            for j in range(8):
                kc = half * 8 + j
                k.op("pe", lambda e, pt=pt, j=j, kc=kc: e.transpose(out=pt[:, j * 128:(j + 1) * 128],
                                                                    in_=h_t[:, kc * 128:(kc + 1) * 128], identity=ident_bf[:, :]),
                     R=[h_t, ident_bf], W=[pt])
            en = "act" if half == 0 else "dve"
            if en == "act":
                k.op("act", lambda e, pt=pt, half=half: e.copy(out=hT[:, half * 8:(half + 1) * 8, col0:col0 + 128],
                                                              in_=pt[:, :].rearrange("p (j t) -> p j t", j=8)), R=[pt], W=[hT])
            else:
                k.op("dve", lambda e, pt=pt, half=half: e.tensor_copy(out=hT[:, half * 8:(half + 1) * 8, col0:col0 + 128],
                                                                     in_=pt[:, :].rearrange("p (j t) -> p j t", j=8)), R=[pt], W=[hT])

    def load_mod(pes, i, l, which, name):
        t = k.sb(pes, name, [128, D], F32)
        k.dma("sp", lambda e: e.dma_start(out=t[:, :], in_=modv_ap(i, l, which)), W=[t])
        return t

    def mla_latent_phase(i):
        pes = ExitStack()
        A1 = load_mod(pes, i, 0, 1, "A1")
        B1 = load_mod(pes, i, 0, 0, "B1")
        Ac = load_mod(pes, NBC, 0, 1, "Ac")
        Bc = load_mod(pes, NBC, 0, 0, "Bc")
        win_sb = k.sb(pes, "win_sb", [128, KC, 1152], BF16)
        k.dma("sp", lambda e: e.dma_start(out=win_sb[:, :, :], in_=win_b.rearrange("(kc p) n -> p kc n", p=128)), W=[win_sb])
        gq = k.sb(pes, "gq", [128, 4], F32)
        gkv = k.sb(pes, "gkv", [128, 4], F32)
        k.dma("sp", lambda e: e.dma_start(out=gq[:, :], in_=gq_in), W=[gq])
        k.dma("sp", lambda e: e.dma_start(out=gkv[:, :], in_=gkv_in), W=[gkv])
        ns = norm_tiles(pes, "n1")
        xt = [k.sb(pes, "xt%d" % j, [128, D], F32) for j in range(2)]
        hb = [k.sb(pes, "hb%d" % j, [128, D], BF16) for j in range(2)]
        hT = k.sb(pes, "hT", [128, KC, 512], BF16)
        lat_f = k.sb(pes, "lat_f", [128, 4, 512], F32)
        lat_sq = k.sb(pes, "lat_sq", [128, 4, 512], BF16)
        lat_n = k.sb(pes, "lat_n", [128, 4, 512], BF16)
        rbc = k.sb(pes, "rbc", [128, 512], F32)
        cs = k.sb(pes, "cs", [64, 2, 512], F32)
        t1 = k.sb(pes, "t1", [64, 512], F32)
        t2 = k.sb(pes, "t2", [64, 512], F32)
        kr_o = k.sb(pes, "kr_o", [64, 512], BF16)
        xi = 0
        chunks = [("ctx", 0, CTX)] + [("lat", c * 512, 512) for c in range(NCH)]
        for kind, t0, w in chunks:
            ntile = w // 128
            for tt in range(ntile):
                xb = xt[xi % 2]
                hbb = hb[xi % 2]
                xi += 1
                if kind == "ctx":
                    src = ctx_in[i * CTX + tt * 128:i * CTX + (tt + 1) * 128, :]
                else:
                    src = x_in[i * S + t0 + tt * 128:i * S + t0 + (tt + 1) * 128, :]
                k.dma("sp", lambda e, xb=xb, src=src: e.dma_start(out=xb[:, :], in_=src), W=[xb])
                norm_mod(ns, xb, Ac if kind == "ctx" else A1, Bc if kind == "ctx" else B1, hbb)
                transpose_bf(hbb, hT, tt * 128)
            groups = ([("cq", 0)] if kind == "lat" else []) + [("ckv", 4)]
            for gname, m0 in groups:
                for j in range(4):
                    for kc in range(KC):
                        k.op("pe", lambda e, j=j, kc=kc, m0=m0: e.matmul(
                            psA[j][:, 0:w], win_sb[:, kc, (m0 + j) * 128:(m0 + j + 1) * 128], hT[:, kc, 0:w],
                            start=(kc == 0), stop=(kc == KC - 1)), R=[win_sb, hT], W=[psA[j]])
                    k.op("act", lambda e, j=j: e.copy(out=lat_f[:, j, 0:w], in_=psA[j][:, 0:w]), R=[psA[j]], W=[lat_f])
                    k.op("act", lambda e, j=j: e.activation(out=lat_sq[:, j, 0:w], in_=psA[j][:, 0:w], func=AF.Square),
                         R=[psA[j]], W=[lat_sq])
                for j in range(4):
                    k.op("pe", lambda e, j=j: e.matmul(psA[4][:, 0:w], ones_bf[:, :], lat_sq[:, j, 0:w], start=(j == 0), stop=(j == 3)),
                         R=[ones_bf, lat_sq], W=[psA[4]])
                k.op("dve", lambda e: e.tensor_scalar(out=rbc[:, 0:w], in0=psA[4][:, 0:w], scalar1=1.0 / 512, scalar2=EPS,
                                                      op0=ALU.mult, op1=ALU.add), R=[psA[4]], W=[rbc])
                k.op("act", lambda e: e.activation(out=rbc[:, 0:w], in_=rbc[:, 0:w], func=AF.Sqrt), R=[rbc], W=[rbc])
                k.op("dve", lambda e: e.reciprocal(out=rbc[:, 0:w], in_=rbc[:, 0:w]), R=[rbc], W=[rbc])
                gg = gq if gname == "cq" else gkv
                for j in range(4):
                    k.op("dve", lambda e, j=j, gg=gg: e.scalar_tensor_tensor(
                        out=lat_n[:, j, 0:w], in0=lat_f[:, j, 0:w], scalar=gg[:, j:j + 1], in1=rbc[:, 0:w],
                        op0=ALU.mult, op1=ALU.mult), R=[lat_f, gg, rbc], W=[lat_n])
                if gname == "cq":
                    dst = cqnT.rearrange("(j p) s -> p j s", p=128)[:, :, t0:t0 + w]
                else:
                    k0 = 0 if kind == "ctx" else CTX + t0
                    dst = kvT.rearrange("(j p) s -> p j s", p=128)[:, :, k0:k0 + w]
                k.dma("act", lambda e, dst=dst: e.dma_start(out=dst, in_=lat_n[:, :, 0:w]), R=[lat_n])
            for which, m0 in ((0, 1024), (1, 1088)):
                pz = psA[4 + which]
                for kc in range(KC):
                    k.op("pe", lambda e, pz=pz, kc=kc, m0=m0: e.matmul(pz[0:64, 0:w], win_sb[:, kc, m0:m0 + 64], hT[:, kc, 0:w],
                                                                        start=(kc == 0), stop=(kc == KC - 1)), R=[win_sb, hT], W=[pz])
            if kind == "ctx":
                k.op("act", lambda e: e.copy(out=kr_o[:, 0:w], in_=psA[4][0:64, 0:w]), R=[psA[4]], W=[kr_o])
                k.op("act", lambda e: e.copy(out=t2[:, 0:w], in_=psA[5][0:64, 0:w]), R=[psA[5]], W=[t2])
                k0 = 0
            else:
                k.dma("sp", lambda e: e.dma_start(out=cs[:, 0, :], in_=ropec_in[:, t0:t0 + 512]), W=[cs])
                k.dma("sp", lambda e: e.dma_start(out=cs[:, 1, :], in_=ropes_in[:, t0:t0 + 512]), W=[cs])
                k.op("dve", lambda e: e.tensor_tensor(out=t1[:, :], in0=psA[4][0:64, :], in1=cs[:, 0, :], op=ALU.mult),
                     R=[psA[4], cs], W=[t1])
                k.op("dve", lambda e: e.tensor_tensor(out=t2[:, :], in0=psA[5][0:64, :], in1=cs[:, 1, :], op=ALU.mult),
                     R=[psA[5], cs], W=[t2])
                k.op("pool", lambda e: e.tensor_tensor(out=kr_o[:, :], in0=t1[:, :], in1=t2[:, :], op=ALU.add),
                     R=[t1, t2], W=[kr_o])
                k0 = CTX + t0
            k.dma("act", lambda e, k0=k0: e.dma_start(out=krT[:, k0:k0 + w], in_=kr_o[:, 0:w]), R=[kr_o])
        k.barrier()
        pes.close()

    def attention_phase(i):
        pes = ExitStack()
        kv_sb = k.sb(pes, "kv_sb", [128, 4, NK], BF16)
        kr_sb = k.sb(pes, "kr_sb", [128, NK], BF16)
        k.dma("sp", lambda e: e.dma_start(out=kv_sb[:, :, :], in_=kvT.rearrange("(j p) s -> p j s", p=128)), W=[kv_sb])
        k.op("dve", lambda e: e.memset(kr_sb[64:128, :], 0.0), W=[kr_sb])
        k.dma("sp", lambda e: e.dma_start(out=kr_sb[0:64, :], in_=krT), W=[kr_sb])
        KnT = k.sb(pes, "KnT", [128, NK], BF16)
        V = k.sb(pes, "V", [128, NKT, 128], BF16)
        wqh = k.sb(pes, "wqh", [128, 4, 256], BF16)
        wkvh = k.sb(pes, "wkvh", [128, 4, 256], BF16)
        sqt = [k.sb(pes, "sqt%d" % j, [128, 512], BF16) for j in range(2)]
        kmx = k.sb(pes, "kmx", [128, 40], F32)
        krmax = k.sb(pes, "krmax", [128, 1], F32)
        kmax2 = k.sb(pes, "kmax2", [128, 1], F32)
        cqc = [k.sb(pes, "cqc%d" % j, [128, 4, 512], BF16) for j in range(2)]
        csq = [k.sb(pes, "csq%d" % j, [64, 2, 512], F32) for j in range(2)]
        QnT = [k.sb(pes, "QnT%d" % j, [128, 512], BF16) for j in range(2)]
        QrT = [k.sb(pes, "QrT%d" % j, [128, 512], BF16) for j in range(2)]
        sqn = k.sb(pes, "sqn", [128, 512], BF16)
        sqr = k.sb(pes, "sqr", [128, 512], BF16)
        for t_ in QrT + [sqr]:
            k.op("dve", lambda e, t_=t_: e.memset(t_[:, :], 0.0), W=[t_])
        t1 = k.sb(pes, "at1", [64, 512], F32)
        t2 = k.sb(pes, "at2", [64, 512], F32)
        qm = [k.sb(pes, "qm%d" % j, [128, 1], F32) for j in range(2)]
        nb = [k.sb(pes, "nb%d" % j, [128, 1], F32) for j in range(2)]
        PT = [k.sb(pes, "PT%d" % j, [128, 512], BF16) for j in range(4)]
        rec = k.sb(pes, "rec", [128, 512], F32)
        ot = [k.sb(pes, "ot%d" % j, [128, 512], BF16) for j in range(2)]
        NKC = -(-NK // 512)
        for c in range(NKC if cfg.get("ASTOP", 99) != -1 else 0):
            c0 = c * 512
            w = min(512, NK - c0)
            s_ = sqt[c % 2]
            k.op("act", lambda e, s_=s_, c0=c0, w=w: e.activation(out=s_[:, 0:w], in_=kr_sb[:, c0:c0 + w], func=AF.Square),
                 R=[kr_sb], W=[s_])
            k.op("pe", lambda e, s_=s_, w=w: e.matmul(psA[0][:, 0:w], ones_bf[:, :], s_[:, 0:w], start=True, stop=True),
                 R=[ones_bf, s_], W=[psA[0]])
            k.op("dve", lambda e, c=c, w=w: e.reduce_max(out=kmx[:, c:c + 1], in_=psA[0][:, 0:w], axis=AX.X), R=[psA[0]], W=[kmx])
        k.op("dve", lambda e: e.reduce_max(out=krmax[:, 0:1], in_=kmx[:, 0:NKC], axis=AX.X), R=[kmx], W=[krmax])
        it = 0
        AST = cfg.get("ASTOP", 99)

        class _Stop(Exception):
            pass

        def chk(n):
            if AST <= n:
                raise _Stop()
        try:
          chk(1)
          for h in range(NH):
              k.dma("sp", lambda e, h=h: e.dma_start(
                  out=wqh[:, :, :], in_=wq_b[:, h * 256:(h + 1) * 256].rearrange("(j p) n -> p j n", p=128)), W=[wqh])
              k.dma("sp", lambda e, h=h: e.dma_start(
                  out=wkvh[:, :, :], in_=wkv_b[:, h * 256:(h + 1) * 256].rearrange("(j p) n -> p j n", p=128)), W=[wkvh])
              for c in range(NKC):
                  c0 = c * 512
                  w = min(512, NK - c0)
                  pz = psA[c % 2]
                  for j in range(4):
                      k.op("pe", lambda e, pz=pz, j=j, c0=c0, w=w: e.matmul(pz[:, 0:w], wkvh[:, j, 0:128], kv_sb[:, j, c0:c0 + w],
                                                                            start=(j == 0), stop=(j == 3)), R=[wkvh, kv_sb], W=[pz])
                  k.op("dve", lambda e, pz=pz, c0=c0, w=w: e.tensor_copy(out=KnT[:, c0:c0 + w], in_=pz[:, 0:w]), R=[pz], W=[KnT])
                  s_ = sqt[c % 2]
                  k.op("act", lambda e, pz=pz, s_=s_, w=w: e.activation(out=s_[:, 0:w], in_=pz[:, 0:w], func=AF.Square), R=[pz], W=[s_])
                  k.op("pe", lambda e, s_=s_, w=w: e.matmul(psA[2][:, 0:w], ones_bf[:, :], s_[:, 0:w], start=True, stop=True),
                       R=[ones_bf, s_], W=[psA[2]])
                  k.op("dve", lambda e, c=c, w=w: e.reduce_max(out=kmx[:, c:c + 1], in_=psA[2][:, 0:w], axis=AX.X), R=[psA[2]], W=[kmx])
              k.op("dve", lambda e: e.reduce_max(out=kmax2[:, 0:1], in_=kmx[:, 0:NKC], axis=AX.X), R=[kmx], W=[kmax2])
              k.op("dve", lambda e: e.tensor_tensor(out=kmax2[:, 0:1], in0=kmax2[:, 0:1], in1=krmax[:, 0:1], op=ALU.add),
                   R=[kmax2, krmax], W=[kmax2])
              chk(2)
              for g in range(-(-NKT // 4)):
                  pz = psA[3 + g % 2]
                  nk_ = min(4, NKT - g * 4)
                  for q in range(nk_):
                      kt = g * 4 + q
                      for j in range(4):
                          k.op("pe", lambda e, pz=pz, q=q, kt=kt, j=j: e.matmul(
                              pz[:, q * 128:(q + 1) * 128], kv_sb[:, j, kt * 128:(kt + 1) * 128], wkvh[:, j, 128:256],
                              start=(j == 0), stop=(j == 3)), R=[kv_sb, wkvh], W=[pz])
                  k.op("act", lambda e, pz=pz, g=g, nk_=nk_: e.copy(
                      out=V[:, g * 4:g * 4 + nk_, :], in_=pz[:, 0:nk_ * 128].rearrange("p (q d) -> p q d", q=nk_)), R=[pz], W=[V])
              chk(3)
              for c in range(NCH):
                  t0 = c * 512
                  b2 = it % 2
                  it += 1
                  cq_ = cqc[b2]
                  cs_ = csq[b2]
                  k.dma("sp", lambda e, cq_=cq_, t0=t0: e.dma_start(
                      out=cq_[:, :, :], in_=cqnT.rearrange("(j p) s -> p j s", p=128)[:, :, t0:t0 + 512]), W=[cq_])
                  k.dma("sp", lambda e, cs_=cs_, t0=t0: e.dma_start(out=cs_[:, 0, :], in_=ropec_in[:, t0:t0 + 512]), W=[cs_])
                  k.dma("sp", lambda e, cs_=cs_, t0=t0: e.dma_start(out=cs_[:, 1, :], in_=ropes_in[:, t0:t0 + 512]), W=[cs_])
                  qn, qr = QnT[b2], QrT[b2]
                  for j in range(4):
                      k.op("pe", lambda e, j=j, cq_=cq_: e.matmul(psA[2][:, :], wqh[:, j, 0:128], cq_[:, j, :], start=(j == 0), stop=(j == 3)),
                           R=[wqh, cq_], W=[psA[2]])
                  k.op("dve", lambda e, qn=qn: e.tensor_copy(out=qn[:, :], in_=psA[2][:, :]), R=[psA[2]], W=[qn])
                  k.op("act", lambda e: e.activation(out=sqn[:, :], in_=psA[2][:, :], func=AF.Square), R=[psA[2]], W=[sqn])
                  for which in range(2):
                      pz = psA[3 + which]
                      for j in range(4):
                          k.op("pe", lambda e, pz=pz, j=j, cq_=cq_, which=which: e.matmul(
                              pz[0:64, :], wqh[:, j, 128 + which * 64:192 + which * 64], cq_[:, j, :], start=(j == 0), stop=(j == 3)),
                              R=[wqh, cq_], W=[pz])
                  k.op("dve", lambda e, cs_=cs_: e.tensor_tensor(out=t1[:, :], in0=psA[3][0:64, :], in1=cs_[:, 0, :], op=ALU.mult),
                       R=[psA[3], cs_], W=[t1])
                  k.op("dve", lambda e, cs_=cs_: e.tensor_tensor(out=t2[:, :], in0=psA[4][0:64, :], in1=cs_[:, 1, :], op=ALU.mult),
                       R=[psA[4], cs_], W=[t2])
                  k.op("pool", lambda e, qr=qr: e.tensor_tensor(out=qr[0:64, :], in0=t1[:, :], in1=t2[:, :], op=ALU.add), R=[t1, t2], W=[qr])
                  k.op("act", lambda e, qr=qr: e.activation(out=sqr[0:64, :], in_=qr[0:64, :], func=AF.Square), R=[qr], W=[sqr])
                  k.op("pe", lambda e: e.matmul(psA[2][:, :], ones_bf[:, :], sqn[:, :], start=True, stop=False), R=[ones_bf, sqn], W=[psA[2]])
                  k.op("pe", lambda e: e.matmul(psA[2][:, :], ones_bf[:, :], sqr[:, :], start=False, stop=True), R=[ones_bf, sqr], W=[psA[2]])
                  qm_, nb_ = qm[b2], nb[b2]
                  k.op("dve", lambda e, qm_=qm_: e.reduce_max(out=qm_[:, 0:1], in_=psA[2][:, :], axis=AX.X), R=[psA[2]], W=[qm_])
                  k.op("dve", lambda e, qm_=qm_: e.tensor_tensor(out=qm_[:, 0:1], in0=qm_[:, 0:1], in1=kmax2[:, 0:1], op=ALU.mult),
                       R=[qm_, kmax2], W=[qm_])
                  k.op("act", lambda e, qm_=qm_: e.activation(out=qm_[:, 0:1], in_=qm_[:, 0:1], func=AF.Sqrt), R=[qm_], W=[qm_])
                  k.op("dve", lambda e, qm_=qm_, nb_=nb_: e.tensor_scalar(out=nb_[:, 0:1], in0=qm_[:, 0:1], scalar1=-ATT_SCALE, scalar2=None,
                                                                         op0=ALU.mult), R=[qm_], W=[nb_])
                  chk(4)
                  Ops, Dps = psA[4], psA[5]
                  for kt in range(NKT):
                      sp_ = psA[kt % 2]
                      pt_ = PT[kt % 4]
                      k.op("pe", lambda e, sp_=sp_, kt=kt, qn=qn: e.matmul(sp_[:, :], KnT[:, kt * 128:(kt + 1) * 128], qn[:, :], start=True, stop=False),
                           R=[KnT, qn], W=[sp_])
                      k.op("pe", lambda e, sp_=sp_, kt=kt, qr=qr: e.matmul(sp_[:, :], kr_sb[:, kt * 128:(kt + 1) * 128], qr[:, :], start=False, stop=True),
                           R=[kr_sb, qr], W=[sp_])
                      k.op("act", lambda e, sp_=sp_, pt_=pt_, nb_=nb_: e.activation(out=pt_[:, :], in_=sp_[:, :], func=AF.Exp,
                                                                                  bias=nb_[:, 0:1], scale=ATT_SCALE), R=[sp_, nb_], W=[pt_])
                      k.op("pe", lambda e, kt=kt, pt_=pt_: e.matmul(Ops[:, :], V[:, kt, :], pt_[:, :], start=(kt == 0), stop=(kt == NKT - 1)),
                           R=[V, pt_], W=[Ops])
                      k.op("pe", lambda e, kt=kt, pt_=pt_: e.matmul(Dps[:, :], ones_bf[:, :], pt_[:, :], start=(kt == 0), stop=(kt == NKT - 1)),
                           R=[ones_bf, pt_], W=[Dps])
                  k.op("dve", lambda e: e.reciprocal(out=rec[:, :], in_=Dps[:, :]), R=[Dps], W=[rec])
                  o_ = ot[b2]
                  k.op("dve", lambda e, o_=o_: e.tensor_tensor(out=o_[:, :], in0=Ops[:, :], in1=rec[:, :], op=ALU.mult), R=[Ops, rec], W=[o_])
                  k.dma("act", lambda e, o_=o_, h=h, t0=t0: e.dma_start(out=oT[h * 128:(h + 1) * 128, t0:t0 + 512], in_=o_[:, :]), R=[o_])
                  chk(5)
        except _Stop:
            pass
        k.barrier()
        pes.close()

    class RouteState:
        pass

    def mix_phase(l, i, w_dram, rs):
        pes = ExitStack()
        w_sb = k.sb(pes, "wo_sb", [128, KC, D], BF16)
        k.dma("sp", lambda e: e.dma_start(out=w_sb[:, :, :], in_=w_dram.rearrange("(kc p) n -> p kc n", p=128)), W=[w_sb])
        G1 = load_mod(pes, i, l, 2, "G1")
        A2 = load_mod(pes, i, l, 4, "A2")
        B2 = load_mod(pes, i, l, 3, "B2")
        wr_sb = k.sb(pes, "wr_sb", [128, KC, E], F32)
        rb_sb = k.sb(pes, "rb_sb", [128, E], F32)
        k.dma("sp", lambda e: e.dma_start(out=wr_sb[:, :, :], in_=wr_in[l].rearrange("(kc p) n -> p kc n", p=128)), W=[wr_sb])
        k.dma("sp", lambda e: e.dma_start(out=rb_sb[:, :], in_=rb_in[l]), W=[rb_sb])
        srcT = k.sb(pes, "srcT", [128, KC, 512], BF16)
        xt = [k.sb(pes, "mxt%d" % j, [128, D], F32) for j in range(2)]
        x1 = [k.sb(pes, "mx1%d" % j, [128, D], F32) for j in range(2)]
        h2f = k.sb(pes, "h2f", [128, D], F32)
        h2b = [k.sb(pes, "h2b%d" % j, [128, D], BF16) for j in range(2)]
        h2T = k.sb(pes, "h2T", [128, KC, 128], F32)
        ns = norm_tiles(pes, "n2")
        m8 = k.sb(pes, "m8", [128, 8], F32)
        msk = k.sb(pes, "msk", [128, E], F32)
        xi = 0
        for c in range(NCH):
            t0 = c * 512
            k.dma("sp", lambda e, t0=t0: e.dma_start(out=srcT[:, :, :], in_=oT.rearrange("(kc p) s -> p kc s", p=128)[:, :, t0:t0 + 512]),
                  W=[srcT])
            for tt in range(4):
                gt = t0 // 128 + tt
                r0 = i * S + gt * 128
                xb, x1b, hbb = xt[xi % 2], x1[xi % 2], h2b[xi % 2]
                xi += 1
                src = x_in[r0:r0 + 128, :] if l == 0 else xres[r0:r0 + 128, :]
                k.dma("sp", lambda e, xb=xb, src=src: e.dma_start(out=xb[:, :], in_=src), W=[xb])
                for n in range(4):
                    for kc in range(KC):
                        k.op("pe", lambda e, n=n, kc=kc, tt=tt: e.matmul(psA[n][:, :], srcT[:, kc, tt * 128:(tt + 1) * 128],
                                                                         w_sb[:, kc, n * 512:(n + 1) * 512], start=(kc == 0), stop=(kc == KC - 1)),
                             R=[srcT, w_sb], W=[psA[n]])
                    k.op("dve", lambda e, n=n, x1b=x1b: e.tensor_tensor(out=x1b[:, n * 512:(n + 1) * 512], in0=psA[n][:, :],
                                                                       in1=G1[:, n * 512:(n + 1) * 512], op=ALU.mult), R=[psA[n], G1], W=[x1b])
                k.op("pool", lambda e, x1b=x1b, xb=xb: e.tensor_tensor(out=x1b[:, :], in0=x1b[:, :], in1=xb[:, :], op=ALU.add),
                     R=[x1b, xb], W=[x1b])
                k.dma("act", lambda e, x1b=x1b, r0=r0: e.dma_start(out=xres[r0:r0 + 128, :], in_=x1b[:, :]), R=[x1b])
                norm_mod(ns, x1b, A2, B2, hbb, out_f32=h2f)
                k.dma("act", lambda e, hbb=hbb, r0=r0: e.dma_start(out=hrows[r0:r0 + 128, :], in_=hbb[:, :]), R=[hbb])
                for g in range(4):
                    pz = psA[4 + g % 2]
                    for q in range(4):
                        kc = g * 4 + q
                        k.op("pe", lambda e, pz=pz, q=q, kc=kc: e.transpose(out=pz[:, q * 128:(q + 1) * 128],
                                                                            in_=h2f[:, kc * 128:(kc + 1) * 128], identity=ident_f[:, :]),
                             R=[h2f, ident_f], W=[pz])
                    k.op("act", lambda e, pz=pz, g=g: e.copy(out=h2T[:, g * 4:(g + 1) * 4, :], in_=pz[:, :].rearrange("p (q t) -> p q t", q=4)),
                         R=[pz], W=[h2T])
                for kc in range(KC):
                    k.op("pe", lambda e, kc=kc: e.matmul(psA[4][:, 0:E], h2T[:, kc, :], wr_sb[:, kc, :], start=(kc == 0), stop=(kc == KC - 1)),
                         R=[h2T, wr_sb], W=[psA[4]])
                lg = rs.logits
                k.op("dve", lambda e, gt=gt: e.tensor_tensor(out=lg[:, gt, :], in0=psA[4][:, 0:E], in1=rb_sb[:, :], op=ALU.add),
                     R=[psA[4], rb_sb], W=[lg])
                k.op("dve", lambda e, gt=gt: e.max(out=m8[:, :], in_=lg[:, gt, :]), R=[lg], W=[m8])
                k.op("dve", lambda e, gt=gt: e.tensor_scalar(out=msk[:, :], in0=lg[:, gt, :], scalar1=m8[:, 3:4], scalar2=None, op0=ALU.is_ge),
                     R=[lg, m8], W=[msk])
                k.op("pe", lambda e: e.matmul(psA[5][:, 0:E], utri_f[:, :], msk[:, :], start=True, stop=True), R=[utri_f, msk], W=[psA[5]])
                k.op("dve", lambda e, gt=gt: e.tensor_tensor(out=rs.pos[:, gt, :], in0=psA[5][:, 0:E], in1=rs.base[:, :], op=ALU.add),
                     R=[psA[5], rs.base], W=[rs.pos])
                k.op("pe", lambda e: e.matmul(psA[5][:, 0:E], ones_f[:, :], msk[:, :], start=True, stop=True), R=[ones_f, msk], W=[psA[5]])
                k.op("dve", lambda e: e.tensor_tensor(out=rs.base[:, :], in0=psA[5][:, 0:E], in1=rs.base[:, :], op=ALU.add),
                     R=[psA[5], rs.base], W=[rs.base])
        k.barrier()
        pes.close()

    def moe_phase(l, i, rs):
        RB = i * S
        pes = ExitStack()
        padf = k.sb(pes, "padf", [128, E], F32)
        padi = k.sb(pes, "padi", [128, E], I32)
        pend = k.sb(pes, "pend", [128, E], F32)
        pstart = k.sb(pes, "pstart", [128, E], F32)
        blks = k.sb(pes, "blks", [128, NBLK], F32)
        blke = k.sb(pes, "blke", [128, NBLK], F32)
        tmpb = k.sb(pes, "tmpb", [128, NBLK], F32)
        k.dma("sp", lambda e: e.dma_start(out=blks[:, :], in_=blks_in), W=[blks])
        k.op("dve", lambda e: e.tensor_scalar(out=padf[:, :], in0=rs.base[:, :], scalar1=float(BLK - 1), scalar2=None, op0=ALU.add),
             R=[rs.base], W=[padf])
        k.op("dve", lambda e: e.tensor_copy(out=padi[:, :], in_=padf[:, :]), R=[padf], W=[padi])
        k.op("dve", lambda e: e.tensor_scalar(out=padi[:, :], in0=padi[:, :], scalar1=9, scalar2=None, op0=ALU.arith_shift_right),
             R=[padi], W=[padi])
        k.op("dve", lambda e: e.tensor_scalar(out=padi[:, :], in0=padi[:, :], scalar1=9, scalar2=None, op0=ALU.logical_shift_left),
             R=[padi], W=[padi])
        k.op("dve", lambda e: e.tensor_copy(out=padf[:, :], in_=padi[:, :]), R=[padi], W=[padf])
        k.op("dve", lambda e: e.tensor_tensor_scan(out=pend[:, :], data0=ones_f[:, 0:E], data1=padf[:, :], initial=0.0,
                                                   op0=ALU.mult, op1=ALU.add), R=[ones_f, padf], W=[pend])
        k.op("dve", lambda e: e.tensor_tensor(out=pstart[:, :], in0=pend[:, :], in1=padf[:, :], op=ALU.subtract), R=[pend, padf], W=[pstart])
        k.op("dve", lambda e: e.memset(blke[:, :], 0.0), W=[blke])
        for ee in range(E):
            k.op("dve", lambda e, ee=ee: e.scalar_tensor_tensor(out=blke[:, :], in0=blks[:, :], scalar=pend[:, ee:ee + 1], in1=blke[:, :],
                                                                 op0=ALU.is_ge, op1=ALU.add), R=[blks, pend, blke], W=[blke])
        k.op("dve", lambda e: e.tensor_scalar(out=blke[:, :], in0=blke[:, :], scalar1=float(E - 1), scalar2=None, op0=ALU.min), R=[blke], W=[blke])
        k.op("dve", lambda e: e.tensor_scalar(out=rs.idxb[:, :], in0=blke[:, :], scalar1=128.0, scalar2=pcol[:, 0:1], op0=ALU.mult, op1=ALU.add),
             R=[blke, pcol], W=[rs.idxb])
        k.op("dve", lambda e: e.tensor_copy(out=rs.idxd[:, :], in_=blke[:, :]), R=[blke], W=[rs.idxd])

        m8 = k.sb(pes, "dm8", [128, 8], F32)
        ex = k.sb(pes, "dex", [128, 4], F32)
        nm = k.sb(pes, "dnm", [128, 1], F32)
        es_ = k.sb(pes, "des", [128, 1], F32)
        oh = k.sb(pes, "doh", [128, E], F32)
        dsum = k.sb(pes, "dsum", [128, E], F32)
        junk = k.sb(pes, "djunk", [128, E], F32)
        dstf = k.sb(pes, "dstf", [128, 4], F32)
        hr = [k.sb(pes, "hr%d" % j, [128, D], BF16) for j in range(3)]
        lg = rs.logits
        for gt in range(NTB):
            hb = hr[gt % 3]
            k.dma("sp", lambda e, hb=hb, gt=gt: e.dma_start(out=hb[:, :], in_=hrows[RB + gt * 128:RB + (gt + 1) * 128, :]), W=[hb])
            k.op("dve", lambda e, gt=gt: e.max(out=m8[:, :], in_=lg[:, gt, :]), R=[lg], W=[m8])
            k.op("dve", lambda e: e.tensor_scalar(out=nm[:, :], in0=m8[:, 0:1], scalar1=-1.0, scalar2=None, op0=ALU.mult), R=[m8], W=[nm])
            k.op("act", lambda e: e.activation(out=ex[:, :], in_=m8[:, 0:4], func=AF.Exp, bias=nm[:, 0:1], scale=1.0, accum_out=es_[:, 0:1]),
                 R=[m8, nm], W=[ex, es_])
            k.op("dve", lambda e: e.reciprocal(out=es_[:, :], in_=es_[:, :]), R=[es_], W=[es_])
            k.op("dve", lambda e, gt=gt: e.tensor_scalar(out=rs.G[:, gt, :], in0=ex[:, :], scalar1=es_[:, 0:1], scalar2=None, op0=ALU.mult),
                 R=[ex, es_], W=[rs.G])
            k.op("dve", lambda e, gt=gt: e.tensor_tensor(out=dsum[:, :], in0=rs.pos[:, gt, :], in1=pstart[:, :], op=ALU.add),
                 R=[rs.pos, pstart], W=[dsum])
            for kk in range(TOPK):
                k.op("dve", lambda e, gt=gt, kk=kk: e.tensor_scalar(out=oh[:, :], in0=lg[:, gt, :], scalar1=m8[:, kk:kk + 1], scalar2=None,
                                                                    op0=ALU.is_equal), R=[lg, m8], W=[oh])
                k.op("dve", lambda e, kk=kk: e.scalar_tensor_tensor(out=junk[:, :], in0=oh[:, :], scalar=1.0, in1=dsum[:, :],
                                                                    op0=ALU.mult, op1=ALU.mult, accum_out=dstf[:, kk:kk + 1]),
                     R=[oh, dsum], W=[junk, dstf])
            k.op("dve", lambda e, gt=gt: e.tensor_copy(out=rs.dest[:, gt, :], in_=dstf[:, :]), R=[dstf], W=[rs.dest])
            for kk in range(TOPK):
                k.dma("pool", lambda e, hb=hb, gt=gt, kk=kk: e.indirect_dma_start(
                    out=xs[:, :], out_offset=bass.IndirectOffsetOnAxis(ap=rs.dest[:, gt, kk:kk + 1], axis=0),
                    in_=hb[:, :], in_offset=None), R=[hb, rs.dest])
        k.barrier()
        pes.close()

        pes = ExitStack()
        xsb = [k.sb(pes, "xsb%d" % j, [128, D], BF16) for j in range(2)]
        xT = k.sb(pes, "xT", [128, KC, 512], BF16)
        wgu = [k.sb(pes, "wgu%d" % j, [128, KC, 256], BF16) for j in range(2)]
        wdn = k.sb(pes, "wdn", [128, NFC, D], BF16)
        bgu = k.sb(pes, "bgu", [128, 2 * NFC], F32)
        bl1 = k.sb(pes, "bl1", [128, NFC], F32)
        bdn = k.sb(pes, "bdn", [128, D], F32)
        actT = k.sb(pes, "actT", [128, NFC, 512], BF16)
        g1 = k.sb(pes, "g1", [128, 512], F32)
        sg = k.sb(pes, "sg", [128, 512], F32)
        l2 = k.sb(pes, "l2", [128, 512], F32)
        gs = k.sb(pes, "gs", [128, 512], F32)
        yo = [k.sb(pes, "yo%d" % j, [128, D], F32) for j in range(2)]
        xi = 0
        yi = 0
        wi = 0
        for blk in range(NBLK):
            k.dma("pool", lambda e, blk=blk: e.indirect_dma_start(
                out=bgu[:, :], out_offset=None, in_=bgu_in[l][:, :],
                in_offset=bass.IndirectOffsetOnAxis(ap=rs.idxb[:, blk:blk + 1], axis=0)), R=[rs.idxb], W=[bgu])
            k.dma("pool", lambda e, blk=blk: e.indirect_dma_start(
                out=bdn[:, :], out_offset=None, in_=bdn_in[l][:, :],
                in_offset=bass.IndirectOffsetOnAxis(ap=rs.idxd[:, blk:blk + 1], axis=0)), R=[rs.idxd], W=[bdn])
            k.op("dve", lambda e: e.tensor_scalar(out=bl1[:, :], in0=bgu[:, NFC:2 * NFC], scalar1=1.0, scalar2=None, op0=ALU.add), R=[bgu], W=[bl1])
            for st in range(4):
                xb = xsb[xi % 2]
                xi += 1
                r0 = blk * BLK + st * 128
                k.dma("sp", lambda e, xb=xb, r0=r0: e.dma_start(out=xb[:, :], in_=xs[r0:r0 + 128, :]), W=[xb])
                transpose_bf(xb, xT, st * 128)
            for fc in range(NFC):
                wg = wgu[wi % 2]
                wi += 1
                k.dma("pool", lambda e, wg=wg, blk=blk, fc=fc: e.indirect_dma_start(
                    out=wg[:, :, :].rearrange("p a b -> p (a b)"), out_offset=None, in_=wgu_b[l][fc][:, :],
                    in_offset=bass.IndirectOffsetOnAxis(ap=rs.idxb[:, blk:blk + 1], axis=0)), R=[rs.idxb], W=[wg])
                for half in range(2):
                    pz = psA[half]
                    for kc in range(KC):
                        k.op("pe", lambda e, pz=pz, wg=wg, kc=kc, half=half: e.matmul(
                            pz[:, :], wg[:, kc, half * 128:(half + 1) * 128], xT[:, kc, :], start=(kc == 0), stop=(kc == KC - 1)),
                            R=[wg, xT], W=[pz])
                k.op("dve", lambda e, fc=fc: e.tensor_scalar(out=g1[:, :], in0=psA[0][:, :], scalar1=bgu[:, fc:fc + 1], scalar2=LIMIT,
                                                             op0=ALU.add, op1=ALU.min), R=[psA[0], bgu], W=[g1])
                k.op("act", lambda e: e.activation(out=sg[:, :], in_=g1[:, :], func=AF.Sigmoid, scale=ALPHA), R=[g1], W=[sg])
                k.op("dve", lambda e, fc=fc: e.tensor_scalar(out=l2[:, :], in0=psA[1][:, :], scalar1=bl1[:, fc:fc + 1], scalar2=1.0 - LIMIT,
                                                             op0=ALU.add, op1=ALU.max), R=[psA[1], bl1], W=[l2])
                k.op("pool", lambda e: e.tensor_tensor(out=gs[:, :], in0=g1[:, :], in1=sg[:, :], op=ALU.mult), R=[g1, sg], W=[gs])
                k.op("dve", lambda e, fc=fc: e.scalar_tensor_tensor(out=actT[:, fc, :], in0=l2[:, :], scalar=1.0 + LIMIT, in1=gs[:, :],
                                                                     op0=ALU.min, op1=ALU.mult), R=[l2, gs], W=[actT])
            for fc in range(NFC):
                k.dma("pool", lambda e, blk=blk, fc=fc: e.indirect_dma_start(
                    out=wdn[:, fc, :], out_offset=None, in_=wdn_b[l][fc][:, :],
                    in_offset=bass.IndirectOffsetOnAxis(ap=rs.idxb[:, blk:blk + 1], axis=0)), R=[rs.idxb], W=[wdn])
            for st in range(4):
                y_ = yo[yi % 2]
                yi += 1
                for n in range(4):
                    pz = psA[2 + n]
                    for fc in range(NFC):
                        k.op("pe", lambda e, pz=pz, fc=fc, st=st, n=n: e.matmul(
                            pz[:, :], actT[:, fc, st * 128:(st + 1) * 128], wdn[:, fc, n * 512:(n + 1) * 512],
                            start=(fc == 0), stop=(fc == NFC - 1)), R=[actT, wdn], W=[pz])
                    k.op("dve", lambda e, pz=pz, y_=y_, n=n: e.tensor_tensor(out=y_[:, n * 512:(n + 1) * 512], in0=pz[:, :],
                                                                            in1=bdn[:, n * 512:(n + 1) * 512], op=ALU.add), R=[pz, bdn], W=[y_])
                r0 = blk * BLK + st * 128
                for hf in range(2):
                    k.dma("act", lambda e, y_=y_, r0=r0, hf=hf: e.dma_start(out=ys[hf][r0:r0 + 128, :], in_=y_[:, hf * 1024:(hf + 1) * 1024]), R=[y_])
        k.barrier()
        pes.close()

        pes = ExitStack()
        rows = [k.sb(pes, "cr%d" % j, [128, D], F32) for j in range(4)]
        G2 = load_mod(pes, i, l, 5, "G2")
        x1 = [k.sb(pes, "cx%d" % j, [128, D], F32) for j in range(2)]
        acc = k.sb(pes, "cacc", [128, D], F32)
        xo = [k.sb(pes, "cxo%d" % j, [128, D], F32) for j in range(2)]
        gfin = k.sb(pes, "gfin", [128, D], F32)
        ns = norm_tiles(pes, "nf") if l == 1 else None
        if l == 1:
            k.dma("sp", lambda e: e.dma_start(out=gfin[:, :], in_=gfin_in), W=[gfin])
        for gt in range(NTB):
            xb = x1[gt % 2]
            k.dma("sp", lambda e, xb=xb, gt=gt: e.dma_start(out=xb[:, :], in_=xres[RB + gt * 128:RB + (gt + 1) * 128, :]), W=[xb])
            for kk in range(TOPK):
                for hf in range(2):
                    k.dma("pool", lambda e, gt=gt, kk=kk, hf=hf: e.indirect_dma_start(
                        out=rows[kk][:, hf * 1024:(hf + 1) * 1024], out_offset=None, in_=ys[hf][:, :],
                        in_offset=bass.IndirectOffsetOnAxis(ap=rs.dest[:, gt, kk:kk + 1], axis=0)), R=[rs.dest], W=[rows[kk]])
            k.op("dve", lambda e, gt=gt: e.tensor_scalar(out=acc[:, :], in0=rows[0][:, :], scalar1=rs.G[:, gt, 0:1], scalar2=None, op0=ALU.mult),
                 R=[rows[0], rs.G], W=[acc])
            for kk in range(1, TOPK):
                k.op("dve", lambda e, gt=gt, kk=kk: e.scalar_tensor_tensor(out=acc[:, :], in0=rows[kk][:, :], scalar=rs.G[:, gt, kk:kk + 1],
                                                                           in1=acc[:, :], op0=ALU.mult, op1=ALU.add), R=[rows[kk], rs.G, acc], W=[acc])
            xo_ = xo[gt % 2]
            k.op("pool", lambda e: e.tensor_tensor(out=acc[:, :], in0=acc[:, :], in1=G2[:, :], op=ALU.mult), R=[acc, G2], W=[acc])
            k.op("pool", lambda e, xo_=xo_, xb=xb: e.tensor_tensor(out=xo_[:, :], in0=acc[:, :], in1=xb[:, :], op=ALU.add), R=[acc, xb], W=[xo_])
            if l == 0:
                k.dma("act", lambda e, xo_=xo_, gt=gt: e.dma_start(out=xres[RB + gt * 128:RB + (gt + 1) * 128, :], in_=xo_[:, :]), R=[xo_])
            else:
                norm_mod(ns, xo_, gfin, None, None)
                k.dma("act", lambda e, gt=gt: e.dma_start(out=out_d[RB + gt * 128:RB + (gt + 1) * 128, :], in_=ns.tmp[:, :]), R=[ns.tmp])
        k.barrier()
        pes.close()

    def conv_phase(i):
        pes = ExitStack()
        A1 = load_mod(pes, i, 1, 1, "cA1")
        B1 = load_mod(pes, i, 1, 0, "cB1")
        ns = norm_tiles(pes, "nc")
        xt = [k.sb(pes, "cxt%d" % j, [128, D], F32) for j in range(2)]
        hb = [k.sb(pes, "chb%d" % j, [128, D], BF16) for j in range(2)]
        hT = k.sb(pes, "chT", [128, KC, 512], BF16)
        wsl = [k.sb(pes, "cws%d" % j, [128, KC, 128], BF16) for j in range(3)]
        tmpf = k.sb(pes, "ctmp", [128, 512], F32)
        zo = k.sb(pes, "zo", [128, KC, 512], BF16)
        go = k.sb(pes, "go", [128, KC, 512], BF16)
        zpad = k.sb(pes, "zpad", [128, KC, 1], BF16)
        k.op("dve", lambda e: e.memset(zpad[:, :, :], 0.0), W=[zpad])
        zv = zT.rearrange("(kc p) s -> p kc s", p=128)
        gv = gbT.rearrange("(kc p) s -> p kc s", p=128)
        with nc.allow_non_contiguous_dma(reason="2-byte pad columns"):
            k.dma("sp", lambda e: e.dma_start(out=zv[:, :, 0:1], in_=zpad[:, :, :]), R=[zpad])
            k.dma("sp", lambda e: e.dma_start(out=zv[:, :, S + 1:S + 2], in_=zpad[:, :, :]), R=[zpad])
        xi = 0
        wi = 0
        for c in range(NCH):
            t0 = c * 512
            for tt in range(4):
                xb, hbb = xt[xi % 2], hb[xi % 2]
                xi += 1
                r0 = i * S + t0 + tt * 128
                k.dma("sp", lambda e, xb=xb, r0=r0: e.dma_start(out=xb[:, :], in_=xres[r0:r0 + 128, :]), W=[xb])
                norm_mod(ns, xb, A1, B1, hbb)
                transpose_bf(hbb, hT, tt * 128)

            def proj(m, pz):
                nonlocal wi
                ws = wsl[wi % 3]
                wi += 1
                k.dma("sp", lambda e, ws=ws, m=m: e.dma_start(out=ws[:, :, :].rearrange("p a b -> p (a b)"), in_=cwin_b[m * 128:(m + 1) * 128, :]),
                      W=[ws])
                for kc in range(KC):
                    k.op("pe", lambda e, ws=ws, kc=kc, pz=pz: e.matmul(pz[:, :], ws[:, kc, :], hT[:, kc, :], start=(kc == 0), stop=(kc == KC - 1)),
                         R=[ws, hT], W=[pz])
            for jc in range(KC):
                proj(16 + jc, psA[0])
                proj(32 + jc, psA[1])
                proj(jc, psA[2])
                k.op("act", lambda e: e.copy(out=tmpf[:, :], in_=psA[0][:, :]), R=[psA[0]], W=[tmpf])
                k.op("dve", lambda e, jc=jc: e.tensor_tensor(out=zo[:, jc, :], in0=psA[1][:, :], in1=tmpf[:, :], op=ALU.mult),
                     R=[psA[1], tmpf], W=[zo])
                k.op("act", lambda e, jc=jc: e.copy(out=go[:, jc, :], in_=psA[2][:, :]), R=[psA[2]], W=[go])
            k.dma("act", lambda e, t0=t0: e.dma_start(out=zv[:, :, 1 + t0:1 + t0 + 512], in_=zo[:, :, :]), R=[zo])
            k.dma("act", lambda e, t0=t0: e.dma_start(out=gv[:, :, t0:t0 + 512], in_=go[:, :, :]), R=[go])
        k.barrier()
        pes.close()
        pes = ExitStack()
        cw = k.sb(pes, "cw", [128, KC, 3], F32)
        k.dma("sp", lambda e: e.dma_start(out=cw[:, :, :].rearrange("p a b -> p (a b)"), in_=cw_in), W=[cw])
        zi = [k.sb(pes, "zi%d" % j, [128, KC, 514], BF16) for j in range(2)]
        gi = [k.sb(pes, "gi%d" % j, [128, KC, 512], BF16) for j in range(2)]
        ca = k.sb(pes, "ca", [128, 512], F32)
        cb = k.sb(pes, "cb", [128, 512], F32)
        vo = [k.sb(pes, "vo%d" % j, [128, KC, 512], BF16) for j in range(2)]
        ov = oT.rearrange("(kc p) s -> p kc s", p=128)
        for c in range(NCH):
            t0 = c * 512
            z_, g_, v_ = zi[c % 2], gi[c % 2], vo[c % 2]
            k.dma("sp", lambda e, z_=z_, t0=t0: e.dma_start(out=z_[:, :, :], in_=zv[:, :, t0:t0 + 514]), W=[z_])
            k.dma("sp", lambda e, g_=g_, t0=t0: e.dma_start(out=g_[:, :, :], in_=gv[:, :, t0:t0 + 512]), W=[g_])
            for jc in range(KC):
                k.op("act", lambda e, z_=z_, jc=jc: e.activation(out=ca[:, :], in_=z_[:, jc, 0:512], func=AF.Copy, scale=cw[:, jc, 0:1]),
                     R=[z_, cw], W=[ca])
                k.op("dve", lambda e, z_=z_, jc=jc: e.scalar_tensor_tensor(out=cb[:, :], in0=z_[:, jc, 1:513], scalar=cw[:, jc, 1:2], in1=ca[:, :],
                                                                           op0=ALU.mult, op1=ALU.add), R=[z_, cw, ca], W=[cb])
                k.op("dve", lambda e, z_=z_, jc=jc: e.scalar_tensor_tensor(out=ca[:, :], in0=z_[:, jc, 2:514], scalar=cw[:, jc, 2:3], in1=cb[:, :],
                                                                           op0=ALU.mult, op1=ALU.add), R=[z_, cw, cb], W=[ca])
                k.op("pool", lambda e, g_=g_, v_=v_, jc=jc: e.tensor_tensor(out=v_[:, jc, :], in0=ca[:, :], in1=g_[:, jc, :], op=ALU.mult),
                     R=[ca, g_], W=[v_])
            k.dma("act", lambda e, v_=v_, t0=t0: e.dma_start(out=ov[:, :, t0:t0 + 512], in_=v_[:, :, :]), R=[v_])
        k.barrier()
        pes.close()

    STOP = cfg.get("STOP", 10 ** 9)
    step = [0]

    def go():
        step[0] += 1
        return step[0] <= STOP

    if go():
        cast_phase()
    if go():
        ada_phase()
    for l in range(2):
        for i in range(NBC):
            ges = ExitStack()
            rs = RouteState()
            rs.logits = k.sb(ges, "logits", [128, NTB, E], F32)
            rs.pos = k.sb(ges, "pos", [128, NTB, E], F32)
            rs.base = k.sb(ges, "base", [128, E], F32)
            rs.G = k.sb(ges, "G", [128, NTB, 4], F32)
            rs.dest = k.sb(ges, "dest", [128, NTB, 4], I32)
            rs.idxb = k.sb(ges, "idxb", [128, NBLK], I32)
            rs.idxd = k.sb(ges, "idxd", [128, NBLK], I32)
            k.op("dve", lambda e: e.memset(rs.base[:, :], 0.0), W=[rs.base])
            if l == 0:
                if go():
                    mla_latent_phase(i)
                if go():
                    attention_phase(i)
                if go():
                    mix_phase(0, i, wout_b, rs)
            else:
                if go():
                    conv_phase(i)
                if go():
                    mix_phase(1, i, cwout_b, rs)
            if go():
                moe_phase(l, i, rs)
            k.barrier()
            ges.close()
    k.barrier()
    es.close()
    return nc


def rope_tables(S):
    rows = S // GRID_W
    row = np.repeat(np.arange(rows), GRID_W).astype(np.float32)
    col = np.tile(np.arange(GRID_W), rows).astype(np.float32)
    half = ROPE // 2
    inv = (1.0 / (np.float32(THETA) ** (np.arange(0, half, 2, dtype=np.float32) / np.float32(half)))).astype(np.float32)
    ang_r = row[:, None] * inv[None, :]
    ang_c = col[:, None] * inv[None, :]
    cr, sr, cc, sc = np.cos(ang_r), np.sin(ang_r), np.cos(ang_c), np.sin(ang_c)
    cosT = np.concatenate([cr, cr, cc, cc], axis=1).T.astype(np.float32)
    sinT = np.concatenate([-sr, sr, -sc, sc], axis=1).T.astype(np.float32)
    return np.ascontiguousarray(cosT), np.ascontiguousarray(sinT)


ROPE_PERM = np.concatenate([np.arange(16, 32), np.arange(0, 16), np.arange(48, 64), np.arange(32, 48)])


def rep128(v):
    v = np.asarray(v, np.float32).reshape(1, -1)
    return np.ascontiguousarray(np.broadcast_to(v, (128, v.shape[1])))


def prep_shared(inp, cfg):
    S, NBC, E, F = cfg["S"], cfg["NBC"], cfg["E"], cfg["F"]
    NFC = F // 128
    A = S * TOPK
    NBLK = -(-(A + E * (BLK - 1)) // BLK)
    m = {}
    for l in range(2):
        m["adaw%d" % l] = np.ascontiguousarray(inp["ada_w"][l])
        m["adab%d" % l] = rep128(inp["ada_b"][l])
        m["gmix%d" % l] = rep128(inp["norm_mix_g"][l])
        m["gffn%d" % l] = rep128(inp["norm_ffn_g"][l])
        m["wr%d" % l] = np.ascontiguousarray(inp["router_w"][l])
        m["rb%d" % l] = rep128(inp["router_b"][l])
        wgu = inp["expert_w_gu"][l]
        wgu = wgu.reshape(E, KC, 128, 2, NFC, 128).transpose(4, 0, 2, 1, 3, 5)
        m["wgu%d" % l] = np.ascontiguousarray(wgu).reshape(NFC * E * 128, KC * 256)
        wdn = inp["expert_w_down"][l].reshape(E, NFC, 128, D).transpose(1, 0, 2, 3)
        m["wdn%d" % l] = np.ascontiguousarray(wdn).reshape(NFC * E * 128, D)
        bgu = inp["expert_b_gu"][l].reshape(E, 2, NFC, 128).transpose(0, 3, 1, 2)
        m["bgu%d" % l] = np.ascontiguousarray(bgu).reshape(E * 128, 2 * NFC)
        m["bdn%d" % l] = np.ascontiguousarray(inp["expert_b_down"][l])
    m["gfin"] = rep128(inp["final_norm_g"])
    w_in = inp["mla_w_in"][0]
    m["mla_win"] = np.ascontiguousarray(np.concatenate([w_in, w_in[:, 1024 + ROPE_PERM]], axis=1))
    m["gq"] = np.ascontiguousarray(inp["mla_q_norm_g"][0].reshape(4, 128).T)
    m["gkv"] = np.ascontiguousarray(inp["mla_kv_norm_g"][0].reshape(4, 128).T)
    wq = inp["mla_w_q_up"][0].reshape(QL, NH, 192)
    m["mla_wq"] = np.ascontiguousarray(np.concatenate([wq, wq[:, :, 128 + ROPE_PERM]], axis=2)).reshape(QL, NH * 256)
    m["mla_wkv"] = np.ascontiguousarray(inp["mla_w_kv_up"][0])
    m["mla_wout"] = np.ascontiguousarray(inp["mla_w_out"][0])
    cwin = inp["conv_w_in"][0].reshape(KC, 128, 48, 128).transpose(2, 1, 0, 3)
    m["conv_win"] = np.ascontiguousarray(cwin).reshape(48 * 128, KC * 128)
    m["conv_w"] = np.ascontiguousarray(inp["conv_w"][0].reshape(3, KC, 128).transpose(2, 1, 0)).reshape(128, KC * 3)
    m["conv_wout"] = np.ascontiguousarray(inp["conv_w_out"][0])
    m["ident"] = np.eye(128, dtype=np.float32)
    m["utri"] = np.triu(np.ones((128, 128), np.float32), 1)
    cosT, sinT = rope_tables(S)
    m["ropec"], m["ropes"] = cosT, sinT
    m["blkstart"] = rep128(np.arange(NBLK, dtype=np.float32) * BLK)
    m["pcol"] = np.arange(128, dtype=np.float32).reshape(128, 1)
    return m


def run(inp, cfg):
    S, NBC, NCORES = cfg["S"], cfg["NBC"], cfg["NCORES"]
    shared = prep_shared(inp, cfg)
    in_maps = []
    for c in range(NCORES):
        b0 = c * NBC
        m = dict(shared)
        m["x"] = np.ascontiguousarray(inp["x"][b0:b0 + NBC]).reshape(NBC * S, D)
        m["ctx"] = np.ascontiguousarray(inp["ctx"][b0:b0 + NBC]).reshape(NBC * CTX, D)
        cc = np.concatenate([inp["c"][b0:b0 + NBC], inp["c_ctx"][None, :]], axis=0)
        m["cT"] = np.ascontiguousarray(cc.reshape(NBC + 1, KC, 128).transpose(2, 0, 1)).reshape(128, (NBC + 1) * KC)
        in_maps.append(m)
    nc = build_nc(cfg)
    res = run_bass_kernel_spmd(nc, in_maps, core_ids=list(range(NCORES)))
    outs = [np.asarray(res.results[c]["out"]).reshape(NBC, S, D) for c in range(NCORES)]
    return np.concatenate(outs, axis=0).astype(np.float32)


def kernel(**inputs):
    inp = {k_: np.asarray(v) for k_, v in inputs.items()}
    return run(inp, FULL_CFG)
```

```python
import numpy as np
import ml_dtypes
from contextlib import ExitStack
import concourse.bass as bass
import concourse.mybir as mybir
from concourse.bass_utils import run_bass_kernel_spmd

F32 = mybir.dt.float32
BF16 = mybir.dt.bfloat16
I32 = mybir.dt.int32
AF = mybir.ActivationFunctionType
ALU = mybir.AluOpType
AX = mybir.AxisListType

D = 2048
KC = D // 128
NH = 16
QL = 512
KVL = 512
ROPE = 64
CTX = 256
TOPK = 4
BLK = 512
EPS = 1e-6
ATT_SCALE = (128 + 64) ** -0.5
LIMIT = 7.0
ALPHA = 1.702
GRID_W = 64
THETA = 10000.0

FULL_CFG = dict(S=8192, NBC=1, E=32, F=2048, NCORES=4, BATCH=4)


class Buf:
    __slots__ = ("w", "r", "name", "ex")

    def __init__(self, name=""):
        self.w = None
        self.r = {}
        self.name = name
        self.ex = False


class Tile:
    def __init__(self, t, name):
        self.t = t
        self.b = Buf(name)

    def __getitem__(self, k):
        return self.t[k]


class K:
    def __init__(self, nc, es):
        self.nc = nc
        self.es = es
        self.eng = {"pe": nc.tensor, "act": nc.scalar, "dve": nc.vector, "pool": nc.gpsimd, "sp": nc.sync}
        self.sem = {}
        self.cnt = {}
        for n in ("pe", "act", "dve", "pool"):
            self.sem[n] = es.enter_context(nc.semaphore("c_" + n))
            self.cnt[n] = 0
        self.waited = {n: {} for n in self.eng}
        self.dsem = {}
        for q, k in (("sp", 20), ("pool", 20), ("act", 8)):
            self.dsem[q] = [[es.enter_context(nc.semaphore("d_%s%d" % (q, i))), 0] for i in range(k)]
        self.drr = {q: 0 for q in self.dsem}
        self.semkey = {}

    def _key(self, sem):
        return id(sem)

    def wait(self, en, tok):
        sem, val, prod = tok
        if en == "pe" and prod == "pe":
            return
        k = self._key(sem)
        if self.waited[en].get(k, 0) >= val:
            return
        self.eng[en].wait_ge(sem, val)
        self.waited[en][k] = val

    def _deps(self, en, R, W):
        for b in R:
            if b.w is not None:
                self.wait(en, b.w)
            if b.ex:
                for key, tok in b.r.items():
                    if key != en:
                        self.wait(en, tok)
        for b in W:
            if b.w is not None:
                self.wait(en, b.w)
            for tok in b.r.values():
                self.wait(en, tok)

    def _post(self, tok, R, W, rkey):
        for b in R:
            b.r[rkey] = tok
        for b in W:
            b.w = tok
            b.r = {}

    def op(self, en, fn, R=(), W=()):
        R = [x.b if isinstance(x, Tile) else x for x in R]
        W = [x.b if isinstance(x, Tile) else x for x in W]
        self._deps(en, R, W)
        ins = fn(self.eng[en])
        self.cnt[en] += 1
        ins.then_inc(self.sem[en], 1)
        tok = (self.sem[en], self.cnt[en], en)
        self._post(tok, R, W, en)
        return tok

    def dma(self, q, fn, R=(), W=()):
        R = [x.b if isinstance(x, Tile) else x for x in R]
        W = [x.b if isinstance(x, Tile) else x for x in W]
        self._deps(q, R, W)
        i = self.drr[q]
        self.drr[q] = (i + 1) % len(self.dsem[q])
        slot = self.dsem[q][i]
        if slot[1] > 0:
            self.wait(q, (slot[0], slot[1], "dma"))
        ins = fn(self.eng[q])
        slot[1] += 16
        ins.then_inc(slot[0], 16)
        tok = (slot[0], slot[1], "dma")
        self._post(tok, R, W, ("d", q, i))
        return tok

    def barrier(self):
        toks = [(self.sem[n], self.cnt[n], n) for n in self.sem if self.cnt[n] > 0]
        for q in self.dsem:
            for s, v in self.dsem[q]:
                if v > 0:
                    toks.append((s, v, "dma"))
        for en in self.eng:
            for tok in toks:
                if en == "pe" and tok[2] == "pe":
                    continue
                self.wait(en, tok)

    def sb(self, es, name, shape, dt):
        self.uid = getattr(self, "uid", 0) + 1
        name = "s%d_%s" % (self.uid, name)
        return Tile(es.enter_context(self.nc.sbuf_tensor(name, list(shape), dt)), name)

    def ps(self, es, name, shape, dt):
        self.uid = getattr(self, "uid", 0) + 1
        name = "p%d_%s" % (self.uid, name)
        t = Tile(es.enter_context(self.nc.psum_tensor(name, list(shape), dt)), name)
        t.b.ex = True
        return t


def build_nc(cfg):
    S, NBC, E, F = cfg["S"], cfg["NBC"], cfg["E"], cfg["F"]
    NFC = F // 128
    T = NBC * S
    NT = T // 128
    NK = S + CTX
    NKT = NK // 128
    A = S * TOPK
    NBLK = -(-(A + E * (BLK - 1)) // BLK)
    NSLOT = NBLK * BLK
    NTB = S // 128
    NCH = S // 512

    nc = bass.Bass("TRN2", target_bir_lowering=False)
    es = ExitStack()
    k = K(nc, es)

    def din(name, shape, dt=F32):
        return nc.dram_tensor(name, list(shape), dt, kind="ExternalInput").ap()

    def dscr(name, shape, dt):
        return nc.dram_tensor(name, list(shape), dt, kind="Internal").ap()

    x_in = din("x", [T, D])
    ctx_in = din("ctx", [NBC * CTX, D])
    cT_in = din("cT", [128, (NBC + 1) * KC])
    out_d = nc.dram_tensor("out", [T, D], F32, kind="ExternalOutput").ap()
    adaw_in = [din("adaw%d" % l, [D, 6 * D]) for l in range(2)]
    adab_in = [din("adab%d" % l, [128, 6 * D]) for l in range(2)]
    gmix_in = [din("gmix%d" % l, [128, D]) for l in range(2)]
    gffn_in = [din("gffn%d" % l, [128, D]) for l in range(2)]
    gfin_in = din("gfin", [128, D])
    win_in = din("mla_win", [D, 1152])
    gq_in = din("gq", [128, 4])
    gkv_in = din("gkv", [128, 4])
    wq_in = din("mla_wq", [QL, NH * 256])
    wkv_in = din("mla_wkv", [KVL, NH * 256])
    wout_in = din("mla_wout", [D, D])
    cwin_in = din("conv_win", [48 * 128, KC * 128])
    cw_in = din("conv_w", [128, KC * 3])
    cwout_in = din("conv_wout", [D, D])
    wr_in = [din("wr%d" % l, [D, E]) for l in range(2)]
    rb_in = [din("rb%d" % l, [128, E]) for l in range(2)]
    wgu_in = [din("wgu%d" % l, [NFC * E * 128, KC * 256]) for l in range(2)]
    wdn_in = [din("wdn%d" % l, [NFC * E * 128, D]) for l in range(2)]
    bgu_in = [din("bgu%d" % l, [E * 128, 2 * NFC]) for l in range(2)]
    bdn_in = [din("bdn%d" % l, [E, D]) for l in range(2)]
    ident_in = din("ident", [128, 128])
    utri_in = din("utri", [128, 128])
    ropec_in = din("ropec", [64, S])
    ropes_in = din("ropes", [64, S])
    blks_in = din("blkstart", [128, NBLK])
    pcol_in = din("pcol", [128, 1])

    adaw_b = [dscr("adaw_b%d" % l, [D, 6 * D], BF16) for l in range(2)]
    win_b = dscr("win_b", [D, 1152], BF16)
    wq_b = dscr("wq_b", [QL, NH * 256], BF16)
    wkv_b = dscr("wkv_b", [KVL, NH * 256], BF16)
    wout_b = dscr("wout_b", [D, D], BF16)
    cwin_b = dscr("cwin_b", [48 * 128, KC * 128], BF16)
    cwout_b = dscr("cwout_b", [D, D], BF16)
    wgu_b = [[dscr("wgu_b%d_%d" % (l, fc), [E * 128, KC * 256], BF16) for fc in range(NFC)] for l in range(2)]
    wdn_b = [[dscr("wdn_b%d_%d" % (l, fc), [E * 128, D], BF16) for fc in range(NFC)] for l in range(2)]
    modv = dscr("modv", [(NBC + 1) * 2 * 6 * 128, D], F32)
    xres = dscr("xres", [T, D], F32)
    hrows = dscr("hrows", [T, D], BF16)
    cqnT = dscr("cqnT", [4 * 128, S], BF16)
    kvT = dscr("kvT", [4 * 128, NK], BF16)
    krT = dscr("krT", [64, NK], BF16)
    oT = dscr("oT", [D, S], BF16)
    zT = dscr("zT", [D, S + 2], BF16)
    gbT = dscr("gbT", [D, S], BF16)
    xs = dscr("xs", [NSLOT, D], BF16)
    ys = [dscr("ys%d" % j, [NSLOT, D // 2], F32) for j in range(2)]

    def modv_ap(i, l, which):
        r0 = ((i * 2 + l) * 6 + which) * 128
        return modv[r0:r0 + 128, :]

    ident_f = k.sb(es, "ident_f", [128, 128], F32)
    ident_bf = k.sb(es, "ident_bf", [128, 128], BF16)
    ones_bf = k.sb(es, "ones_bf", [128, 128], BF16)
    ones_f = k.sb(es, "ones_f", [128, 128], F32)
    utri_bf = k.sb(es, "utri_bf", [128, 128], BF16)
    utri_f = k.sb(es, "utri_f", [128, 128], F32)
    pcol = k.sb(es, "pcol", [128, 1], F32)
    psA = [k.ps(es, "psA%d" % i, [128, 512], F32) for i in range(6)]
    psT = [k.ps(es, "psT%d" % i, [128, 1024], BF16) for i in range(2)]

    k.dma("sp", lambda e: e.dma_start(out=ident_f[:, :], in_=ident_in), W=[ident_f])
    k.dma("sp", lambda e: e.dma_start(out=utri_f[:, :], in_=utri_in), W=[utri_f])
    k.dma("sp", lambda e: e.dma_start(out=pcol[:, :], in_=pcol_in), W=[pcol])
    k.op("dve", lambda e: e.tensor_copy(out=ident_bf[:, :], in_=ident_f[:, :]), R=[ident_f], W=[ident_bf])
    k.op("dve", lambda e: e.tensor_copy(out=utri_bf[:, :], in_=utri_f[:, :]), R=[utri_f], W=[utri_bf])
    k.op("dve", lambda e: e.memset(ones_bf[:, :], 1.0), W=[ones_bf])
    k.op("dve", lambda e: e.memset(ones_f[:, :], 1.0), W=[ones_f])

    def cast_phase():
        pes = ExitStack()
        CHW = 4096
        NB_ = 3
        ibuf = [k.sb(pes, "cw_i%d" % i, [128, CHW], F32) for i in range(NB_)]
        obuf = [k.sb(pes, "cw_o%d" % i, [128, CHW], BF16) for i in range(NB_)]
        jobs = []
        pairs = [(adaw_in[0], adaw_b[0]), (adaw_in[1], adaw_b[1]), (win_in, win_b), (wq_in, wq_b),
                 (wkv_in, wkv_b), (wout_in, wout_b), (cwin_in, cwin_b), (cwout_in, cwout_b)]
        for l in range(2):
            for fc in range(NFC):
                pairs.append((wgu_in[l][fc * E * 128:(fc + 1) * E * 128, :], wgu_b[l][fc]))
                pairs.append((wdn_in[l][fc * E * 128:(fc + 1) * E * 128, :], wdn_b[l][fc]))
        for src, dst in pairs:
            R_, C_ = src.shape
            assert R_ % 128 == 0
            sv = src.rearrange("(p a) c -> p (a c)", p=128)
            dv = dst.rearrange("(p a) c -> p (a c)", p=128)
            tot = (R_ // 128) * C_
            for c0 in range(0, tot, CHW):
                w = min(CHW, tot - c0)
                jobs.append((sv[:, c0:c0 + w], dv[:, c0:c0 + w], w))
        engs = ["dve", "pool", "act"]
        PRE = 2
        for j in range(min(PRE, len(jobs))):
            s_, d_, w = jobs[j]
            k.dma("sp", lambda e, s_=s_, w=w, j=j: e.dma_start(out=ibuf[j % NB_][:, 0:w], in_=s_), W=[ibuf[j % NB_]])
        for j, (s_, d_, w) in enumerate(jobs):
            if j + PRE < len(jobs):
                s2, d2, w2 = jobs[j + PRE]
                bi = (j + PRE) % NB_
                k.dma("sp", lambda e, s2=s2, w2=w2, bi=bi: e.dma_start(out=ibuf[bi][:, 0:w2], in_=s2), W=[ibuf[bi]])
            b = j % NB_
            en = engs[j % 3]
            if en == "act":
                k.op("act", lambda e, b=b, w=w: e.copy(out=obuf[b][:, 0:w], in_=ibuf[b][:, 0:w]), R=[ibuf[b]], W=[obuf[b]])
            else:
                k.op(en, lambda e, b=b, w=w: e.tensor_copy(out=obuf[b][:, 0:w], in_=ibuf[b][:, 0:w]), R=[ibuf[b]], W=[obuf[b]])
            k.dma("act", lambda e, b=b, w=w, d_=d_: e.dma_start(out=d_, in_=obuf[b][:, 0:w]), R=[obuf[b]])
        k.barrier()
        pes.close()

    def ada_phase():
        pes = ExitStack()
        cT = k.sb(pes, "cT", [128, (NBC + 1) * KC], F32)
        sT = k.sb(pes, "sT", [128, (NBC + 1) * KC], F32)
        scb = k.sb(pes, "scb", [128, (NBC + 1) * KC, 128], BF16)
        k.dma("sp", lambda e: e.dma_start(out=cT[:, :], in_=cT_in), W=[cT])
        k.op("act", lambda e: e.activation(out=sT[:, :], in_=cT[:, :], func=AF.Silu), R=[cT], W=[sT])
        for j in range((NBC + 1) * KC):
            k.op("dve", lambda e, j=j: e.tensor_scalar(out=scb[:, j, :], in0=ones_f[:, :], scalar1=sT[:, j:j + 1],
                                                       scalar2=None, op0=ALU.mult), R=[sT, ones_f], W=[scb])
        wsl = [k.sb(pes, "ada_w%d" % i, [128, KC, 512], BF16) for i in range(2)]
        bsl = [k.sb(pes, "ada_b%d" % i, [128, 512], F32) for i in range(2)]
        gsl = [k.sb(pes, "ada_g%d" % i, [128, 512], F32) for i in range(2)]
        osl = [k.sb(pes, "ada_o%d" % i, [128, 512], F32) for i in range(3)]
        it = 0
        oi = 0
        for l in range(2):
            for j in range(24):
                which, sub = j // 4, j % 4
                b = it % 2
                it += 1
                k.dma("sp", lambda e, l=l, j=j, b=b: e.dma_start(
                    out=wsl[b][:, :, :], in_=adaw_b[l][:, j * 512:(j + 1) * 512].rearrange("(kc p) n -> p kc n", p=128)),
                    W=[wsl[b]])
                k.dma("sp", lambda e, l=l, j=j, b=b: e.dma_start(out=bsl[b][:, :], in_=adab_in[l][:, j * 512:(j + 1) * 512]),
                      W=[bsl[b]])
                if which in (1, 4):
                    gsrc = gmix_in[l] if which == 1 else gffn_in[l]
                    k.dma("sp", lambda e, gsrc=gsrc, sub=sub, b=b: e.dma_start(out=gsl[b][:, :], in_=gsrc[:, sub * 512:(sub + 1) * 512]),
                          W=[gsl[b]])
                rows = list(range(NBC))
                if l == 0 and which < 2:
                    rows.append(NBC)
                for i in rows:
                    pz = psA[i % 4]
                    for kc in range(KC):
                        k.op("pe", lambda e, pz=pz, i=i, kc=kc, b=b: e.matmul(
                            pz[:, :], scb[:, i * KC + kc, :], wsl[b][:, kc, :], start=(kc == 0), stop=(kc == KC - 1)),
                            R=[scb, wsl[b]], W=[pz])
                    o = osl[oi % 3]
                    oi += 1
                    k.op("dve", lambda e, o=o, pz=pz, b=b: e.tensor_tensor(out=o[:, :], in0=pz[:, :], in1=bsl[b][:, :], op=ALU.add),
                         R=[pz, bsl[b]], W=[o])
                    if which in (1, 4):
                        k.op("dve", lambda e, o=o, b=b: e.scalar_tensor_tensor(out=o[:, :], in0=o[:, :], scalar=1.0, in1=gsl[b][:, :],
                                                                             op0=ALU.add, op1=ALU.mult), R=[o, gsl[b]], W=[o])
                    k.dma("act", lambda e, o=o, i=i, l=l, which=which, sub=sub: e.dma_start(
                        out=modv_ap(i, l, which)[:, sub * 512:(sub + 1) * 512], in_=o[:, :]), R=[o])
        k.barrier()
        pes.close()

    def rstd_from_ssq(ssq, rstd, n, width=1):
        k.op("dve", lambda e: e.tensor_scalar(out=rstd[:, 0:width], in0=ssq[:, 0:width], scalar1=1.0 / n, scalar2=EPS,
                                              op0=ALU.mult, op1=ALU.add), R=[ssq], W=[rstd])
        k.op("act", lambda e: e.activation(out=rstd[:, 0:width], in_=rstd[:, 0:width], func=AF.Sqrt), R=[rstd], W=[rstd])
        k.op("dve", lambda e: e.reciprocal(out=rstd[:, 0:width], in_=rstd[:, 0:width]), R=[rstd], W=[rstd])

    class NormState:
        pass

    def norm_tiles(pes, tag):
        ns = NormState()
        ns.junk = k.sb(pes, tag + "_junk", [128, D], BF16)
        ns.ssq = [k.sb(pes, tag + "_ssq%d" % i, [128, 1], F32) for i in range(2)]
        ns.rstd = [k.sb(pes, tag + "_rstd%d" % i, [128, 1], F32) for i in range(2)]
        ns.tmp = k.sb(pes, tag + "_tmp", [128, D], F32)
        ns.i = 0
        return ns

    def norm_mod(ns, xt, A_t, B_t, out_t, out_f32=None):
        i = ns.i % 2
        ns.i += 1
        ssq, rstd = ns.ssq[i], ns.rstd[i]
        k.op("act", lambda e: e.activation(out=ns.junk[:, :], in_=xt[:, :], func=AF.Square, accum_out=ssq[:, 0:1]),
             R=[xt], W=[ns.junk, ssq])
        rstd_from_ssq(ssq, rstd, D)
        k.op("dve", lambda e: e.scalar_tensor_tensor(out=ns.tmp[:, :], in0=xt[:, :], scalar=rstd[:, 0:1], in1=A_t[:, :],
                                                     op0=ALU.mult, op1=ALU.mult), R=[xt, rstd, A_t], W=[ns.tmp])
        if B_t is None:
            return
        if out_f32 is not None:
            k.op("pool", lambda e: e.tensor_tensor(out=out_f32[:, :], in0=ns.tmp[:, :], in1=B_t[:, :], op=ALU.add),
                 R=[ns.tmp, B_t], W=[out_f32])
            k.op("act", lambda e: e.copy(out=out_t[:, :], in_=out_f32[:, :]), R=[out_f32], W=[out_t])
        else:
            k.op("pool", lambda e: e.tensor_tensor(out=out_t[:, :], in0=ns.tmp[:, :], in1=B_t[:, :], op=ALU.add),
                 R=[ns.tmp, B_t], W=[out_t])

    def transpose_bf(h_t, hT, col0, ntile_cols=128):
        for half in range(2):
            pt = psT[half]
            for j in range(8):
                kc = half * 8 + j
                k.op("pe", lambda e, pt=pt, j=j, kc=kc: e.transpose(out=pt[:, j * 128:(j + 1) * 128],
                                                                    in_=h_t[:, kc * 128:(kc + 1) * 128], identity=ident_bf[:, :]),
                     R=[h_t, ident_bf], W=[pt])
            en = "act" if half == 0 else "dve"
            if en == "act":
                k.op("act", lambda e, pt=pt, half=half: e.copy(out=hT[:, half * 8:(half + 1) * 8, col0:col0 + 128],
                                                              in_=pt[:, :].rearrange("p (j t) -> p j t", j=8)), R=[pt], W=[hT])
            else:
                k.op("dve", lambda e, pt=pt, half=half: e.tensor_copy(out=hT[:, half * 8:(half + 1) * 8, col0:col0 + 128],
                                                                     in_=pt[:, :].rearrange("p (j t) -> p j t", j=8)), R=[pt], W=[hT])

    def load_mod(pes, i, l, which, name):
        t = k.sb(pes, name, [128, D], F32)
        k.dma("sp", lambda e: e.dma_start(out=t[:, :], in_=modv_ap(i, l, which)), W=[t])
        return t

    def mla_latent_phase(i):
        pes = ExitStack()
        A1 = load_mod(pes, i, 0, 1, "A1")
        B1 = load_mod(pes, i, 0, 0, "B1")
        Ac = load_mod(pes, NBC, 0, 1, "Ac")
        Bc = load_mod(pes, NBC, 0, 0, "Bc")
        win_sb = k.sb(pes, "win_sb", [128, KC, 1152], BF16)
        k.dma("sp", lambda e: e.dma_start(out=win_sb[:, :, :], in_=win_b.rearrange("(kc p) n -> p kc n", p=128)), W=[win_sb])
        gq = k.sb(pes, "gq", [128, 4], F32)
        gkv = k.sb(pes, "gkv", [128, 4], F32)
        k.dma("sp", lambda e: e.dma_start(out=gq[:, :], in_=gq_in), W=[gq])
        k.dma("sp", lambda e: e.dma_start(out=gkv[:, :], in_=gkv_in), W=[gkv])
        ns = norm_tiles(pes, "n1")
        xt = [k.sb(pes, "xt%d" % j, [128, D], F32) for j in range(2)]
        hb = [k.sb(pes, "hb%d" % j, [128, D], BF16) for j in range(2)]
        hT = k.sb(pes, "hT", [128, KC, 512], BF16)
        lat_f = k.sb(pes, "lat_f", [128, 4, 512], F32)
        lat_sq = k.sb(pes, "lat_sq", [128, 4, 512], BF16)
        lat_n = k.sb(pes, "lat_n", [128, 4, 512], BF16)
        rbc = k.sb(pes, "rbc", [128, 512], F32)
        cs = k.sb(pes, "cs", [64, 2, 512], F32)
        t1 = k.sb(pes, "t1", [64, 512], F32)
        t2 = k.sb(pes, "t2", [64, 512], F32)
        kr_o = k.sb(pes, "kr_o", [64, 512], BF16)
        xi = 0
        chunks = [("ctx", 0, CTX)] + [("lat", c * 512, 512) for c in range(NCH)]
        for kind, t0, w in chunks:
            ntile = w // 128
            for tt in range(ntile):
                xb = xt[xi % 2]
                hbb = hb[xi % 2]
                xi += 1
                if kind == "ctx":
                    src = ctx_in[i * CTX + tt * 128:i * CTX + (tt + 1) * 128, :]
                else:
                    src = x_in[i * S + t0 + tt * 128:i * S + t0 + (tt + 1) * 128, :]
                k.dma("sp", lambda e, xb=xb, src=src: e.dma_start(out=xb[:, :], in_=src), W=[xb])
                norm_mod(ns, xb, Ac if kind == "ctx" else A1, Bc if kind == "ctx" else B1, hbb)
                transpose_bf(hbb, hT, tt * 128)
            groups = ([("cq", 0)] if kind == "lat" else []) + [("ckv", 4)]
            for gname, m0 in groups:
                for j in range(4):
                    for kc in range(KC):
                        k.op("pe", lambda e, j=j, kc=kc, m0=m0: e.matmul(
                            psA[j][:, 0:w], win_sb[:, kc, (m0 + j) * 128:(m0 + j + 1) * 128], hT[:, kc, 0:w],
                            start=(kc == 0), stop=(kc == KC - 1)), R=[win_sb, hT], W=[psA[j]])
                    k.op("act", lambda e, j=j: e.copy(out=lat_f[:, j, 0:w], in_=psA[j][:, 0:w]), R=[psA[j]], W=[lat_f])
                    k.op("act", lambda e, j=j: e.activation(out=lat_sq[:, j, 0:w], in_=psA[j][:, 0:w], func=AF.Square),
                         R=[psA[j]], W=[lat_sq])
                for j in range(4):
                    k.op("pe", lambda e, j=j: e.matmul(psA[4][:, 0:w], ones_bf[:, :], lat_sq[:, j, 0:w], start=(j == 0), stop=(j == 3)),
                         R=[ones_bf, lat_sq], W=[psA[4]])
                k.op("dve", lambda e: e.tensor_scalar(out=rbc[:, 0:w], in0=psA[4][:, 0:w], scalar1=1.0 / 512, scalar2=EPS,
                                                      op0=ALU.mult, op1=ALU.add), R=[psA[4]], W=[rbc])
                k.op("act", lambda e: e.activation(out=rbc[:, 0:w], in_=rbc[:, 0:w], func=AF.Sqrt), R=[rbc], W=[rbc])
                k.op("dve", lambda e: e.reciprocal(out=rbc[:, 0:w], in_=rbc[:, 0:w]), R=[rbc], W=[rbc])
                gg = gq if gname == "cq" else gkv
                for j in range(4):
                    k.op("dve", lambda e, j=j, gg=gg: e.scalar_tensor_tensor(
                        out=lat_n[:, j, 0:w], in0=lat_f[:, j, 0:w], scalar=gg[:, j:j + 1], in1=rbc[:, 0:w],
                        op0=ALU.mult, op1=ALU.mult), R=[lat_f, gg, rbc], W=[lat_n])
                if gname == "cq":
                    dst = cqnT.rearrange("(j p) s -> p j s", p=128)[:, :, t0:t0 + w]
                else:
                    k0 = 0 if kind == "ctx" else CTX + t0
                    dst = kvT.rearrange("(j p) s -> p j s", p=128)[:, :, k0:k0 + w]
                k.dma("act", lambda e, dst=dst: e.dma_start(out=dst, in_=lat_n[:, :, 0:w]), R=[lat_n])
            for which, m0 in ((0, 1024), (1, 1088)):
                pz = psA[4 + which]
                for kc in range(KC):
                    k.op("pe", lambda e, pz=pz, kc=kc, m0=m0: e.matmul(pz[0:64, 0:w], win_sb[:, kc, m0:m0 + 64], hT[:, kc, 0:w],
                                                                        start=(kc == 0), stop=(kc == KC - 1)), R=[win_sb, hT], W=[pz])
            if kind == "ctx":
                k.op("act", lambda e: e.copy(out=kr_o[:, 0:w], in_=psA[4][0:64, 0:w]), R=[psA[4]], W=[kr_o])
                k.op("act", lambda e: e.copy(out=t2[:, 0:w], in_=psA[5][0:64, 0:w]), R=[psA[5]], W=[t2])
                k0 = 0
            else:
                k.dma("sp", lambda e: e.dma_start(out=cs[:, 0, :], in_=ropec_in[:, t0:t0 + 512]), W=[cs])
                k.dma("sp", lambda e: e.dma_start(out=cs[:, 1, :], in_=ropes_in[:, t0:t0 + 512]), W=[cs])
                k.op("dve", lambda e: e.tensor_tensor(out=t1[:, :], in0=psA[4][0:64, :], in1=cs[:, 0, :], op=ALU.mult),
                     R=[psA[4], cs], W=[t1])
                k.op("dve", lambda e: e.tensor_tensor(out=t2[:, :], in0=psA[5][0:64, :], in1=cs[:, 1, :], op=ALU.mult),
                     R=[psA[5], cs], W=[t2])
                k.op("pool", lambda e: e.tensor_tensor(out=kr_o[:, :], in0=t1[:, :], in1=t2[:, :], op=ALU.add),
                     R=[t1, t2], W=[kr_o])
                k0 = CTX + t0
            k.dma("act", lambda e, k0=k0: e.dma_start(out=krT[:, k0:k0 + w], in_=kr_o[:, 0:w]), R=[kr_o])
        k.barrier()
        pes.close()

    def attention_phase(i):
        pes = ExitStack()
        kv_sb = k.sb(pes, "kv_sb", [128, 4, NK], BF16)
        kr_sb = k.sb(pes, "kr_sb", [128, NK], BF16)
        k.dma("sp", lambda e: e.dma_start(out=kv_sb[:, :, :], in_=kvT.rearrange("(j p) s -> p j s", p=128)), W=[kv_sb])
        k.op("dve", lambda e: e.memset(kr_sb[64:128, :], 0.0), W=[kr_sb])
        k.dma("sp", lambda e: e.dma_start(out=kr_sb[0:64, :], in_=krT), W=[kr_sb])
        KnT = k.sb(pes, "KnT", [128, NK], BF16)
        V = k.sb(pes, "V", [128, NKT, 128], BF16)
        wqh = k.sb(pes, "wqh", [128, 4, 256], BF16)
        wkvh = k.sb(pes, "wkvh", [128, 4, 256], BF16)
        sqt = [k.sb(pes, "sqt%d" % j, [128, 512], BF16) for j in range(2)]
        kmx = k.sb(pes, "kmx", [128, 40], F32)
        krmax = k.sb(pes, "krmax", [128, 1], F32)
        kmax2 = k.sb(pes, "kmax2", [128, 1], F32)
        cqc = [k.sb(pes, "cqc%d" % j, [128, 4, 512], BF16) for j in range(2)]
        csq = [k.sb(pes, "csq%d" % j, [64, 2, 512], F32) for j in range(2)]
        QnT = [k.sb(pes, "QnT%d" % j, [128, 512], BF16) for j in range(2)]
        QrT = [k.sb(pes, "QrT%d" % j, [128, 512], BF16) for j in range(2)]
        sqn = k.sb(pes, "sqn", [128, 512], BF16)
        sqr = k.sb(pes, "sqr", [128, 512], BF16)
        for t_ in QrT + [sqr]:
            k.op("dve", lambda e, t_=t_: e.memset(t_[:, :], 0.0), W=[t_])
        t1 = k.sb(pes, "at1", [64, 512], F32)
        t2 = k.sb(pes, "at2", [64, 512], F32)
        qm = [k.sb(pes, "qm%d" % j, [128, 1], F32) for j in range(2)]
        nb = [k.sb(pes, "nb%d" % j, [128, 1], F32) for j in range(2)]
        PT = [k.sb(pes, "PT%d" % j, [128, 512], BF16) for j in range(4)]
        rec = k.sb(pes, "rec", [128, 512], F32)
        ot = [k.sb(pes, "ot%d" % j, [128, 512], BF16) for j in range(2)]
        NKC = -(-NK // 512)
        for c in range(NKC if cfg.get("ASTOP", 99) != -1 else 0):
            c0 = c * 512
            w = min(512, NK - c0)
            s_ = sqt[c % 2]
            k.op("act", lambda e, s_=s_, c0=c0, w=w: e.activation(out=s_[:, 0:w], in_=kr_sb[:, c0:c0 + w], func=AF.Square),
                 R=[kr_sb], W=[s_])
            k.op("pe", lambda e, s_=s_, w=w: e.matmul(psA[0][:, 0:w], ones_bf[:, :], s_[:, 0:w], start=True, stop=True),
                 R=[ones_bf, s_], W=[psA[0]])
            k.op("dve", lambda e, c=c, w=w: e.reduce_max(out=kmx[:, c:c + 1], in_=psA[0][:, 0:w], axis=AX.X), R=[psA[0]], W=[kmx])
        k.op("dve", lambda e: e.reduce_max(out=krmax[:, 0:1], in_=kmx[:, 0:NKC], axis=AX.X), R=[kmx], W=[krmax])
        it = 0
        AST = cfg.get("ASTOP", 99)

        class _Stop(Exception):
            pass

        def chk(n):
            if AST <= n:
                raise _Stop()
        try:
          chk(1)
          for h in range(NH):
              k.dma("sp", lambda e, h=h: e.dma_start(
                  out=wqh[:, :, :], in_=wq_b[:, h * 256:(h + 1) * 256].rearrange("(j p) n -> p j n", p=128)), W=[wqh])
              k.dma("sp", lambda e, h=h: e.dma_start(
                  out=wkvh[:, :, :], in_=wkv_b[:, h * 256:(h + 1) * 256].rearrange("(j p) n -> p j n", p=128)), W=[wkvh])
              for c in range(NKC):
                  c0 = c * 512
                  w = min(512, NK - c0)
                  pz = psA[c % 2]
                  for j in range(4):
                      k.op("pe", lambda e, pz=pz, j=j, c0=c0, w=w: e.matmul(pz[:, 0:w], wkvh[:, j, 0:128], kv_sb[:, j, c0:c0 + w],
                                                                            start=(j == 0), stop=(j == 3)), R=[wkvh, kv_sb], W=[pz])
                  k.op("dve", lambda e, pz=pz, c0=c0, w=w: e.tensor_copy(out=KnT[:, c0:c0 + w], in_=pz[:, 0:w]), R=[pz], W=[KnT])
                  s_ = sqt[c % 2]
                  k.op("act", lambda e, pz=pz, s_=s_, w=w: e.activation(out=s_[:, 0:w], in_=pz[:, 0:w], func=AF.Square), R=[pz], W=[s_])
                  k.op("pe", lambda e, s_=s_, w=w: e.matmul(psA[2][:, 0:w], ones_bf[:, :], s_[:, 0:w], start=True, stop=True),
                       R=[ones_bf, s_], W=[psA[2]])
                  k.op("dve", lambda e, c=c, w=w: e.reduce_max(out=kmx[:, c:c + 1], in_=psA[2][:, 0:w], axis=AX.X), R=[psA[2]], W=[kmx])
              k.op("dve", lambda e: e.reduce_max(out=kmax2[:, 0:1], in_=kmx[:, 0:NKC], axis=AX.X), R=[kmx], W=[kmax2])
              k.op("dve", lambda e: e.tensor_tensor(out=kmax2[:, 0:1], in0=kmax2[:, 0:1], in1=krmax[:, 0:1], op=ALU.add),
                   R=[kmax2, krmax], W=[kmax2])
              chk(2)
              for g in range(-(-NKT // 4)):
                  pz = psA[3 + g % 2]
                  nk_ = min(4, NKT - g * 4)
                  for q in range(nk_):
                      kt = g * 4 + q
                      for j in range(4):
                          k.op("pe", lambda e, pz=pz, q=q, kt=kt, j=j: e.matmul(
                              pz[:, q * 128:(q + 1) * 128], kv_sb[:, j, kt * 128:(kt + 1) * 128], wkvh[:, j, 128:256],
                              start=(j == 0), stop=(j == 3)), R=[kv_sb, wkvh], W=[pz])
                  k.op("act", lambda e, pz=pz, g=g, nk_=nk_: e.copy(
                      out=V[:, g * 4:g * 4 + nk_, :], in_=pz[:, 0:nk_ * 128].rearrange("p (q d) -> p q d", q=nk_)), R=[pz], W=[V])
              chk(3)
              for c in range(NCH):
                  t0 = c * 512
                  b2 = it % 2
                  it += 1
                  cq_ = cqc[b2]
                  cs_ = csq[b2]
                  k.dma("sp", lambda e, cq_=cq_, t0=t0: e.dma_start(
                      out=cq_[:, :, :], in_=cqnT.rearrange("(j p) s -> p j s", p=128)[:, :, t0:t0 + 512]), W=[cq_])
                  k.dma("sp", lambda e, cs_=cs_, t0=t0: e.dma_start(out=cs_[:, 0, :], in_=ropec_in[:, t0:t0 + 512]), W=[cs_])
                  k.dma("sp", lambda e, cs_=cs_, t0=t0: e.dma_start(out=cs_[:, 1, :], in_=ropes_in[:, t0:t0 + 512]), W=[cs_])
                  qn, qr = QnT[b2], QrT[b2]
                  for j in range(4):
                      k.op("pe", lambda e, j=j, cq_=cq_: e.matmul(psA[2][:, :], wqh[:, j, 0:128], cq_[:, j, :], start=(j == 0), stop=(j == 3)),
                           R=[wqh, cq_], W=[psA[2]])
                  k.op("dve", lambda e, qn=qn: e.tensor_copy(out=qn[:, :], in_=psA[2][:, :]), R=[psA[2]], W=[qn])
                  k.op("act", lambda e: e.activation(out=sqn[:, :], in_=psA[2][:, :], func=AF.Square), R=[psA[2]], W=[sqn])
                  for which in range(2):
                      pz = psA[3 + which]
                      for j in range(4):
                          k.op("pe", lambda e, pz=pz, j=j, cq_=cq_, which=which: e.matmul(
                              pz[0:64, :], wqh[:, j, 128 + which * 64:192 + which * 64], cq_[:, j, :], start=(j == 0), stop=(j == 3)),
                              R=[wqh, cq_], W=[pz])
                  k.op("dve", lambda e, cs_=cs_: e.tensor_tensor(out=t1[:, :], in0=psA[3][0:64, :], in1=cs_[:, 0, :], op=ALU.mult),
                       R=[psA[3], cs_], W=[t1])
                  k.op("dve", lambda e, cs_=cs_: e.tensor_tensor(out=t2[:, :], in0=psA[4][0:64, :], in1=cs_[:, 1, :], op=ALU.mult),
                       R=[psA[4], cs_], W=[t2])
                  k.op("pool", lambda e, qr=qr: e.tensor_tensor(out=qr[0:64, :], in0=t1[:, :], in1=t2[:, :], op=ALU.add), R=[t1, t2], W=[qr])
                  k.op("act", lambda e, qr=qr: e.activation(out=sqr[0:64, :], in_=qr[0:64, :], func=AF.Square), R=[qr], W=[sqr])
                  k.op("pe", lambda e: e.matmul(psA[2][:, :], ones_bf[:, :], sqn[:, :], start=True, stop=False), R=[ones_bf, sqn], W=[psA[2]])
                  k.op("pe", lambda e: e.matmul(psA[2][:, :], ones_bf[:, :], sqr[:, :], start=False, stop=True), R=[ones_bf, sqr], W=[psA[2]])
                  qm_, nb_ = qm[b2], nb[b2]
                  k.op("dve", lambda e, qm_=qm_: e.reduce_max(out=qm_[:, 0:1], in_=psA[2][:, :], axis=AX.X), R=[psA[2]], W=[qm_])
                  k.op("dve", lambda e, qm_=qm_: e.tensor_tensor(out=qm_[:, 0:1], in0=qm_[:, 0:1], in1=kmax2[:, 0:1], op=ALU.mult),
                       R=[qm_, kmax2], W=[qm_])
                  k.op("act", lambda e, qm_=qm_: e.activation(out=qm_[:, 0:1], in_=qm_[:, 0:1], func=AF.Sqrt), R=[qm_], W=[qm_])
                  k.op("dve", lambda e, qm_=qm_, nb_=nb_: e.tensor_scalar(out=nb_[:, 0:1], in0=qm_[:, 0:1], scalar1=-ATT_SCALE, scalar2=None,
                                                                         op0=ALU.mult), R=[qm_], W=[nb_])
                  chk(4)
                  Ops, Dps = psA[4], psA[5]
                  def emitS(kt):
                      sp_ = psA[kt % 2]
                      k.op("pe", lambda e, sp_=sp_, kt=kt, qn=qn: e.matmul(sp_[:, :], KnT[:, kt * 128:(kt + 1) * 128], qn[:, :], start=True, stop=False),
                           R=[KnT, qn], W=[sp_])
                      k.op("pe", lambda e, sp_=sp_, kt=kt, qr=qr: e.matmul(sp_[:, :], kr_sb[:, kt * 128:(kt + 1) * 128], qr[:, :], start=False, stop=True),
                           R=[kr_sb, qr], W=[sp_])
                  emitS(0)
                  if NKT > 1:
                      emitS(1)
                  for kt in range(NKT):
                      sp_ = psA[kt % 2]
                      pt_ = PT[kt % 4]
                      k.op("act", lambda e, sp_=sp_, pt_=pt_, nb_=nb_: e.activation(out=pt_[:, :], in_=sp_[:, :], func=AF.Exp,
                                                                                  bias=nb_[:, 0:1], scale=ATT_SCALE), R=[sp_, nb_], W=[pt_])
                      k.op("pe", lambda e, kt=kt, pt_=pt_: e.matmul(Ops[:, :], V[:, kt, :], pt_[:, :], start=(kt == 0), stop=(kt == NKT - 1)),
                           R=[V, pt_], W=[Ops])
                      k.op("pe", lambda e, kt=kt, pt_=pt_: e.matmul(Dps[:, :], ones_bf[:, :], pt_[:, :], start=(kt == 0), stop=(kt == NKT - 1)),
                           R=[ones_bf, pt_], W=[Dps])
                      if kt + 2 < NKT:
                          emitS(kt + 2)
                  k.op("dve", lambda e: e.reciprocal(out=rec[:, :], in_=Dps[:, :]), R=[Dps], W=[rec])
                  o_ = ot[b2]
                  k.op("dve", lambda e, o_=o_: e.tensor_tensor(out=o_[:, :], in0=Ops[:, :], in1=rec[:, :], op=ALU.mult), R=[Ops, rec], W=[o_])
                  k.dma("act", lambda e, o_=o_, h=h, t0=t0: e.dma_start(out=oT[h * 128:(h + 1) * 128, t0:t0 + 512], in_=o_[:, :]), R=[o_])
                  chk(5)
        except _Stop:
            pass
        k.barrier()
        pes.close()

    class RouteState:
        pass

    def mix_phase(l, i, w_dram, rs):
        pes = ExitStack()
        w_sb = k.sb(pes, "wo_sb", [128, KC, D], BF16)
        k.dma("sp", lambda e: e.dma_start(out=w_sb[:, :, :], in_=w_dram.rearrange("(kc p) n -> p kc n", p=128)), W=[w_sb])
        G1 = load_mod(pes, i, l, 2, "G1")
        A2 = load_mod(pes, i, l, 4, "A2")
        B2 = load_mod(pes, i, l, 3, "B2")
        wr_sb = k.sb(pes, "wr_sb", [128, KC, E], F32)
        rb_sb = k.sb(pes, "rb_sb", [128, E], F32)
        k.dma("sp", lambda e: e.dma_start(out=wr_sb[:, :, :], in_=wr_in[l].rearrange("(kc p) n -> p kc n", p=128)), W=[wr_sb])
        k.dma("sp", lambda e: e.dma_start(out=rb_sb[:, :], in_=rb_in[l]), W=[rb_sb])
        srcT = k.sb(pes, "srcT", [128, KC, 512], BF16)
        xt = [k.sb(pes, "mxt%d" % j, [128, D], F32) for j in range(2)]
        x1 = [k.sb(pes, "mx1%d" % j, [128, D], F32) for j in range(2)]
        h2f = k.sb(pes, "h2f", [128, D], F32)
        h2b = [k.sb(pes, "h2b%d" % j, [128, D], BF16) for j in range(2)]
        h2T = k.sb(pes, "h2T", [128, KC, 128], F32)
        ns = norm_tiles(pes, "n2")
        m8 = k.sb(pes, "m8", [128, 8], F32)
        msk = k.sb(pes, "msk", [128, E], F32)
        xi = 0
        for c in range(NCH):
            t0 = c * 512
            k.dma("sp", lambda e, t0=t0: e.dma_start(out=srcT[:, :, :], in_=oT.rearrange("(kc p) s -> p kc s", p=128)[:, :, t0:t0 + 512]),
                  W=[srcT])
            for tt in range(4):
                gt = t0 // 128 + tt
                r0 = i * S + gt * 128
                xb, x1b, hbb = xt[xi % 2], x1[xi % 2], h2b[xi % 2]
                xi += 1
                src = x_in[r0:r0 + 128, :] if l == 0 else xres[r0:r0 + 128, :]
                k.dma("sp", lambda e, xb=xb, src=src: e.dma_start(out=xb[:, :], in_=src), W=[xb])
                for n in range(4):
                    for kc in range(KC):
                        k.op("pe", lambda e, n=n, kc=kc, tt=tt: e.matmul(psA[n][:, :], srcT[:, kc, tt * 128:(tt + 1) * 128],
                                                                         w_sb[:, kc, n * 512:(n + 1) * 512], start=(kc == 0), stop=(kc == KC - 1)),
                             R=[srcT, w_sb], W=[psA[n]])
                    k.op("dve", lambda e, n=n, x1b=x1b: e.tensor_tensor(out=x1b[:, n * 512:(n + 1) * 512], in0=psA[n][:, :],
                                                                       in1=G1[:, n * 512:(n + 1) * 512], op=ALU.mult), R=[psA[n], G1], W=[x1b])
                k.op("pool", lambda e, x1b=x1b, xb=xb: e.tensor_tensor(out=x1b[:, :], in0=x1b[:, :], in1=xb[:, :], op=ALU.add),
                     R=[x1b, xb], W=[x1b])
                k.dma("act", lambda e, x1b=x1b, r0=r0: e.dma_start(out=xres[r0:r0 + 128, :], in_=x1b[:, :]), R=[x1b])
                norm_mod(ns, x1b, A2, B2, hbb, out_f32=h2f)
                k.dma("act", lambda e, hbb=hbb, r0=r0: e.dma_start(out=hrows[r0:r0 + 128, :], in_=hbb[:, :]), R=[hbb])
                for g in range(4):
                    pz = psA[4 + g % 2]
                    for q in range(4):
                        kc = g * 4 + q
                        k.op("pe", lambda e, pz=pz, q=q, kc=kc: e.transpose(out=pz[:, q * 128:(q + 1) * 128],
                                                                            in_=h2f[:, kc * 128:(kc + 1) * 128], identity=ident_f[:, :]),
                             R=[h2f, ident_f], W=[pz])
                    k.op("act", lambda e, pz=pz, g=g: e.copy(out=h2T[:, g * 4:(g + 1) * 4, :], in_=pz[:, :].rearrange("p (q t) -> p q t", q=4)),
                         R=[pz], W=[h2T])
                for kc in range(KC):
                    k.op("pe", lambda e, kc=kc: e.matmul(psA[4][:, 0:E], h2T[:, kc, :], wr_sb[:, kc, :], start=(kc == 0), stop=(kc == KC - 1)),
                         R=[h2T, wr_sb], W=[psA[4]])
                lg = rs.logits
                k.op("dve", lambda e, gt=gt: e.tensor_tensor(out=lg[:, gt, :], in0=psA[4][:, 0:E], in1=rb_sb[:, :], op=ALU.add),
                     R=[psA[4], rb_sb], W=[lg])
                k.op("dve", lambda e, gt=gt: e.max(out=m8[:, :], in_=lg[:, gt, :]), R=[lg], W=[m8])
                k.op("dve", lambda e, gt=gt: e.tensor_scalar(out=msk[:, :], in0=lg[:, gt, :], scalar1=m8[:, 3:4], scalar2=None, op0=ALU.is_ge),
                     R=[lg, m8], W=[msk])
                k.op("pe", lambda e: e.matmul(psA[5][:, 0:E], utri_f[:, :], msk[:, :], start=True, stop=True), R=[utri_f, msk], W=[psA[5]])
                k.op("dve", lambda e, gt=gt: e.tensor_tensor(out=rs.pos[:, gt, :], in0=psA[5][:, 0:E], in1=rs.base[:, :], op=ALU.add),
                     R=[psA[5], rs.base], W=[rs.pos])
                k.op("pe", lambda e: e.matmul(psA[5][:, 0:E], ones_f[:, :], msk[:, :], start=True, stop=True), R=[ones_f, msk], W=[psA[5]])
                k.op("dve", lambda e: e.tensor_tensor(out=rs.base[:, :], in0=psA[5][:, 0:E], in1=rs.base[:, :], op=ALU.add),
                     R=[psA[5], rs.base], W=[rs.base])
        k.barrier()
        pes.close()

    def moe_phase(l, i, rs):
        RB = i * S
        pes = ExitStack()
        padf = k.sb(pes, "padf", [128, E], F32)
        padi = k.sb(pes, "padi", [128, E], I32)
        pend = k.sb(pes, "pend", [128, E], F32)
        pstart = k.sb(pes, "pstart", [128, E], F32)
        blks = k.sb(pes, "blks", [128, NBLK], F32)
        blke = k.sb(pes, "blke", [128, NBLK], F32)
        tmpb = k.sb(pes, "tmpb", [128, NBLK], F32)
        k.dma("sp", lambda e: e.dma_start(out=blks[:, :], in_=blks_in), W=[blks])
        k.op("dve", lambda e: e.tensor_scalar(out=padf[:, :], in0=rs.base[:, :], scalar1=float(BLK - 1), scalar2=None, op0=ALU.add),
             R=[rs.base], W=[padf])
        k.op("dve", lambda e: e.tensor_copy(out=padi[:, :], in_=padf[:, :]), R=[padf], W=[padi])
        k.op("dve", lambda e: e.tensor_scalar(out=padi[:, :], in0=padi[:, :], scalar1=9, scalar2=None, op0=ALU.arith_shift_right),
             R=[padi], W=[padi])
        k.op("dve", lambda e: e.tensor_scalar(out=padi[:, :], in0=padi[:, :], scalar1=9, scalar2=None, op0=ALU.logical_shift_left),
             R=[padi], W=[padi])
        k.op("dve", lambda e: e.tensor_copy(out=padf[:, :], in_=padi[:, :]), R=[padi], W=[padf])
        k.op("dve", lambda e: e.tensor_tensor_scan(out=pend[:, :], data0=ones_f[:, 0:E], data1=padf[:, :], initial=0.0,
                                                   op0=ALU.mult, op1=ALU.add), R=[ones_f, padf], W=[pend])
        k.op("dve", lambda e: e.tensor_tensor(out=pstart[:, :], in0=pend[:, :], in1=padf[:, :], op=ALU.subtract), R=[pend, padf], W=[pstart])
        k.op("dve", lambda e: e.memset(blke[:, :], 0.0), W=[blke])
        for ee in range(E):
            k.op("dve", lambda e, ee=ee: e.scalar_tensor_tensor(out=blke[:, :], in0=blks[:, :], scalar=pend[:, ee:ee + 1], in1=blke[:, :],
                                                                 op0=ALU.is_ge, op1=ALU.add), R=[blks, pend, blke], W=[blke])
        k.op("dve", lambda e: e.tensor_scalar(out=blke[:, :], in0=blke[:, :], scalar1=float(E - 1), scalar2=None, op0=ALU.min), R=[blke], W=[blke])
        k.op("dve", lambda e: e.tensor_scalar(out=rs.idxb[:, :], in0=blke[:, :], scalar1=128.0, scalar2=pcol[:, 0:1], op0=ALU.mult, op1=ALU.add),
             R=[blke, pcol], W=[rs.idxb])
        k.op("dve", lambda e: e.tensor_copy(out=rs.idxd[:, :], in_=blke[:, :]), R=[blke], W=[rs.idxd])

        m8 = k.sb(pes, "dm8", [128, 8], F32)
        ex = k.sb(pes, "dex", [128, 4], F32)
        nm = k.sb(pes, "dnm", [128, 1], F32)
        es_ = k.sb(pes, "des", [128, 1], F32)
        oh = k.sb(pes, "doh", [128, E], F32)
        dsum = k.sb(pes, "dsum", [128, E], F32)
        junk = k.sb(pes, "djunk", [128, E], F32)
        dstf = k.sb(pes, "dstf", [128, 4], F32)
        hr = [k.sb(pes, "hr%d" % j, [128, D], BF16) for j in range(3)]
        lg = rs.logits
        for gt in range(NTB):
            hb = hr[gt % 3]
            k.dma("sp", lambda e, hb=hb, gt=gt: e.dma_start(out=hb[:, :], in_=hrows[RB + gt * 128:RB + (gt + 1) * 128, :]), W=[hb])
            k.op("dve", lambda e, gt=gt: e.max(out=m8[:, :], in_=lg[:, gt, :]), R=[lg], W=[m8])
            k.op("dve", lambda e: e.tensor_scalar(out=nm[:, :], in0=m8[:, 0:1], scalar1=-1.0, scalar2=None, op0=ALU.mult), R=[m8], W=[nm])
            k.op("act", lambda e: e.activation(out=ex[:, :], in_=m8[:, 0:4], func=AF.Exp, bias=nm[:, 0:1], scale=1.0, accum_out=es_[:, 0:1]),
                 R=[m8, nm], W=[ex, es_])
            k.op("dve", lambda e: e.reciprocal(out=es_[:, :], in_=es_[:, :]), R=[es_], W=[es_])
            k.op("dve", lambda e, gt=gt: e.tensor_scalar(out=rs.G[:, gt, :], in0=ex[:, :], scalar1=es_[:, 0:1], scalar2=None, op0=ALU.mult),
                 R=[ex, es_], W=[rs.G])
            k.op("dve", lambda e, gt=gt: e.tensor_tensor(out=dsum[:, :], in0=rs.pos[:, gt, :], in1=pstart[:, :], op=ALU.add),
                 R=[rs.pos, pstart], W=[dsum])
            for kk in range(TOPK):
                k.op("dve", lambda e, gt=gt, kk=kk: e.tensor_scalar(out=oh[:, :], in0=lg[:, gt, :], scalar1=m8[:, kk:kk + 1], scalar2=None,
                                                                    op0=ALU.is_equal), R=[lg, m8], W=[oh])
                k.op("dve", lambda e, kk=kk: e.scalar_tensor_tensor(out=junk[:, :], in0=oh[:, :], scalar=1.0, in1=dsum[:, :],
                                                                    op0=ALU.mult, op1=ALU.mult, accum_out=dstf[:, kk:kk + 1]),
                     R=[oh, dsum], W=[junk, dstf])
            k.op("dve", lambda e, gt=gt: e.tensor_copy(out=rs.dest[:, gt, :], in_=dstf[:, :]), R=[dstf], W=[rs.dest])
            for kk in range(TOPK):
                k.dma("pool", lambda e, hb=hb, gt=gt, kk=kk: e.indirect_dma_start(
                    out=xs[:, :], out_offset=bass.IndirectOffsetOnAxis(ap=rs.dest[:, gt, kk:kk + 1], axis=0),
                    in_=hb[:, :], in_offset=None), R=[hb, rs.dest])
        k.barrier()
        pes.close()

        pes = ExitStack()
        xsb = [k.sb(pes, "xsb%d" % j, [128, D], BF16) for j in range(2)]
        xT = k.sb(pes, "xT", [128, KC, 512], BF16)
        wgu = [k.sb(pes, "wgu%d" % j, [128, KC, 256], BF16) for j in range(2)]
        wdn = [k.sb(pes, "wdn%d" % j, [128, D], BF16) for j in range(NFC)]
        bgu = k.sb(pes, "bgu", [128, 2 * NFC], F32)
        bl1 = k.sb(pes, "bl1", [128, NFC], F32)
        bdn = k.sb(pes, "bdn", [128, D], F32)
        actT = k.sb(pes, "actT", [128, NFC, 512], BF16)
        g1s = [k.sb(pes, "g1%d" % j, [128, 512], F32) for j in range(2)]
        sgs = [k.sb(pes, "sg%d" % j, [128, 512], F32) for j in range(2)]
        l2s = [k.sb(pes, "l2%d" % j, [128, 512], F32) for j in range(2)]
        gss = [k.sb(pes, "gs%d" % j, [128, 512], F32) for j in range(2)]
        yo = [k.sb(pes, "yo%d" % j, [128, D], F32) for j in range(2)]
        xi = 0
        yi = 0
        wi = 0
        for blk in range(NBLK):
            k.dma("pool", lambda e, blk=blk: e.indirect_dma_start(
                out=bgu[:, :], out_offset=None, in_=bgu_in[l][:, :],
                in_offset=bass.IndirectOffsetOnAxis(ap=rs.idxb[:, blk:blk + 1], axis=0)), R=[rs.idxb], W=[bgu])
            k.dma("pool", lambda e, blk=blk: e.indirect_dma_start(
                out=bdn[:, :], out_offset=None, in_=bdn_in[l][:, :],
                in_offset=bass.IndirectOffsetOnAxis(ap=rs.idxd[:, blk:blk + 1], axis=0)), R=[rs.idxd], W=[bdn])
            k.op("dve", lambda e: e.tensor_scalar(out=bl1[:, :], in0=bgu[:, NFC:2 * NFC], scalar1=1.0, scalar2=None, op0=ALU.add), R=[bgu], W=[bl1])
            for st in range(4):
                xb = xsb[xi % 2]
                xi += 1
                r0 = blk * BLK + st * 128
                k.dma("sp", lambda e, xb=xb, r0=r0: e.dma_start(out=xb[:, :], in_=xs[r0:r0 + 128, :]), W=[xb])
                transpose_bf(xb, xT, st * 128)
            for fc in range(NFC):
                wg = wgu[wi % 2]
                wi += 1
                k.dma("pool", lambda e, wg=wg, blk=blk, fc=fc: e.indirect_dma_start(
                    out=wg[:, :, :].rearrange("p a b -> p (a b)"), out_offset=None, in_=wgu_b[l][fc][:, :],
                    in_offset=bass.IndirectOffsetOnAxis(ap=rs.idxb[:, blk:blk + 1], axis=0)), R=[rs.idxb], W=[wg])
                k.dma("pool", lambda e, blk=blk, fc=fc: e.indirect_dma_start(
                    out=wdn[fc][:, :], out_offset=None, in_=wdn_b[l][fc][:, :],
                    in_offset=bass.IndirectOffsetOnAxis(ap=rs.idxb[:, blk:blk + 1], axis=0)), R=[rs.idxb], W=[wdn[fc]])
                pg, pl = psA[2 * (fc % 2)], psA[2 * (fc % 2) + 1]
                g1, sg, l2, gs = g1s[fc % 2], sgs[fc % 2], l2s[fc % 2], gss[fc % 2]
                for half in range(2):
                    pz = pg if half == 0 else pl
                    for kc in range(KC):
                        k.op("pe", lambda e, pz=pz, wg=wg, kc=kc, half=half: e.matmul(
                            pz[:, :], wg[:, kc, half * 128:(half + 1) * 128], xT[:, kc, :], start=(kc == 0), stop=(kc == KC - 1)),
                            R=[wg, xT], W=[pz])
                k.op("dve", lambda e, fc=fc, g1=g1, pg=pg: e.tensor_scalar(out=g1[:, :], in0=pg[:, :], scalar1=bgu[:, fc:fc + 1], scalar2=LIMIT,
                                                             op0=ALU.add, op1=ALU.min), R=[pg, bgu], W=[g1])
                k.op("act", lambda e, g1=g1, sg=sg: e.activation(out=sg[:, :], in_=g1[:, :], func=AF.Sigmoid, scale=ALPHA), R=[g1], W=[sg])
                k.op("dve", lambda e, fc=fc, l2=l2, pl=pl: e.tensor_scalar(out=l2[:, :], in0=pl[:, :], scalar1=bl1[:, fc:fc + 1], scalar2=1.0 - LIMIT,
                                                             op0=ALU.add, op1=ALU.max), R=[pl, bl1], W=[l2])
                k.op("pool", lambda e, g1=g1, sg=sg, gs=gs: e.tensor_tensor(out=gs[:, :], in0=g1[:, :], in1=sg[:, :], op=ALU.mult), R=[g1, sg], W=[gs])
                k.op("dve", lambda e, fc=fc, l2=l2, gs=gs: e.scalar_tensor_tensor(out=actT[:, fc, :], in0=l2[:, :], scalar=1.0 + LIMIT, in1=gs[:, :],
                                                                     op0=ALU.min, op1=ALU.mult), R=[l2, gs], W=[actT])
            dn = 0
            for st in range(4):
                y_ = yo[yi % 2]
                yi += 1
                for n in range(4):
                    pz = psA[4 + dn % 2]
                    dn += 1
                    for fc in range(NFC):
                        k.op("pe", lambda e, pz=pz, fc=fc, st=st, n=n: e.matmul(
                            pz[:, :], actT[:, fc, st * 128:(st + 1) * 128], wdn[fc][:, n * 512:(n + 1) * 512],
                            start=(fc == 0), stop=(fc == NFC - 1)), R=[actT, wdn[fc]], W=[pz])
                    k.op("dve", lambda e, pz=pz, y_=y_, n=n: e.tensor_tensor(out=y_[:, n * 512:(n + 1) * 512], in0=pz[:, :],
                                                                            in1=bdn[:, n * 512:(n + 1) * 512], op=ALU.add), R=[pz, bdn], W=[y_])
                r0 = blk * BLK + st * 128
                for hf in range(2):
                    k.dma("act", lambda e, y_=y_, r0=r0, hf=hf: e.dma_start(out=ys[hf][r0:r0 + 128, :], in_=y_[:, hf * 1024:(hf + 1) * 1024]), R=[y_])
        k.barrier()
        pes.close()

        pes = ExitStack()
        rows = [k.sb(pes, "cr%d" % j, [128, D], F32) for j in range(4)]
        G2 = load_mod(pes, i, l, 5, "G2")
        x1 = [k.sb(pes, "cx%d" % j, [128, D], F32) for j in range(2)]
        acc = k.sb(pes, "cacc", [128, D], F32)
        xo = [k.sb(pes, "cxo%d" % j, [128, D], F32) for j in range(2)]
        gfin = k.sb(pes, "gfin", [128, D], F32)
        ns = norm_tiles(pes, "nf") if l == 1 else None
        if l == 1:
            k.dma("sp", lambda e: e.dma_start(out=gfin[:, :], in_=gfin_in), W=[gfin])
        for gt in range(NTB):
            xb = x1[gt % 2]
            k.dma("sp", lambda e, xb=xb, gt=gt: e.dma_start(out=xb[:, :], in_=xres[RB + gt * 128:RB + (gt + 1) * 128, :]), W=[xb])
            for kk in range(TOPK):
                for hf in range(2):
                    k.dma("pool", lambda e, gt=gt, kk=kk, hf=hf: e.indirect_dma_start(
                        out=rows[kk][:, hf * 1024:(hf + 1) * 1024], out_offset=None, in_=ys[hf][:, :],
                        in_offset=bass.IndirectOffsetOnAxis(ap=rs.dest[:, gt, kk:kk + 1], axis=0)), R=[rs.dest], W=[rows[kk]])
            k.op("dve", lambda e, gt=gt: e.tensor_scalar(out=acc[:, :], in0=rows[0][:, :], scalar1=rs.G[:, gt, 0:1], scalar2=None, op0=ALU.mult),
                 R=[rows[0], rs.G], W=[acc])
            for kk in range(1, TOPK):
                k.op("dve", lambda e, gt=gt, kk=kk: e.scalar_tensor_tensor(out=acc[:, :], in0=rows[kk][:, :], scalar=rs.G[:, gt, kk:kk + 1],
                                                                           in1=acc[:, :], op0=ALU.mult, op1=ALU.add), R=[rows[kk], rs.G, acc], W=[acc])
            xo_ = xo[gt % 2]
            k.op("pool", lambda e: e.tensor_tensor(out=acc[:, :], in0=acc[:, :], in1=G2[:, :], op=ALU.mult), R=[acc, G2], W=[acc])
            k.op("pool", lambda e, xo_=xo_, xb=xb: e.tensor_tensor(out=xo_[:, :], in0=acc[:, :], in1=xb[:, :], op=ALU.add), R=[acc, xb], W=[xo_])
            if l == 0:
                k.dma("act", lambda e, xo_=xo_, gt=gt: e.dma_start(out=xres[RB + gt * 128:RB + (gt + 1) * 128, :], in_=xo_[:, :]), R=[xo_])
            else:
                norm_mod(ns, xo_, gfin, None, None)
                k.dma("act", lambda e, gt=gt: e.dma_start(out=out_d[RB + gt * 128:RB + (gt + 1) * 128, :], in_=ns.tmp[:, :]), R=[ns.tmp])
        k.barrier()
        pes.close()

    def conv_phase(i):
        pes = ExitStack()
        A1 = load_mod(pes, i, 1, 1, "cA1")
        B1 = load_mod(pes, i, 1, 0, "cB1")
        ns = norm_tiles(pes, "nc")
        xt = [k.sb(pes, "cxt%d" % j, [128, D], F32) for j in range(2)]
        hb = [k.sb(pes, "chb%d" % j, [128, D], BF16) for j in range(2)]
        hT = k.sb(pes, "chT", [128, KC, 512], BF16)
        wsl = [k.sb(pes, "cws%d" % j, [128, KC, 128], BF16) for j in range(3)]
        tmpf = k.sb(pes, "ctmp", [128, 512], F32)
        zo = k.sb(pes, "zo", [128, KC, 512], BF16)
        go = k.sb(pes, "go", [128, KC, 512], BF16)
        zpad = k.sb(pes, "zpad", [128, KC, 1], BF16)
        k.op("dve", lambda e: e.memset(zpad[:, :, :], 0.0), W=[zpad])
        zv = zT.rearrange("(kc p) s -> p kc s", p=128)
        gv = gbT.rearrange("(kc p) s -> p kc s", p=128)
        with nc.allow_non_contiguous_dma(reason="2-byte pad columns"):
            k.dma("sp", lambda e: e.dma_start(out=zv[:, :, 0:1], in_=zpad[:, :, :]), R=[zpad])
            k.dma("sp", lambda e: e.dma_start(out=zv[:, :, S + 1:S + 2], in_=zpad[:, :, :]), R=[zpad])
        xi = 0
        wi = 0
        for c in range(NCH):
            t0 = c * 512
            for tt in range(4):
                xb, hbb = xt[xi % 2], hb[xi % 2]
                xi += 1
                r0 = i * S + t0 + tt * 128
                k.dma("sp", lambda e, xb=xb, r0=r0: e.dma_start(out=xb[:, :], in_=xres[r0:r0 + 128, :]), W=[xb])
                norm_mod(ns, xb, A1, B1, hbb)
                transpose_bf(hbb, hT, tt * 128)

            def proj(m, pz):
                nonlocal wi
                ws = wsl[wi % 3]
                wi += 1
                k.dma("sp", lambda e, ws=ws, m=m: e.dma_start(out=ws[:, :, :].rearrange("p a b -> p (a b)"), in_=cwin_b[m * 128:(m + 1) * 128, :]),
                      W=[ws])
                for kc in range(KC):
                    k.op("pe", lambda e, ws=ws, kc=kc, pz=pz: e.matmul(pz[:, :], ws[:, kc, :], hT[:, kc, :], start=(kc == 0), stop=(kc == KC - 1)),
                         R=[ws, hT], W=[pz])
            for jc in range(KC):
                proj(16 + jc, psA[0])
                proj(32 + jc, psA[1])
                proj(jc, psA[2])
                k.op("act", lambda e: e.copy(out=tmpf[:, :], in_=psA[0][:, :]), R=[psA[0]], W=[tmpf])
                k.op("dve", lambda e, jc=jc: e.tensor_tensor(out=zo[:, jc, :], in0=psA[1][:, :], in1=tmpf[:, :], op=ALU.mult),
                     R=[psA[1], tmpf], W=[zo])
                k.op("act", lambda e, jc=jc: e.copy(out=go[:, jc, :], in_=psA[2][:, :]), R=[psA[2]], W=[go])
            k.dma("act", lambda e, t0=t0: e.dma_start(out=zv[:, :, 1 + t0:1 + t0 + 512], in_=zo[:, :, :]), R=[zo])
            k.dma("act", lambda e, t0=t0: e.dma_start(out=gv[:, :, t0:t0 + 512], in_=go[:, :, :]), R=[go])
        k.barrier()
        pes.close()
        pes = ExitStack()
        cw = k.sb(pes, "cw", [128, KC, 3], F32)
        k.dma("sp", lambda e: e.dma_start(out=cw[:, :, :].rearrange("p a b -> p (a b)"), in_=cw_in), W=[cw])
        zi = [k.sb(pes, "zi%d" % j, [128, KC, 514], BF16) for j in range(2)]
        gi = [k.sb(pes, "gi%d" % j, [128, KC, 512], BF16) for j in range(2)]
        ca = k.sb(pes, "ca", [128, 512], F32)
        cb = k.sb(pes, "cb", [128, 512], F32)
        vo = [k.sb(pes, "vo%d" % j, [128, KC, 512], BF16) for j in range(2)]
        ov = oT.rearrange("(kc p) s -> p kc s", p=128)
        for c in range(NCH):
            t0 = c * 512
            z_, g_, v_ = zi[c % 2], gi[c % 2], vo[c % 2]
            k.dma("sp", lambda e, z_=z_, t0=t0: e.dma_start(out=z_[:, :, :], in_=zv[:, :, t0:t0 + 514]), W=[z_])
            k.dma("sp", lambda e, g_=g_, t0=t0: e.dma_start(out=g_[:, :, :], in_=gv[:, :, t0:t0 + 512]), W=[g_])
            for jc in range(KC):
                k.op("act", lambda e, z_=z_, jc=jc: e.activation(out=ca[:, :], in_=z_[:, jc, 0:512], func=AF.Copy, scale=cw[:, jc, 0:1]),
                     R=[z_, cw], W=[ca])
                k.op("dve", lambda e, z_=z_, jc=jc: e.scalar_tensor_tensor(out=cb[:, :], in0=z_[:, jc, 1:513], scalar=cw[:, jc, 1:2], in1=ca[:, :],
                                                                           op0=ALU.mult, op1=ALU.add), R=[z_, cw, ca], W=[cb])
                k.op("dve", lambda e, z_=z_, jc=jc: e.scalar_tensor_tensor(out=ca[:, :], in0=z_[:, jc, 2:514], scalar=cw[:, jc, 2:3], in1=cb[:, :],
                                                                           op0=ALU.mult, op1=ALU.add), R=[z_, cw, cb], W=[ca])
                k.op("pool", lambda e, g_=g_, v_=v_, jc=jc: e.tensor_tensor(out=v_[:, jc, :], in0=ca[:, :], in1=g_[:, jc, :], op=ALU.mult),
                     R=[ca, g_], W=[v_])
            k.dma("act", lambda e, v_=v_, t0=t0: e.dma_start(out=ov[:, :, t0:t0 + 512], in_=v_[:, :, :]), R=[v_])
        k.barrier()
        pes.close()

    STOP = cfg.get("STOP", 10 ** 9)
    step = [0]

    def go():
        step[0] += 1
        return step[0] <= STOP

    if go():
        cast_phase()
    if go():
        ada_phase()
    for l in range(2):
        for i in range(NBC):
            ges = ExitStack()
            rs = RouteState()
            rs.logits = k.sb(ges, "logits", [128, NTB, E], F32)
            rs.pos = k.sb(ges, "pos", [128, NTB, E], F32)
            rs.base = k.sb(ges, "base", [128, E], F32)
            rs.G = k.sb(ges, "G", [128, NTB, 4], F32)
            rs.dest = k.sb(ges, "dest", [128, NTB, 4], I32)
            rs.idxb = k.sb(ges, "idxb", [128, NBLK], I32)
            rs.idxd = k.sb(ges, "idxd", [128, NBLK], I32)
            k.op("dve", lambda e: e.memset(rs.base[:, :], 0.0), W=[rs.base])
            if l == 0:
                if go():
                    mla_latent_phase(i)
                if go():
                    attention_phase(i)
                if go():
                    mix_phase(0, i, wout_b, rs)
            else:
                if go():
                    conv_phase(i)
                if go():
                    mix_phase(1, i, cwout_b, rs)
            if go():
                moe_phase(l, i, rs)
            k.barrier()
            ges.close()
    k.barrier()
    es.close()
    return nc


def rope_tables(S):
    rows = S // GRID_W
    row = np.repeat(np.arange(rows), GRID_W).astype(np.float32)
    col = np.tile(np.arange(GRID_W), rows).astype(np.float32)
    half = ROPE // 2
    inv = (1.0 / (np.float32(THETA) ** (np.arange(0, half, 2, dtype=np.float32) / np.float32(half)))).astype(np.float32)
    ang_r = row[:, None] * inv[None, :]
    ang_c = col[:, None] * inv[None, :]
    cr, sr, cc, sc = np.cos(ang_r), np.sin(ang_r), np.cos(ang_c), np.sin(ang_c)
    cosT = np.concatenate([cr, cr, cc, cc], axis=1).T.astype(np.float32)
    sinT = np.concatenate([-sr, sr, -sc, sc], axis=1).T.astype(np.float32)
    return np.ascontiguousarray(cosT), np.ascontiguousarray(sinT)


ROPE_PERM = np.concatenate([np.arange(16, 32), np.arange(0, 16), np.arange(48, 64), np.arange(32, 48)])


def rep128(v):
    v = np.asarray(v, np.float32).reshape(1, -1)
    return np.ascontiguousarray(np.broadcast_to(v, (128, v.shape[1])))


def prep_shared(inp, cfg):
    S, NBC, E, F = cfg["S"], cfg["NBC"], cfg["E"], cfg["F"]
    NFC = F // 128
    A = S * TOPK
    NBLK = -(-(A + E * (BLK - 1)) // BLK)
    m = {}
    for l in range(2):
        m["adaw%d" % l] = np.ascontiguousarray(inp["ada_w"][l])
        m["adab%d" % l] = rep128(inp["ada_b"][l])
        m["gmix%d" % l] = rep128(inp["norm_mix_g"][l])
        m["gffn%d" % l] = rep128(inp["norm_ffn_g"][l])
        m["wr%d" % l] = np.ascontiguousarray(inp["router_w"][l])
        m["rb%d" % l] = rep128(inp["router_b"][l])
        wgu = inp["expert_w_gu"][l]
        wgu = wgu.reshape(E, KC, 128, 2, NFC, 128).transpose(4, 0, 2, 1, 3, 5)
        m["wgu%d" % l] = np.ascontiguousarray(wgu).reshape(NFC * E * 128, KC * 256)
        wdn = inp["expert_w_down"][l].reshape(E, NFC, 128, D).transpose(1, 0, 2, 3)
        m["wdn%d" % l] = np.ascontiguousarray(wdn).reshape(NFC * E * 128, D)
        bgu = inp["expert_b_gu"][l].reshape(E, 2, NFC, 128).transpose(0, 3, 1, 2)
        m["bgu%d" % l] = np.ascontiguousarray(bgu).reshape(E * 128, 2 * NFC)
        m["bdn%d" % l] = np.ascontiguousarray(inp["expert_b_down"][l])
    m["gfin"] = rep128(inp["final_norm_g"])
    w_in = inp["mla_w_in"][0]
    m["mla_win"] = np.ascontiguousarray(np.concatenate([w_in, w_in[:, 1024 + ROPE_PERM]], axis=1))
    m["gq"] = np.ascontiguousarray(inp["mla_q_norm_g"][0].reshape(4, 128).T)
    m["gkv"] = np.ascontiguousarray(inp["mla_kv_norm_g"][0].reshape(4, 128).T)
    wq = inp["mla_w_q_up"][0].reshape(QL, NH, 192)
    m["mla_wq"] = np.ascontiguousarray(np.concatenate([wq, wq[:, :, 128 + ROPE_PERM]], axis=2)).reshape(QL, NH * 256)
    m["mla_wkv"] = np.ascontiguousarray(inp["mla_w_kv_up"][0])
    m["mla_wout"] = np.ascontiguousarray(inp["mla_w_out"][0])
    cwin = inp["conv_w_in"][0].reshape(KC, 128, 48, 128).transpose(2, 1, 0, 3)
    m["conv_win"] = np.ascontiguousarray(cwin).reshape(48 * 128, KC * 128)
    m["conv_w"] = np.ascontiguousarray(inp["conv_w"][0].reshape(3, KC, 128).transpose(2, 1, 0)).reshape(128, KC * 3)
    m["conv_wout"] = np.ascontiguousarray(inp["conv_w_out"][0])
    m["ident"] = np.eye(128, dtype=np.float32)
    m["utri"] = np.triu(np.ones((128, 128), np.float32), 1)
    cosT, sinT = rope_tables(S)
    m["ropec"], m["ropes"] = cosT, sinT
    m["blkstart"] = rep128(np.arange(NBLK, dtype=np.float32) * BLK)
    m["pcol"] = np.arange(128, dtype=np.float32).reshape(128, 1)
    return m


def run(inp, cfg):
    S, NBC, NCORES = cfg["S"], cfg["NBC"], cfg["NCORES"]
    shared = prep_shared(inp, cfg)
    in_maps = []
    for c in range(NCORES):
        b0 = c * NBC
        m = dict(shared)
        m["x"] = np.ascontiguousarray(inp["x"][b0:b0 + NBC]).reshape(NBC * S, D)
        m["ctx"] = np.ascontiguousarray(inp["ctx"][b0:b0 + NBC]).reshape(NBC * CTX, D)
        cc = np.concatenate([inp["c"][b0:b0 + NBC], inp["c_ctx"][None, :]], axis=0)
        m["cT"] = np.ascontiguousarray(cc.reshape(NBC + 1, KC, 128).transpose(2, 0, 1)).reshape(128, (NBC + 1) * KC)
        in_maps.append(m)
    nc = build_nc(cfg)
    res = run_bass_kernel_spmd(nc, in_maps, core_ids=list(range(NCORES)))
    outs = [np.asarray(res.results[c]["out"]).reshape(NBC, S, D) for c in range(NCORES)]
    return np.concatenate(outs, axis=0).astype(np.float32)


def kernel(**inputs):
    inp = {k_: np.asarray(v) for k_, v in inputs.items()}
    return run(inp, FULL_CFG)
```

```python
import numpy as np
import ml_dtypes
from contextlib import ExitStack
import concourse.bass as bass
import concourse.mybir as mybir
from concourse.bass_utils import run_bass_kernel_spmd

F32 = mybir.dt.float32
BF16 = mybir.dt.bfloat16
I32 = mybir.dt.int32
AF = mybir.ActivationFunctionType
ALU = mybir.AluOpType
AX = mybir.AxisListType

D = 2048
KC = D // 128
NH = 16
QL = 512
KVL = 512
ROPE = 64
CTX = 256
TOPK = 4
BLK = 512
EPS = 1e-6
ATT_SCALE = (128 + 64) ** -0.5
LIMIT = 7.0
ALPHA = 1.702
GRID_W = 64
THETA = 10000.0

FULL_CFG = dict(S=8192, NBC=1, E=32, F=2048, NCORES=4, BATCH=4)


class Buf:
    __slots__ = ("w", "r", "name", "ex")

    def __init__(self, name=""):
        self.w = None
        self.r = {}
        self.name = name
        self.ex = False


class Tile:
    def __init__(self, t, name):
        self.t = t
        self.b = Buf(name)

    def __getitem__(self, k):
        return self.t[k]


class K:
    def __init__(self, nc, es):
        self.nc = nc
        self.es = es
        self.eng = {"pe": nc.tensor, "act": nc.scalar, "dve": nc.vector, "pool": nc.gpsimd, "sp": nc.sync}
        self.sem = {}
        self.cnt = {}
        for n in ("pe", "act", "dve", "pool"):
            self.sem[n] = es.enter_context(nc.semaphore("c_" + n))
            self.cnt[n] = 0
        self.waited = {n: {} for n in self.eng}
        self.dsem = {}
        for q, k in (("sp", 20), ("pool", 20), ("act", 8)):
            self.dsem[q] = [[es.enter_context(nc.semaphore("d_%s%d" % (q, i))), 0] for i in range(k)]
        self.drr = {q: 0 for q in self.dsem}
        self.semkey = {}

    def _key(self, sem):
        return id(sem)

    def wait(self, en, tok):
        sem, val, prod = tok
        if en == "pe" and prod == "pe":
            return
        k = self._key(sem)
        if self.waited[en].get(k, 0) >= val:
            return
        self.eng[en].wait_ge(sem, val)
        self.waited[en][k] = val

    def _deps(self, en, R, W):
        for b in R:
            if b.w is not None:
                self.wait(en, b.w)
            if b.ex:
                for key, tok in b.r.items():
                    if key != en:
                        self.wait(en, tok)
        for b in W:
            if b.w is not None:
                self.wait(en, b.w)
            for tok in b.r.values():
                self.wait(en, tok)

    def _post(self, tok, R, W, rkey):
        for b in R:
            b.r[rkey] = tok
        for b in W:
            b.w = tok
            b.r = {}

    def op(self, en, fn, R=(), W=()):
        R = [x.b if isinstance(x, Tile) else x for x in R]
        W = [x.b if isinstance(x, Tile) else x for x in W]
        self._deps(en, R, W)
        ins = fn(self.eng[en])
        self.cnt[en] += 1
        ins.then_inc(self.sem[en], 1)
        tok = (self.sem[en], self.cnt[en], en)
        self._post(tok, R, W, en)
        return tok

    def dma(self, q, fn, R=(), W=()):
        R = [x.b if isinstance(x, Tile) else x for x in R]
        W = [x.b if isinstance(x, Tile) else x for x in W]
        self._deps(q, R, W)
        i = self.drr[q]
        self.drr[q] = (i + 1) % len(self.dsem[q])
        slot = self.dsem[q][i]
        if slot[1] > 0:
            self.wait(q, (slot[0], slot[1], "dma"))
        ins = fn(self.eng[q])
        slot[1] += 16
        ins.then_inc(slot[0], 16)
        tok = (slot[0], slot[1], "dma")
        self._post(tok, R, W, ("d", q, i))
        return tok

    def barrier(self):
        toks = [(self.sem[n], self.cnt[n], n) for n in self.sem if self.cnt[n] > 0]
        for q in self.dsem:
            for s, v in self.dsem[q]:
                if v > 0:
                    toks.append((s, v, "dma"))
        for en in self.eng:
            for tok in toks:
                if en == "pe" and tok[2] == "pe":
                    continue
                self.wait(en, tok)

    def sb(self, es, name, shape, dt):
        self.uid = getattr(self, "uid", 0) + 1
        name = "s%d_%s" % (self.uid, name)
        return Tile(es.enter_context(self.nc.sbuf_tensor(name, list(shape), dt)), name)

    def ps(self, es, name, shape, dt):
        self.uid = getattr(self, "uid", 0) + 1
        name = "p%d_%s" % (self.uid, name)
        t = Tile(es.enter_context(self.nc.psum_tensor(name, list(shape), dt)), name)
        t.b.ex = True
        return t


def build_nc(cfg):
    S, NBC, E, F = cfg["S"], cfg["NBC"], cfg["E"], cfg["F"]
    NFC = F // 128
    T = NBC * S
    NT = T // 128
    NK = S + CTX
    NKT = NK // 128
    A = S * TOPK
    NBLK = -(-(A + E * (BLK - 1)) // BLK)
    NSLOT = NBLK * BLK
    NTB = S // 128
    NCH = S // 512

    nc = bass.Bass("TRN2", target_bir_lowering=False)
    es = ExitStack()
    k = K(nc, es)

    def din(name, shape, dt=F32):
        return nc.dram_tensor(name, list(shape), dt, kind="ExternalInput").ap()

    def dscr(name, shape, dt):
        return nc.dram_tensor(name, list(shape), dt, kind="Internal").ap()

    x_in = din("x", [T, D])
    ctx_in = din("ctx", [NBC * CTX, D])
    cT_in = din("cT", [128, (NBC + 1) * KC])
    out_d = nc.dram_tensor("out", [T, D], F32, kind="ExternalOutput").ap()
    adaw_in = [din("adaw%d" % l, [D, 6 * D]) for l in range(2)]
    adab_in = [din("adab%d" % l, [128, 6 * D]) for l in range(2)]
    gmix_in = [din("gmix%d" % l, [128, D]) for l in range(2)]
    gffn_in = [din("gffn%d" % l, [128, D]) for l in range(2)]
    gfin_in = din("gfin", [128, D])
    win_in = din("mla_win", [D, 1152])
    gq_in = din("gq", [128, 4])
    gkv_in = din("gkv", [128, 4])
    wq_in = din("mla_wq", [QL, NH * 256])
    wkv_in = din("mla_wkv", [KVL, NH * 256])
    wout_in = din("mla_wout", [D, D])
    cwin_in = din("conv_win", [48 * 128, KC * 128])
    cw_in = din("conv_w", [128, KC * 3])
    cwout_in = din("conv_wout", [D, D])
    wr_in = [din("wr%d" % l, [D, E]) for l in range(2)]
    rb_in = [din("rb%d" % l, [128, E]) for l in range(2)]
    wgu_in = [din("wgu%d" % l, [NFC * E * 128, KC * 256]) for l in range(2)]
    wdn_in = [din("wdn%d" % l, [NFC * E * 128, D]) for l in range(2)]
    bgu_in = [din("bgu%d" % l, [E * 128, 2 * NFC]) for l in range(2)]
    bdn_in = [din("bdn%d" % l, [E, D]) for l in range(2)]
    ident_in = din("ident", [128, 128])
    utri_in = din("utri", [128, 128])
    ropec_in = din("ropec", [64, S])
    ropes_in = din("ropes", [64, S])
    blks_in = din("blkstart", [128, NBLK])
    pcol_in = din("pcol", [128, 1])

    adaw_b = [dscr("adaw_b%d" % l, [D, 6 * D], BF16) for l in range(2)]
    win_b = dscr("win_b", [D, 1152], BF16)
    wq_b = dscr("wq_b", [QL, NH * 256], BF16)
    wkv_b = dscr("wkv_b", [KVL, NH * 256], BF16)
    wout_b = dscr("wout_b", [D, D], BF16)
    cwin_b = dscr("cwin_b", [48 * 128, KC * 128], BF16)
    cwout_b = dscr("cwout_b", [D, D], BF16)
    wgu_b = [[dscr("wgu_b%d_%d" % (l, fc), [E * 128, KC * 256], BF16) for fc in range(NFC)] for l in range(2)]
    wdn_b = [[dscr("wdn_b%d_%d" % (l, fc), [E * 128, D], BF16) for fc in range(NFC)] for l in range(2)]
    modv = dscr("modv", [(NBC + 1) * 2 * 6 * 128, D], F32)
    xres = dscr("xres", [T, D], F32)
    hrows = dscr("hrows", [T, D], BF16)
    cqnT = dscr("cqnT", [4 * 128, S], BF16)
    kvT = dscr("kvT", [4 * 128, NK], BF16)
    krT = dscr("krT", [64, NK], BF16)
    oT = dscr("oT", [D, S], BF16)
    zT = dscr("zT", [D, S + 2], BF16)
    gbT = dscr("gbT", [D, S], BF16)
    xs = dscr("xs", [NSLOT, D], BF16)
    ys = [dscr("ys%d" % j, [NSLOT, D // 2], F32) for j in range(2)]

    def modv_ap(i, l, which):
        r0 = ((i * 2 + l) * 6 + which) * 128
        return modv[r0:r0 + 128, :]

    ident_f = k.sb(es, "ident_f", [128, 128], F32)
    ident_bf = k.sb(es, "ident_bf", [128, 128], BF16)
    ones_bf = k.sb(es, "ones_bf", [128, 128], BF16)
    ones_f = k.sb(es, "ones_f", [128, 128], F32)
    utri_bf = k.sb(es, "utri_bf", [128, 128], BF16)
    utri_f = k.sb(es, "utri_f", [128, 128], F32)
    pcol = k.sb(es, "pcol", [128, 1], F32)
    psA = [k.ps(es, "psA%d" % i, [128, 512], F32) for i in range(6)]
    psT = [k.ps(es, "psT%d" % i, [128, 1024], BF16) for i in range(2)]

    k.dma("sp", lambda e: e.dma_start(out=ident_f[:, :], in_=ident_in), W=[ident_f])
    k.dma("sp", lambda e: e.dma_start(out=utri_f[:, :], in_=utri_in), W=[utri_f])
    k.dma("sp", lambda e: e.dma_start(out=pcol[:, :], in_=pcol_in), W=[pcol])
    k.op("dve", lambda e: e.tensor_copy(out=ident_bf[:, :], in_=ident_f[:, :]), R=[ident_f], W=[ident_bf])
    k.op("dve", lambda e: e.tensor_copy(out=utri_bf[:, :], in_=utri_f[:, :]), R=[utri_f], W=[utri_bf])
    k.op("dve", lambda e: e.memset(ones_bf[:, :], 1.0), W=[ones_bf])
    k.op("dve", lambda e: e.memset(ones_f[:, :], 1.0), W=[ones_f])

    def cast_phase():
        pes = ExitStack()
        CHW = 4096
        NB_ = 3
        ibuf = [k.sb(pes, "cw_i%d" % i, [128, CHW], F32) for i in range(NB_)]
        obuf = [k.sb(pes, "cw_o%d" % i, [128, CHW], BF16) for i in range(NB_)]
        jobs = []
        pairs = [(adaw_in[0], adaw_b[0]), (adaw_in[1], adaw_b[1]), (win_in, win_b), (wq_in, wq_b),
                 (wkv_in, wkv_b), (wout_in, wout_b), (cwin_in, cwin_b), (cwout_in, cwout_b)]
        for src, dst in pairs:
            R_, C_ = src.shape
            assert R_ % 128 == 0
            sv = src.rearrange("(p a) c -> p (a c)", p=128)
            dv = dst.rearrange("(p a) c -> p (a c)", p=128)
            tot = (R_ // 128) * C_
            for c0 in range(0, tot, CHW):
                w = min(CHW, tot - c0)
                jobs.append((sv[:, c0:c0 + w], dv[:, c0:c0 + w], w))
        engs = ["dve", "pool", "act"]
        PRE = 2
        for j in range(min(PRE, len(jobs))):
            s_, d_, w = jobs[j]
            k.dma("sp", lambda e, s_=s_, w=w, j=j: e.dma_start(out=ibuf[j % NB_][:, 0:w], in_=s_), W=[ibuf[j % NB_]])
        for j, (s_, d_, w) in enumerate(jobs):
            if j + PRE < len(jobs):
                s2, d2, w2 = jobs[j + PRE]
                bi = (j + PRE) % NB_
                k.dma("sp", lambda e, s2=s2, w2=w2, bi=bi: e.dma_start(out=ibuf[bi][:, 0:w2], in_=s2), W=[ibuf[bi]])
            b = j % NB_
            en = engs[j % 3]
            if en == "act":
                k.op("act", lambda e, b=b, w=w: e.copy(out=obuf[b][:, 0:w], in_=ibuf[b][:, 0:w]), R=[ibuf[b]], W=[obuf[b]])
            else:
                k.op(en, lambda e, b=b, w=w: e.tensor_copy(out=obuf[b][:, 0:w], in_=ibuf[b][:, 0:w]), R=[ibuf[b]], W=[obuf[b]])
            k.dma("act", lambda e, b=b, w=w, d_=d_: e.dma_start(out=d_, in_=obuf[b][:, 0:w]), R=[obuf[b]])
        k.barrier()
        pes.close()

    XCH = 1024
    xjobs = []
    for l in range(2):
        for fc in range(NFC):
            for src, dst in ((wgu_in[l][fc * E * 128:(fc + 1) * E * 128, :], wgu_b[l][fc]),
                             (wdn_in[l][fc * E * 128:(fc + 1) * E * 128, :], wdn_b[l][fc])):
                R_, C_ = src.shape
                sv = src.rearrange("(p a) c -> p (a c)", p=128)
                dv = dst.rearrange("(p a) c -> p (a c)", p=128)
                tot = (R_ // 128) * C_
                for c0 in range(0, tot, XCH):
                    w = min(XCH, tot - c0)
                    xjobs.append((sv[:, c0:c0 + w], dv[:, c0:c0 + w], w))
    xstate = {"i": 0, "ld": 0}

    def xcast_emit(n, ib, ob):
        NB_ = len(ib)
        for _ in range(n):
            j = xstate["i"]
            if j >= len(xjobs):
                return
            while xstate["ld"] < min(j + 3, len(xjobs)):
                jl = xstate["ld"]
                s_, d_, w = xjobs[jl]
                k.dma("sp", lambda e, s_=s_, w=w, jl=jl: e.dma_start(out=ib[jl % NB_][:, 0:w], in_=s_), W=[ib[jl % NB_]])
                xstate["ld"] += 1
            s_, d_, w = xjobs[j]
            b = j % NB_
            en = "dve" if j % 2 == 0 else "pool"
            k.op(en, lambda e, b=b, w=w: e.tensor_copy(out=ob[b][:, 0:w], in_=ib[b][:, 0:w]), R=[ib[b]], W=[ob[b]])
            k.dma("sp", lambda e, b=b, w=w, d_=d_: e.dma_start(out=d_, in_=ob[b][:, 0:w]), R=[ob[b]])
            xstate["i"] += 1

    def ada_phase():
        pes = ExitStack()
        cT = k.sb(pes, "cT", [128, (NBC + 1) * KC], F32)
        sT = k.sb(pes, "sT", [128, (NBC + 1) * KC], F32)
        scb = k.sb(pes, "scb", [128, (NBC + 1) * KC, 128], BF16)
        k.dma("sp", lambda e: e.dma_start(out=cT[:, :], in_=cT_in), W=[cT])
        k.op("act", lambda e: e.activation(out=sT[:, :], in_=cT[:, :], func=AF.Silu), R=[cT], W=[sT])
        for j in range((NBC + 1) * KC):
            k.op("dve", lambda e, j=j: e.tensor_scalar(out=scb[:, j, :], in0=ones_f[:, :], scalar1=sT[:, j:j + 1],
                                                       scalar2=None, op0=ALU.mult), R=[sT, ones_f], W=[scb])
        wsl = [k.sb(pes, "ada_w%d" % i, [128, KC, 512], BF16) for i in range(2)]
        bsl = [k.sb(pes, "ada_b%d" % i, [128, 512], F32) for i in range(2)]
        gsl = [k.sb(pes, "ada_g%d" % i, [128, 512], F32) for i in range(2)]
        osl = [k.sb(pes, "ada_o%d" % i, [128, 512], F32) for i in range(3)]
        it = 0
        oi = 0
        for l in range(2):
            for j in range(24):
                which, sub = j // 4, j % 4
                b = it % 2
                it += 1
                k.dma("sp", lambda e, l=l, j=j, b=b: e.dma_start(
                    out=wsl[b][:, :, :], in_=adaw_b[l][:, j * 512:(j + 1) * 512].rearrange("(kc p) n -> p kc n", p=128)),
                    W=[wsl[b]])
                k.dma("sp", lambda e, l=l, j=j, b=b: e.dma_start(out=bsl[b][:, :], in_=adab_in[l][:, j * 512:(j + 1) * 512]),
                      W=[bsl[b]])
                if which in (1, 4):
                    gsrc = gmix_in[l] if which == 1 else gffn_in[l]
                    k.dma("sp", lambda e, gsrc=gsrc, sub=sub, b=b: e.dma_start(out=gsl[b][:, :], in_=gsrc[:, sub * 512:(sub + 1) * 512]),
                          W=[gsl[b]])
                rows = list(range(NBC))
                if l == 0 and which < 2:
                    rows.append(NBC)
                for i in rows:
                    pz = psA[i % 4]
                    for kc in range(KC):
                        k.op("pe", lambda e, pz=pz, i=i, kc=kc, b=b: e.matmul(
                            pz[:, :], scb[:, i * KC + kc, :], wsl[b][:, kc, :], start=(kc == 0), stop=(kc == KC - 1)),
                            R=[scb, wsl[b]], W=[pz])
                    o = osl[oi % 3]
                    oi += 1
                    k.op("dve", lambda e, o=o, pz=pz, b=b: e.tensor_tensor(out=o[:, :], in0=pz[:, :], in1=bsl[b][:, :], op=ALU.add),
                         R=[pz, bsl[b]], W=[o])
                    if which in (1, 4):
                        k.op("dve", lambda e, o=o, b=b: e.scalar_tensor_tensor(out=o[:, :], in0=o[:, :], scalar=1.0, in1=gsl[b][:, :],
                                                                             op0=ALU.add, op1=ALU.mult), R=[o, gsl[b]], W=[o])
                    k.dma("act", lambda e, o=o, i=i, l=l, which=which, sub=sub: e.dma_start(
                        out=modv_ap(i, l, which)[:, sub * 512:(sub + 1) * 512], in_=o[:, :]), R=[o])
        k.barrier()
        pes.close()

    def rstd_from_ssq(ssq, rstd, n, width=1):
        k.op("dve", lambda e: e.tensor_scalar(out=rstd[:, 0:width], in0=ssq[:, 0:width], scalar1=1.0 / n, scalar2=EPS,
                                              op0=ALU.mult, op1=ALU.add), R=[ssq], W=[rstd])
        k.op("act", lambda e: e.activation(out=rstd[:, 0:width], in_=rstd[:, 0:width], func=AF.Sqrt), R=[rstd], W=[rstd])
        k.op("dve", lambda e: e.reciprocal(out=rstd[:, 0:width], in_=rstd[:, 0:width]), R=[rstd], W=[rstd])

    class NormState:
        pass

    def norm_tiles(pes, tag):
        ns = NormState()
        ns.junk = k.sb(pes, tag + "_junk", [128, D], BF16)
        ns.ssq = [k.sb(pes, tag + "_ssq%d" % i, [128, 1], F32) for i in range(2)]
        ns.rstd = [k.sb(pes, tag + "_rstd%d" % i, [128, 1], F32) for i in range(2)]
        ns.tmp = k.sb(pes, tag + "_tmp", [128, D], F32)
        ns.i = 0
        return ns

    def norm_mod(ns, xt, A_t, B_t, out_t, out_f32=None):
        i = ns.i % 2
        ns.i += 1
        ssq, rstd = ns.ssq[i], ns.rstd[i]
        k.op("act", lambda e: e.activation(out=ns.junk[:, :], in_=xt[:, :], func=AF.Square, accum_out=ssq[:, 0:1]),
             R=[xt], W=[ns.junk, ssq])
        rstd_from_ssq(ssq, rstd, D)
        k.op("dve", lambda e: e.scalar_tensor_tensor(out=ns.tmp[:, :], in0=xt[:, :], scalar=rstd[:, 0:1], in1=A_t[:, :],
                                                     op0=ALU.mult, op1=ALU.mult), R=[xt, rstd, A_t], W=[ns.tmp])
        if B_t is None:
            return
        if out_f32 is not None:
            k.op("pool", lambda e: e.tensor_tensor(out=out_f32[:, :], in0=ns.tmp[:, :], in1=B_t[:, :], op=ALU.add),
                 R=[ns.tmp, B_t], W=[out_f32])
            k.op("act", lambda e: e.copy(out=out_t[:, :], in_=out_f32[:, :]), R=[out_f32], W=[out_t])
        else:
            k.op("pool", lambda e: e.tensor_tensor(out=out_t[:, :], in0=ns.tmp[:, :], in1=B_t[:, :], op=ALU.add),
                 R=[ns.tmp, B_t], W=[out_t])

    def transpose_bf(h_t, hT, col0, ntile_cols=128):
        for half in range(2):
            pt = psT[half]
            for j in range(8):
                kc = half * 8 + j
                k.op("pe", lambda e, pt=pt, j=j, kc=kc: e.transpose(out=pt[:, j * 128:(j + 1) * 128],
                                                                    in_=h_t[:, kc * 128:(kc + 1) * 128], identity=ident_bf[:, :]),
                     R=[h_t, ident_bf], W=[pt])
            en = "act" if half == 0 else "dve"
            if en == "act":
                k.op("act", lambda e, pt=pt, half=half: e.copy(out=hT[:, half * 8:(half + 1) * 8, col0:col0 + 128],
                                                              in_=pt[:, :].rearrange("p (j t) -> p j t", j=8)), R=[pt], W=[hT])
            else:
                k.op("dve", lambda e, pt=pt, half=half: e.tensor_copy(out=hT[:, half * 8:(half + 1) * 8, col0:col0 + 128],
                                                                     in_=pt[:, :].rearrange("p (j t) -> p j t", j=8)), R=[pt], W=[hT])

    def load_mod(pes, i, l, which, name):
        t = k.sb(pes, name, [128, D], F32)
        k.dma("sp", lambda e: e.dma_start(out=t[:, :], in_=modv_ap(i, l, which)), W=[t])
        return t

    def mla_latent_phase(i):
        pes = ExitStack()
        A1 = load_mod(pes, i, 0, 1, "A1")
        B1 = load_mod(pes, i, 0, 0, "B1")
        Ac = load_mod(pes, NBC, 0, 1, "Ac")
        Bc = load_mod(pes, NBC, 0, 0, "Bc")
        win_sb = k.sb(pes, "win_sb", [128, KC, 1152], BF16)
        k.dma("sp", lambda e: e.dma_start(out=win_sb[:, :, :], in_=win_b.rearrange("(kc p) n -> p kc n", p=128)), W=[win_sb])
        gq = k.sb(pes, "gq", [128, 4], F32)
        gkv = k.sb(pes, "gkv", [128, 4], F32)
        k.dma("sp", lambda e: e.dma_start(out=gq[:, :], in_=gq_in), W=[gq])
        k.dma("sp", lambda e: e.dma_start(out=gkv[:, :], in_=gkv_in), W=[gkv])
        ns = norm_tiles(pes, "n1")
        xt = [k.sb(pes, "xt%d" % j, [128, D], F32) for j in range(2)]
        hb = [k.sb(pes, "hb%d" % j, [128, D], BF16) for j in range(2)]
        hT = k.sb(pes, "hT", [128, KC, 512], BF16)
        lat_f = k.sb(pes, "lat_f", [128, 4, 512], F32)
        lat_sq = k.sb(pes, "lat_sq", [128, 4, 512], BF16)
        lat_n = k.sb(pes, "lat_n", [128, 4, 512], BF16)
        rbc = k.sb(pes, "rbc", [128, 512], F32)
        cs = k.sb(pes, "cs", [64, 2, 512], F32)
        t1 = k.sb(pes, "t1", [64, 512], F32)
        t2 = k.sb(pes, "t2", [64, 512], F32)
        kr_o = k.sb(pes, "kr_o", [64, 512], BF16)
        xi = 0
        chunks = [("ctx", 0, CTX)] + [("lat", c * 512, 512) for c in range(NCH)]
        for kind, t0, w in chunks:
            ntile = w // 128
            for tt in range(ntile):
                xb = xt[xi % 2]
                hbb = hb[xi % 2]
                xi += 1
                if kind == "ctx":
                    src = ctx_in[i * CTX + tt * 128:i * CTX + (tt + 1) * 128, :]
                else:
                    src = x_in[i * S + t0 + tt * 128:i * S + t0 + (tt + 1) * 128, :]
                k.dma("sp", lambda e, xb=xb, src=src: e.dma_start(out=xb[:, :], in_=src), W=[xb])
                norm_mod(ns, xb, Ac if kind == "ctx" else A1, Bc if kind == "ctx" else B1, hbb)
                transpose_bf(hbb, hT, tt * 128)
            groups = ([("cq", 0)] if kind == "lat" else []) + [("ckv", 4)]
            for gname, m0 in groups:
                for j in range(4):
                    for kc in range(KC):
                        k.op("pe", lambda e, j=j, kc=kc, m0=m0: e.matmul(
                            psA[j][:, 0:w], win_sb[:, kc, (m0 + j) * 128:(m0 + j + 1) * 128], hT[:, kc, 0:w],
                            start=(kc == 0), stop=(kc == KC - 1)), R=[win_sb, hT], W=[psA[j]])
                    k.op("act", lambda e, j=j: e.copy(out=lat_f[:, j, 0:w], in_=psA[j][:, 0:w]), R=[psA[j]], W=[lat_f])
                    k.op("act", lambda e, j=j: e.activation(out=lat_sq[:, j, 0:w], in_=psA[j][:, 0:w], func=AF.Square),
                         R=[psA[j]], W=[lat_sq])
                for j in range(4):
                    k.op("pe", lambda e, j=j: e.matmul(psA[4][:, 0:w], ones_bf[:, :], lat_sq[:, j, 0:w], start=(j == 0), stop=(j == 3)),
                         R=[ones_bf, lat_sq], W=[psA[4]])
                k.op("dve", lambda e: e.tensor_scalar(out=rbc[:, 0:w], in0=psA[4][:, 0:w], scalar1=1.0 / 512, scalar2=EPS,
                                                      op0=ALU.mult, op1=ALU.add), R=[psA[4]], W=[rbc])
                k.op("act", lambda e: e.activation(out=rbc[:, 0:w], in_=rbc[:, 0:w], func=AF.Sqrt), R=[rbc], W=[rbc])
                k.op("dve", lambda e: e.reciprocal(out=rbc[:, 0:w], in_=rbc[:, 0:w]), R=[rbc], W=[rbc])
                gg = gq if gname == "cq" else gkv
                for j in range(4):
                    k.op("dve", lambda e, j=j, gg=gg: e.scalar_tensor_tensor(
                        out=lat_n[:, j, 0:w], in0=lat_f[:, j, 0:w], scalar=gg[:, j:j + 1], in1=rbc[:, 0:w],
                        op0=ALU.mult, op1=ALU.mult), R=[lat_f, gg, rbc], W=[lat_n])
                if gname == "cq":
                    dst = cqnT.rearrange("(j p) s -> p j s", p=128)[:, :, t0:t0 + w]
                else:
                    k0 = 0 if kind == "ctx" else CTX + t0
                    dst = kvT.rearrange("(j p) s -> p j s", p=128)[:, :, k0:k0 + w]
                k.dma("act", lambda e, dst=dst: e.dma_start(out=dst, in_=lat_n[:, :, 0:w]), R=[lat_n])
            for which, m0 in ((0, 1024), (1, 1088)):
                pz = psA[4 + which]
                for kc in range(KC):
                    k.op("pe", lambda e, pz=pz, kc=kc, m0=m0: e.matmul(pz[0:64, 0:w], win_sb[:, kc, m0:m0 + 64], hT[:, kc, 0:w],
                                                                        start=(kc == 0), stop=(kc == KC - 1)), R=[win_sb, hT], W=[pz])
            if kind == "ctx":
                k.op("act", lambda e: e.copy(out=kr_o[:, 0:w], in_=psA[4][0:64, 0:w]), R=[psA[4]], W=[kr_o])
                k.op("act", lambda e: e.copy(out=t2[:, 0:w], in_=psA[5][0:64, 0:w]), R=[psA[5]], W=[t2])
                k0 = 0
            else:
                k.dma("sp", lambda e: e.dma_start(out=cs[:, 0, :], in_=ropec_in[:, t0:t0 + 512]), W=[cs])
                k.dma("sp", lambda e: e.dma_start(out=cs[:, 1, :], in_=ropes_in[:, t0:t0 + 512]), W=[cs])
                k.op("dve", lambda e: e.tensor_tensor(out=t1[:, :], in0=psA[4][0:64, :], in1=cs[:, 0, :], op=ALU.mult),
                     R=[psA[4], cs], W=[t1])
                k.op("dve", lambda e: e.tensor_tensor(out=t2[:, :], in0=psA[5][0:64, :], in1=cs[:, 1, :], op=ALU.mult),
                     R=[psA[5], cs], W=[t2])
                k.op("pool", lambda e: e.tensor_tensor(out=kr_o[:, :], in0=t1[:, :], in1=t2[:, :], op=ALU.add),
                     R=[t1, t2], W=[kr_o])
                k0 = CTX + t0
            k.dma("act", lambda e, k0=k0: e.dma_start(out=krT[:, k0:k0 + w], in_=kr_o[:, 0:w]), R=[kr_o])
        k.barrier()
        pes.close()

    def attention_phase(i):
        pes = ExitStack()
        kv_sb = k.sb(pes, "kv_sb", [128, 4, NK], BF16)
        kr_sb = k.sb(pes, "kr_sb", [128, NK], BF16)
        k.dma("sp", lambda e: e.dma_start(out=kv_sb[:, :, :], in_=kvT.rearrange("(j p) s -> p j s", p=128)), W=[kv_sb])
        k.op("dve", lambda e: e.memset(kr_sb[64:128, :], 0.0), W=[kr_sb])
        k.dma("sp", lambda e: e.dma_start(out=kr_sb[0:64, :], in_=krT), W=[kr_sb])
        KnT = k.sb(pes, "KnT", [128, NK], BF16)
        V = k.sb(pes, "V", [128, NKT, 128], BF16)
        wqh = k.sb(pes, "wqh", [128, 4, 256], BF16)
        wkvh = k.sb(pes, "wkvh", [128, 4, 256], BF16)
        sqt = [k.sb(pes, "sqt%d" % j, [128, 512], BF16) for j in range(2)]
        kmx = k.sb(pes, "kmx", [128, 40], F32)
        krmax = k.sb(pes, "krmax", [128, 1], F32)
        kmax2 = k.sb(pes, "kmax2", [128, 1], F32)
        cqc = [k.sb(pes, "cqc%d" % j, [128, 4, 512], BF16) for j in range(2)]
        csq = [k.sb(pes, "csq%d" % j, [64, 2, 512], F32) for j in range(2)]
        QnT = [k.sb(pes, "QnT%d" % j, [128, 512], BF16) for j in range(2)]
        QrT = [k.sb(pes, "QrT%d" % j, [128, 512], BF16) for j in range(2)]
        sqn = k.sb(pes, "sqn", [128, 512], BF16)
        sqr = k.sb(pes, "sqr", [128, 512], BF16)
        for t_ in QrT + [sqr]:
            k.op("dve", lambda e, t_=t_: e.memset(t_[:, :], 0.0), W=[t_])
        t1 = k.sb(pes, "at1", [64, 512], F32)
        t2 = k.sb(pes, "at2", [64, 512], F32)
        qm = [k.sb(pes, "qm%d" % j, [128, 1], F32) for j in range(2)]
        nb = [k.sb(pes, "nb%d" % j, [128, 1], F32) for j in range(2)]
        PT = [k.sb(pes, "PT%d" % j, [128, 512], BF16) for j in range(4)]
        rec = k.sb(pes, "rec", [128, 512], F32)
        ot = [k.sb(pes, "ot%d" % j, [128, 512], BF16) for j in range(2)]
        xib = [k.sb(pes, "xib%d" % j, [128, XCH], F32) for j in range(4)]
        xob = [k.sb(pes, "xob%d" % j, [128, XCH], BF16) for j in range(4)]
        jps = -(-(len(xjobs) - xstate["i"]) // (NH * NCH))
        NKC = -(-NK // 512)
        for c in range(NKC if cfg.get("ASTOP", 99) != -1 else 0):
            c0 = c * 512
            w = min(512, NK - c0)
            s_ = sqt[c % 2]
            k.op("act", lambda e, s_=s_, c0=c0, w=w: e.activation(out=s_[:, 0:w], in_=kr_sb[:, c0:c0 + w], func=AF.Square),
                 R=[kr_sb], W=[s_])
            k.op("pe", lambda e, s_=s_, w=w: e.matmul(psA[0][:, 0:w], ones_bf[:, :], s_[:, 0:w], start=True, stop=True),
                 R=[ones_bf, s_], W=[psA[0]])
            k.op("dve", lambda e, c=c, w=w: e.reduce_max(out=kmx[:, c:c + 1], in_=psA[0][:, 0:w], axis=AX.X), R=[psA[0]], W=[kmx])
        k.op("dve", lambda e: e.reduce_max(out=krmax[:, 0:1], in_=kmx[:, 0:NKC], axis=AX.X), R=[kmx], W=[krmax])
        it = 0
        AST = cfg.get("ASTOP", 99)

        class _Stop(Exception):
            pass

        def chk(n):
            if AST <= n:
                raise _Stop()
        try:
          chk(1)
          for h in range(NH):
              k.dma("sp", lambda e, h=h: e.dma_start(
                  out=wqh[:, :, :], in_=wq_b[:, h * 256:(h + 1) * 256].rearrange("(j p) n -> p j n", p=128)), W=[wqh])
              k.dma("sp", lambda e, h=h: e.dma_start(
                  out=wkvh[:, :, :], in_=wkv_b[:, h * 256:(h + 1) * 256].rearrange("(j p) n -> p j n", p=128)), W=[wkvh])
              for c in range(NKC):
                  c0 = c * 512
                  w = min(512, NK - c0)
                  pz = psA[c % 2]
                  for j in range(4):
                      k.op("pe", lambda e, pz=pz, j=j, c0=c0, w=w: e.matmul(pz[:, 0:w], wkvh[:, j, 0:128], kv_sb[:, j, c0:c0 + w],
                                                                            start=(j == 0), stop=(j == 3)), R=[wkvh, kv_sb], W=[pz])
                  k.op("dve", lambda e, pz=pz, c0=c0, w=w: e.tensor_copy(out=KnT[:, c0:c0 + w], in_=pz[:, 0:w]), R=[pz], W=[KnT])
                  s_ = sqt[c % 2]
                  k.op("act", lambda e, pz=pz, s_=s_, w=w: e.activation(out=s_[:, 0:w], in_=pz[:, 0:w], func=AF.Square), R=[pz], W=[s_])
                  k.op("pe", lambda e, s_=s_, w=w: e.matmul(psA[2][:, 0:w], ones_bf[:, :], s_[:, 0:w], start=True, stop=True),
                       R=[ones_bf, s_], W=[psA[2]])
                  k.op("dve", lambda e, c=c, w=w: e.reduce_max(out=kmx[:, c:c + 1], in_=psA[2][:, 0:w], axis=AX.X), R=[psA[2]], W=[kmx])
              k.op("dve", lambda e: e.reduce_max(out=kmax2[:, 0:1], in_=kmx[:, 0:NKC], axis=AX.X), R=[kmx], W=[kmax2])
              k.op("dve", lambda e: e.tensor_tensor(out=kmax2[:, 0:1], in0=kmax2[:, 0:1], in1=krmax[:, 0:1], op=ALU.add),
                   R=[kmax2, krmax], W=[kmax2])
              chk(2)
              for g in range(-(-NKT // 4)):
                  pz = psA[3 + g % 2]
                  nk_ = min(4, NKT - g * 4)
                  for q in range(nk_):
                      kt = g * 4 + q
                      for j in range(4):
                          k.op("pe", lambda e, pz=pz, q=q, kt=kt, j=j: e.matmul(
                              pz[:, q * 128:(q + 1) * 128], kv_sb[:, j, kt * 128:(kt + 1) * 128], wkvh[:, j, 128:256],
                              start=(j == 0), stop=(j == 3)), R=[kv_sb, wkvh], W=[pz])
                  k.op("act", lambda e, pz=pz, g=g, nk_=nk_: e.copy(
                      out=V[:, g * 4:g * 4 + nk_, :], in_=pz[:, 0:nk_ * 128].rearrange("p (q d) -> p q d", q=nk_)), R=[pz], W=[V])
              chk(3)
              for c in range(NCH):
                  t0 = c * 512
                  b2 = it % 2
                  it += 1
                  cq_ = cqc[b2]
                  cs_ = csq[b2]

                  def qloads(bb, tq):
                      k.dma("sp", lambda e: e.dma_start(
                          out=cqc[bb][:, :, :], in_=cqnT.rearrange("(j p) s -> p j s", p=128)[:, :, tq:tq + 512]), W=[cqc[bb]])
                      k.dma("sp", lambda e: e.dma_start(out=csq[bb][:, 0, :], in_=ropec_in[:, tq:tq + 512]), W=[csq[bb]])
                      k.dma("sp", lambda e: e.dma_start(out=csq[bb][:, 1, :], in_=ropes_in[:, tq:tq + 512]), W=[csq[bb]])
                  if h == 0 and c == 0:
                      qloads(b2, t0)
                  qn, qr = QnT[b2], QrT[b2]
                  for j in range(4):
                      k.op("pe", lambda e, j=j, cq_=cq_: e.matmul(psA[2][:, :], wqh[:, j, 0:128], cq_[:, j, :], start=(j == 0), stop=(j == 3)),
                           R=[wqh, cq_], W=[psA[2]])
                  k.op("dve", lambda e, qn=qn: e.tensor_copy(out=qn[:, :], in_=psA[2][:, :]), R=[psA[2]], W=[qn])
                  k.op("act", lambda e: e.activation(out=sqn[:, :], in_=psA[2][:, :], func=AF.Square), R=[psA[2]], W=[sqn])
                  for which in range(2):
                      pz = psA[3 + which]
                      for j in range(4):
                          k.op("pe", lambda e, pz=pz, j=j, cq_=cq_, which=which: e.matmul(
                              pz[0:64, :], wqh[:, j, 128 + which * 64:192 + which * 64], cq_[:, j, :], start=(j == 0), stop=(j == 3)),
                              R=[wqh, cq_], W=[pz])
                  k.op("dve", lambda e, cs_=cs_: e.tensor_tensor(out=t1[:, :], in0=psA[3][0:64, :], in1=cs_[:, 0, :], op=ALU.mult),
                       R=[psA[3], cs_], W=[t1])
                  k.op("dve", lambda e, cs_=cs_: e.tensor_tensor(out=t2[:, :], in0=psA[4][0:64, :], in1=cs_[:, 1, :], op=ALU.mult),
                       R=[psA[4], cs_], W=[t2])
                  k.op("pool", lambda e, qr=qr: e.tensor_tensor(out=qr[0:64, :], in0=t1[:, :], in1=t2[:, :], op=ALU.add), R=[t1, t2], W=[qr])
                  k.op("act", lambda e, qr=qr: e.activation(out=sqr[0:64, :], in_=qr[0:64, :], func=AF.Square), R=[qr], W=[sqr])
                  k.op("pe", lambda e: e.matmul(psA[2][:, :], ones_bf[:, :], sqn[:, :], start=True, stop=False), R=[ones_bf, sqn], W=[psA[2]])
                  k.op("pe", lambda e: e.matmul(psA[2][:, :], ones_bf[:, :], sqr[:, :], start=False, stop=True), R=[ones_bf, sqr], W=[psA[2]])
                  qm_, nb_ = qm[b2], nb[b2]
                  k.op("dve", lambda e, qm_=qm_: e.reduce_max(out=qm_[:, 0:1], in_=psA[2][:, :], axis=AX.X), R=[psA[2]], W=[qm_])
                  k.op("dve", lambda e, qm_=qm_: e.tensor_tensor(out=qm_[:, 0:1], in0=qm_[:, 0:1], in1=kmax2[:, 0:1], op=ALU.mult),
                       R=[qm_, kmax2], W=[qm_])
                  k.op("act", lambda e, qm_=qm_: e.activation(out=qm_[:, 0:1], in_=qm_[:, 0:1], func=AF.Sqrt), R=[qm_], W=[qm_])
                  k.op("dve", lambda e, qm_=qm_, nb_=nb_: e.tensor_scalar(out=nb_[:, 0:1], in0=qm_[:, 0:1], scalar1=-ATT_SCALE, scalar2=None,
                                                                         op0=ALU.mult), R=[qm_], W=[nb_])
                  chk(4)
                  Ops, Dps = psA[4], psA[5]
                  def emitS(kt):
                      sp_ = psA[kt % 2]
                      k.op("pe", lambda e, sp_=sp_, kt=kt, qn=qn: e.matmul(sp_[:, :], KnT[:, kt * 128:(kt + 1) * 128], qn[:, :], start=True, stop=False),
                           R=[KnT, qn], W=[sp_])
                      k.op("pe", lambda e, sp_=sp_, kt=kt, qr=qr: e.matmul(sp_[:, :], kr_sb[:, kt * 128:(kt + 1) * 128], qr[:, :], start=False, stop=True),
                           R=[kr_sb, qr], W=[sp_])
                  if not (h == NH - 1 and c == NCH - 1):
                      qloads(1 - b2, ((c + 1) % NCH) * 512)
                  emitS(0)
                  if NKT > 1:
                      emitS(1)
                  for kt in range(NKT):
                      sp_ = psA[kt % 2]
                      pt_ = PT[kt % 4]
                      k.op("act", lambda e, sp_=sp_, pt_=pt_, nb_=nb_: e.activation(out=pt_[:, :], in_=sp_[:, :], func=AF.Exp,
                                                                                  bias=nb_[:, 0:1], scale=ATT_SCALE), R=[sp_, nb_], W=[pt_])
                      k.op("pe", lambda e, kt=kt, pt_=pt_: e.matmul(Ops[:, :], V[:, kt, :], pt_[:, :], start=(kt == 0), stop=(kt == NKT - 1)),
                           R=[V, pt_], W=[Ops])
                      k.op("pe", lambda e, kt=kt, pt_=pt_: e.matmul(Dps[:, :], ones_bf[:, :], pt_[:, :], start=(kt == 0), stop=(kt == NKT - 1)),
                           R=[ones_bf, pt_], W=[Dps])
                      if kt + 2 < NKT:
                          emitS(kt + 2)
                      xcast_emit(((kt + 1) * jps) // NKT - (kt * jps) // NKT, xib, xob)
                  k.op("dve", lambda e: e.reciprocal(out=rec[:, :], in_=Dps[:, :]), R=[Dps], W=[rec])
                  o_ = ot[b2]
                  k.op("dve", lambda e, o_=o_: e.tensor_tensor(out=o_[:, :], in0=Ops[:, :], in1=rec[:, :], op=ALU.mult), R=[Ops, rec], W=[o_])
                  k.dma("act", lambda e, o_=o_, h=h, t0=t0: e.dma_start(out=oT[h * 128:(h + 1) * 128, t0:t0 + 512], in_=o_[:, :]), R=[o_])
                  chk(5)
        except _Stop:
            pass
        xcast_emit(len(xjobs), xib, xob)
        k.barrier()
        pes.close()

    class RouteState:
        pass

    def mix_phase(l, i, w_dram, rs):
        pes = ExitStack()
        w_sb = k.sb(pes, "wo_sb", [128, KC, D], BF16)
        k.dma("sp", lambda e: e.dma_start(out=w_sb[:, :, :], in_=w_dram.rearrange("(kc p) n -> p kc n", p=128)), W=[w_sb])
        G1 = load_mod(pes, i, l, 2, "G1")
        A2 = load_mod(pes, i, l, 4, "A2")
        B2 = load_mod(pes, i, l, 3, "B2")
        wr_sb = k.sb(pes, "wr_sb", [128, KC, E], F32)
        rb_sb = k.sb(pes, "rb_sb", [128, E], F32)
        k.dma("sp", lambda e: e.dma_start(out=wr_sb[:, :, :], in_=wr_in[l].rearrange("(kc p) n -> p kc n", p=128)), W=[wr_sb])
        k.dma("sp", lambda e: e.dma_start(out=rb_sb[:, :], in_=rb_in[l]), W=[rb_sb])
        srcT = k.sb(pes, "srcT", [128, KC, 512], BF16)
        xt = [k.sb(pes, "mxt%d" % j, [128, D], F32) for j in range(2)]
        x1 = [k.sb(pes, "mx1%d" % j, [128, D], F32) for j in range(2)]
        h2f = k.sb(pes, "h2f", [128, D], F32)
        h2b = [k.sb(pes, "h2b%d" % j, [128, D], BF16) for j in range(2)]
        h2T = k.sb(pes, "h2T", [128, KC, 128], F32)
        ns = norm_tiles(pes, "n2")
        m8 = k.sb(pes, "m8", [128, 8], F32)
        msk = k.sb(pes, "msk", [128, E], F32)
        xi = 0
        for c in range(NCH):
            t0 = c * 512
            k.dma("sp", lambda e, t0=t0: e.dma_start(out=srcT[:, :, :], in_=oT.rearrange("(kc p) s -> p kc s", p=128)[:, :, t0:t0 + 512]),
                  W=[srcT])
            for tt in range(4):
                gt = t0 // 128 + tt
                r0 = i * S + gt * 128
                xb, x1b, hbb = xt[xi % 2], x1[xi % 2], h2b[xi % 2]
                xi += 1
                src = x_in[r0:r0 + 128, :] if l == 0 else xres[r0:r0 + 128, :]
                k.dma("sp", lambda e, xb=xb, src=src: e.dma_start(out=xb[:, :], in_=src), W=[xb])
                for n in range(4):
                    for kc in range(KC):
                        k.op("pe", lambda e, n=n, kc=kc, tt=tt: e.matmul(psA[n][:, :], srcT[:, kc, tt * 128:(tt + 1) * 128],
                                                                         w_sb[:, kc, n * 512:(n + 1) * 512], start=(kc == 0), stop=(kc == KC - 1)),
                             R=[srcT, w_sb], W=[psA[n]])
                    k.op("dve", lambda e, n=n, x1b=x1b: e.tensor_tensor(out=x1b[:, n * 512:(n + 1) * 512], in0=psA[n][:, :],
                                                                       in1=G1[:, n * 512:(n + 1) * 512], op=ALU.mult), R=[psA[n], G1], W=[x1b])
                k.op("pool", lambda e, x1b=x1b, xb=xb: e.tensor_tensor(out=x1b[:, :], in0=x1b[:, :], in1=xb[:, :], op=ALU.add),
                     R=[x1b, xb], W=[x1b])
                k.dma("act", lambda e, x1b=x1b, r0=r0: e.dma_start(out=xres[r0:r0 + 128, :], in_=x1b[:, :]), R=[x1b])
                norm_mod(ns, x1b, A2, B2, hbb, out_f32=h2f)
                k.dma("act", lambda e, hbb=hbb, r0=r0: e.dma_start(out=hrows[r0:r0 + 128, :], in_=hbb[:, :]), R=[hbb])
                for g in range(4):
                    pz = psA[4 + g % 2]
                    for q in range(4):
                        kc = g * 4 + q
                        k.op("pe", lambda e, pz=pz, q=q, kc=kc: e.transpose(out=pz[:, q * 128:(q + 1) * 128],
                                                                            in_=h2f[:, kc * 128:(kc + 1) * 128], identity=ident_f[:, :]),
                             R=[h2f, ident_f], W=[pz])
                    k.op("act", lambda e, pz=pz, g=g: e.copy(out=h2T[:, g * 4:(g + 1) * 4, :], in_=pz[:, :].rearrange("p (q t) -> p q t", q=4)),
                         R=[pz], W=[h2T])
                for kc in range(KC):
                    k.op("pe", lambda e, kc=kc: e.matmul(psA[4][:, 0:E], h2T[:, kc, :], wr_sb[:, kc, :], start=(kc == 0), stop=(kc == KC - 1)),
                         R=[h2T, wr_sb], W=[psA[4]])
                lg = rs.logits
                k.op("dve", lambda e, gt=gt: e.tensor_tensor(out=lg[:, gt, :], in0=psA[4][:, 0:E], in1=rb_sb[:, :], op=ALU.add),
                     R=[psA[4], rb_sb], W=[lg])
                k.op("dve", lambda e, gt=gt: e.max(out=m8[:, :], in_=lg[:, gt, :]), R=[lg], W=[m8])
                k.op("dve", lambda e, gt=gt: e.tensor_scalar(out=msk[:, :], in0=lg[:, gt, :], scalar1=m8[:, 3:4], scalar2=None, op0=ALU.is_ge),
                     R=[lg, m8], W=[msk])
                k.op("pe", lambda e: e.matmul(psA[5][:, 0:E], utri_f[:, :], msk[:, :], start=True, stop=True), R=[utri_f, msk], W=[psA[5]])
                k.op("dve", lambda e, gt=gt: e.tensor_tensor(out=rs.pos[:, gt, :], in0=psA[5][:, 0:E], in1=rs.base[:, :], op=ALU.add),
                     R=[psA[5], rs.base], W=[rs.pos])
                k.op("pe", lambda e: e.matmul(psA[5][:, 0:E], ones_f[:, :], msk[:, :], start=True, stop=True), R=[ones_f, msk], W=[psA[5]])
                k.op("dve", lambda e: e.tensor_tensor(out=rs.base[:, :], in0=psA[5][:, 0:E], in1=rs.base[:, :], op=ALU.add),
                     R=[psA[5], rs.base], W=[rs.base])
        k.barrier()
        pes.close()

    def moe_phase(l, i, rs):
        RB = i * S
        pes = ExitStack()
        padf = k.sb(pes, "padf", [128, E], F32)
        padi = k.sb(pes, "padi", [128, E], I32)
        pend = k.sb(pes, "pend", [128, E], F32)
        pstart = k.sb(pes, "pstart", [128, E], F32)
        blks = k.sb(pes, "blks", [128, NBLK], F32)
        blke = k.sb(pes, "blke", [128, NBLK], F32)
        tmpb = k.sb(pes, "tmpb", [128, NBLK], F32)
        k.dma("sp", lambda e: e.dma_start(out=blks[:, :], in_=blks_in), W=[blks])
        k.op("dve", lambda e: e.tensor_scalar(out=padf[:, :], in0=rs.base[:, :], scalar1=float(BLK - 1), scalar2=None, op0=ALU.add),
             R=[rs.base], W=[padf])
        k.op("dve", lambda e: e.tensor_copy(out=padi[:, :], in_=padf[:, :]), R=[padf], W=[padi])
        k.op("dve", lambda e: e.tensor_scalar(out=padi[:, :], in0=padi[:, :], scalar1=9, scalar2=None, op0=ALU.arith_shift_right),
             R=[padi], W=[padi])
        k.op("dve", lambda e: e.tensor_scalar(out=padi[:, :], in0=padi[:, :], scalar1=9, scalar2=None, op0=ALU.logical_shift_left),
             R=[padi], W=[padi])
        k.op("dve", lambda e: e.tensor_copy(out=padf[:, :], in_=padi[:, :]), R=[padi], W=[padf])
        k.op("dve", lambda e: e.tensor_tensor_scan(out=pend[:, :], data0=ones_f[:, 0:E], data1=padf[:, :], initial=0.0,
                                                   op0=ALU.mult, op1=ALU.add), R=[ones_f, padf], W=[pend])
        k.op("dve", lambda e: e.tensor_tensor(out=pstart[:, :], in0=pend[:, :], in1=padf[:, :], op=ALU.subtract), R=[pend, padf], W=[pstart])
        k.op("dve", lambda e: e.memset(blke[:, :], 0.0), W=[blke])
        for ee in range(E):
            k.op("dve", lambda e, ee=ee: e.scalar_tensor_tensor(out=blke[:, :], in0=blks[:, :], scalar=pend[:, ee:ee + 1], in1=blke[:, :],
                                                                 op0=ALU.is_ge, op1=ALU.add), R=[blks, pend, blke], W=[blke])
        k.op("dve", lambda e: e.tensor_scalar(out=blke[:, :], in0=blke[:, :], scalar1=float(E - 1), scalar2=None, op0=ALU.min), R=[blke], W=[blke])
        k.op("dve", lambda e: e.tensor_scalar(out=rs.idxb[:, :], in0=blke[:, :], scalar1=128.0, scalar2=pcol[:, 0:1], op0=ALU.mult, op1=ALU.add),
             R=[blke, pcol], W=[rs.idxb])
        k.op("dve", lambda e: e.tensor_copy(out=rs.idxd[:, :], in_=blke[:, :]), R=[blke], W=[rs.idxd])

        m8 = k.sb(pes, "dm8", [128, 8], F32)
        ex = k.sb(pes, "dex", [128, 4], F32)
        nm = k.sb(pes, "dnm", [128, 1], F32)
        es_ = k.sb(pes, "des", [128, 1], F32)
        oh = k.sb(pes, "doh", [128, E], F32)
        dsum = k.sb(pes, "dsum", [128, E], F32)
        junk = k.sb(pes, "djunk", [128, E], F32)
        dstf = k.sb(pes, "dstf", [128, 4], F32)
        hr = [k.sb(pes, "hr%d" % j, [128, D], BF16) for j in range(3)]
        lg = rs.logits
        for gt in range(NTB):
            hb = hr[gt % 3]
            k.dma("sp", lambda e, hb=hb, gt=gt: e.dma_start(out=hb[:, :], in_=hrows[RB + gt * 128:RB + (gt + 1) * 128, :]), W=[hb])
            k.op("dve", lambda e, gt=gt: e.max(out=m8[:, :], in_=lg[:, gt, :]), R=[lg], W=[m8])
            k.op("dve", lambda e: e.tensor_scalar(out=nm[:, :], in0=m8[:, 0:1], scalar1=-1.0, scalar2=None, op0=ALU.mult), R=[m8], W=[nm])
            k.op("act", lambda e: e.activation(out=ex[:, :], in_=m8[:, 0:4], func=AF.Exp, bias=nm[:, 0:1], scale=1.0, accum_out=es_[:, 0:1]),
                 R=[m8, nm], W=[ex, es_])
            k.op("dve", lambda e: e.reciprocal(out=es_[:, :], in_=es_[:, :]), R=[es_], W=[es_])
            k.op("dve", lambda e, gt=gt: e.tensor_scalar(out=rs.G[:, gt, :], in0=ex[:, :], scalar1=es_[:, 0:1], scalar2=None, op0=ALU.mult),
                 R=[ex, es_], W=[rs.G])
            k.op("dve", lambda e, gt=gt: e.tensor_tensor(out=dsum[:, :], in0=rs.pos[:, gt, :], in1=pstart[:, :], op=ALU.add),
                 R=[rs.pos, pstart], W=[dsum])
            for kk in range(TOPK):
                k.op("dve", lambda e, gt=gt, kk=kk: e.tensor_scalar(out=oh[:, :], in0=lg[:, gt, :], scalar1=m8[:, kk:kk + 1], scalar2=None,
                                                                    op0=ALU.is_equal), R=[lg, m8], W=[oh])
                k.op("dve", lambda e, kk=kk: e.scalar_tensor_tensor(out=junk[:, :], in0=oh[:, :], scalar=1.0, in1=dsum[:, :],
                                                                    op0=ALU.mult, op1=ALU.mult, accum_out=dstf[:, kk:kk + 1]),
                     R=[oh, dsum], W=[junk, dstf])
            k.op("dve", lambda e, gt=gt: e.tensor_copy(out=rs.dest[:, gt, :], in_=dstf[:, :]), R=[dstf], W=[rs.dest])
            for kk in range(TOPK):
                k.dma("pool", lambda e, hb=hb, gt=gt, kk=kk: e.indirect_dma_start(
                    out=xs[:, :], out_offset=bass.IndirectOffsetOnAxis(ap=rs.dest[:, gt, kk:kk + 1], axis=0),
                    in_=hb[:, :], in_offset=None), R=[hb, rs.dest])
        k.barrier()
        pes.close()

        pes = ExitStack()
        xsb = [k.sb(pes, "xsb%d" % j, [128, D], BF16) for j in range(4)]
        xT = k.sb(pes, "xT", [128, KC, 512], BF16)
        wgu = [k.sb(pes, "wgu%d" % j, [128, KC, 256], BF16) for j in range(4)]
        wdn = [k.sb(pes, "wdn%d" % j, [128, D], BF16) for j in range(NFC)]
        bgu = k.sb(pes, "bgu", [128, 2 * NFC], F32)
        bl1 = k.sb(pes, "bl1", [128, NFC], F32)
        bdn = k.sb(pes, "bdn", [128, D], F32)
        actT = k.sb(pes, "actT", [128, NFC, 512], BF16)
        g1s = [k.sb(pes, "g1%d" % j, [128, 512], F32) for j in range(2)]
        sgs = [k.sb(pes, "sg%d" % j, [128, 512], F32) for j in range(2)]
        l2s = [k.sb(pes, "l2%d" % j, [128, 512], F32) for j in range(2)]
        gss = [k.sb(pes, "gs%d" % j, [128, 512], F32) for j in range(2)]
        yo = [k.sb(pes, "yo%d" % j, [128, D], F32) for j in range(2)]
        xi = 0
        yi = 0
        wi = 0
        for blk in range(NBLK):
            k.dma("pool", lambda e, blk=blk: e.indirect_dma_start(
                out=bgu[:, :], out_offset=None, in_=bgu_in[l][:, :],
                in_offset=bass.IndirectOffsetOnAxis(ap=rs.idxb[:, blk:blk + 1], axis=0)), R=[rs.idxb], W=[bgu])
            k.dma("pool", lambda e, blk=blk: e.indirect_dma_start(
                out=bdn[:, :], out_offset=None, in_=bdn_in[l][:, :],
                in_offset=bass.IndirectOffsetOnAxis(ap=rs.idxd[:, blk:blk + 1], axis=0)), R=[rs.idxd], W=[bdn])
            k.op("dve", lambda e: e.tensor_scalar(out=bl1[:, :], in0=bgu[:, NFC:2 * NFC], scalar1=1.0, scalar2=None, op0=ALU.add), R=[bgu], W=[bl1])
            for st in range(4):
                xb = xsb[xi % 4]
                xi += 1
                r0 = blk * BLK + st * 128
                k.dma("sp", lambda e, xb=xb, r0=r0: e.dma_start(out=xb[:, :], in_=xs[r0:r0 + 128, :]), W=[xb])
                transpose_bf(xb, xT, st * 128)
            for fc in range(NFC):
                wg = wgu[wi % 4]
                wi += 1
                k.dma("pool", lambda e, wg=wg, blk=blk, fc=fc: e.indirect_dma_start(
                    out=wg[:, :, :].rearrange("p a b -> p (a b)"), out_offset=None, in_=wgu_b[l][fc][:, :],
                    in_offset=bass.IndirectOffsetOnAxis(ap=rs.idxb[:, blk:blk + 1], axis=0)), R=[rs.idxb], W=[wg])
                k.dma("pool", lambda e, blk=blk, fc=fc: e.indirect_dma_start(
                    out=wdn[fc][:, :], out_offset=None, in_=wdn_b[l][fc][:, :],
                    in_offset=bass.IndirectOffsetOnAxis(ap=rs.idxb[:, blk:blk + 1], axis=0)), R=[rs.idxb], W=[wdn[fc]])
                pg, pl = psA[2 * (fc % 2)], psA[2 * (fc % 2) + 1]
                g1, sg, l2, gs = g1s[fc % 2], sgs[fc % 2], l2s[fc % 2], gss[fc % 2]
                for half in range(2):
                    pz = pg if half == 0 else pl
                    for kc in range(KC):
                        k.op("pe", lambda e, pz=pz, wg=wg, kc=kc, half=half: e.matmul(
                            pz[:, :], wg[:, kc, half * 128:(half + 1) * 128], xT[:, kc, :], start=(kc == 0), stop=(kc == KC - 1)),
                            R=[wg, xT], W=[pz])
                k.op("dve", lambda e, fc=fc, g1=g1, pg=pg: e.tensor_scalar(out=g1[:, :], in0=pg[:, :], scalar1=bgu[:, fc:fc + 1], scalar2=LIMIT,
                                                             op0=ALU.add, op1=ALU.min), R=[pg, bgu], W=[g1])
                k.op("act", lambda e, g1=g1, sg=sg: e.activation(out=sg[:, :], in_=g1[:, :], func=AF.Sigmoid, scale=ALPHA), R=[g1], W=[sg])
                k.op("dve", lambda e, fc=fc, l2=l2, pl=pl: e.tensor_scalar(out=l2[:, :], in0=pl[:, :], scalar1=bl1[:, fc:fc + 1], scalar2=1.0 - LIMIT,
                                                             op0=ALU.add, op1=ALU.max), R=[pl, bl1], W=[l2])
                k.op("dve", lambda e, g1=g1, sg=sg, gs=gs: e.tensor_tensor(out=gs[:, :], in0=g1[:, :], in1=sg[:, :], op=ALU.mult), R=[g1, sg], W=[gs])
                k.op("dve", lambda e, fc=fc, l2=l2, gs=gs: e.scalar_tensor_tensor(out=actT[:, fc, :], in0=l2[:, :], scalar=1.0 + LIMIT, in1=gs[:, :],
                                                                     op0=ALU.min, op1=ALU.mult), R=[l2, gs], W=[actT])
            dn = 0
            for st in range(4):
                y_ = yo[yi % 2]
                yi += 1
                for n in range(4):
                    pz = psA[4 + dn % 2]
                    dn += 1
                    for fc in range(NFC):
                        k.op("pe", lambda e, pz=pz, fc=fc, st=st, n=n: e.matmul(
                            pz[:, :], actT[:, fc, st * 128:(st + 1) * 128], wdn[fc][:, n * 512:(n + 1) * 512],
                            start=(fc == 0), stop=(fc == NFC - 1)), R=[actT, wdn[fc]], W=[pz])
                    k.op("dve", lambda e, pz=pz, y_=y_, n=n: e.tensor_tensor(out=y_[:, n * 512:(n + 1) * 512], in0=pz[:, :],
                                                                            in1=bdn[:, n * 512:(n + 1) * 512], op=ALU.add), R=[pz, bdn], W=[y_])
                r0 = blk * BLK + st * 128
                for hf in range(2):
                    k.dma("act", lambda e, y_=y_, r0=r0, hf=hf: e.dma_start(out=ys[hf][r0:r0 + 128, :], in_=y_[:, hf * 1024:(hf + 1) * 1024]), R=[y_])
        k.barrier()
        pes.close()

        pes = ExitStack()
        rows = [k.sb(pes, "cr%d" % j, [128, D], F32) for j in range(4)]
        G2 = load_mod(pes, i, l, 5, "G2")
        x1 = [k.sb(pes, "cx%d" % j, [128, D], F32) for j in range(2)]
        acc = k.sb(pes, "cacc", [128, D], F32)
        xo = [k.sb(pes, "cxo%d" % j, [128, D], F32) for j in range(2)]
        gfin = k.sb(pes, "gfin", [128, D], F32)
        ns = norm_tiles(pes, "nf") if l == 1 else None
        if l == 1:
            k.dma("sp", lambda e: e.dma_start(out=gfin[:, :], in_=gfin_in), W=[gfin])
        for gt in range(NTB):
            xb = x1[gt % 2]
            k.dma("sp", lambda e, xb=xb, gt=gt: e.dma_start(out=xb[:, :], in_=xres[RB + gt * 128:RB + (gt + 1) * 128, :]), W=[xb])
            for kk in range(TOPK):
                for hf in range(2):
                    k.dma("pool", lambda e, gt=gt, kk=kk, hf=hf: e.indirect_dma_start(
                        out=rows[kk][:, hf * 1024:(hf + 1) * 1024], out_offset=None, in_=ys[hf][:, :],
                        in_offset=bass.IndirectOffsetOnAxis(ap=rs.dest[:, gt, kk:kk + 1], axis=0)), R=[rs.dest], W=[rows[kk]])
            k.op("dve", lambda e, gt=gt: e.tensor_scalar(out=acc[:, :], in0=rows[0][:, :], scalar1=rs.G[:, gt, 0:1], scalar2=None, op0=ALU.mult),
                 R=[rows[0], rs.G], W=[acc])
            for kk in range(1, TOPK):
                k.op("dve", lambda e, gt=gt, kk=kk: e.scalar_tensor_tensor(out=acc[:, :], in0=rows[kk][:, :], scalar=rs.G[:, gt, kk:kk + 1],
                                                                           in1=acc[:, :], op0=ALU.mult, op1=ALU.add), R=[rows[kk], rs.G, acc], W=[acc])
            xo_ = xo[gt % 2]
            k.op("pool", lambda e: e.tensor_tensor(out=acc[:, :], in0=acc[:, :], in1=G2[:, :], op=ALU.mult), R=[acc, G2], W=[acc])
            k.op("pool", lambda e, xo_=xo_, xb=xb: e.tensor_tensor(out=xo_[:, :], in0=acc[:, :], in1=xb[:, :], op=ALU.add), R=[acc, xb], W=[xo_])
            if l == 0:
                k.dma("act", lambda e, xo_=xo_, gt=gt: e.dma_start(out=xres[RB + gt * 128:RB + (gt + 1) * 128, :], in_=xo_[:, :]), R=[xo_])
            else:
                norm_mod(ns, xo_, gfin, None, None)
                k.dma("act", lambda e, gt=gt: e.dma_start(out=out_d[RB + gt * 128:RB + (gt + 1) * 128, :], in_=ns.tmp[:, :]), R=[ns.tmp])
        k.barrier()
        pes.close()

    def conv_phase(i):
        pes = ExitStack()
        A1 = load_mod(pes, i, 1, 1, "cA1")
        B1 = load_mod(pes, i, 1, 0, "cB1")
        ns = norm_tiles(pes, "nc")
        xt = [k.sb(pes, "cxt%d" % j, [128, D], F32) for j in range(2)]
        hb = [k.sb(pes, "chb%d" % j, [128, D], BF16) for j in range(2)]
        hT = k.sb(pes, "chT", [128, KC, 512], BF16)
        wsl = [k.sb(pes, "cws%d" % j, [128, KC, 128], BF16) for j in range(3)]
        tmpf = k.sb(pes, "ctmp", [128, 512], F32)
        zo = k.sb(pes, "zo", [128, KC, 512], BF16)
        go = k.sb(pes, "go", [128, KC, 512], BF16)
        zpad = k.sb(pes, "zpad", [128, KC, 1], BF16)
        k.op("dve", lambda e: e.memset(zpad[:, :, :], 0.0), W=[zpad])
        zv = zT.rearrange("(kc p) s -> p kc s", p=128)
        gv = gbT.rearrange("(kc p) s -> p kc s", p=128)
        with nc.allow_non_contiguous_dma(reason="2-byte pad columns"):
            k.dma("sp", lambda e: e.dma_start(out=zv[:, :, 0:1], in_=zpad[:, :, :]), R=[zpad])
            k.dma("sp", lambda e: e.dma_start(out=zv[:, :, S + 1:S + 2], in_=zpad[:, :, :]), R=[zpad])
        xi = 0
        wi = 0
        for c in range(NCH):
            t0 = c * 512
            for tt in range(4):
                xb, hbb = xt[xi % 2], hb[xi % 2]
                xi += 1
                r0 = i * S + t0 + tt * 128
                k.dma("sp", lambda e, xb=xb, r0=r0: e.dma_start(out=xb[:, :], in_=xres[r0:r0 + 128, :]), W=[xb])
                norm_mod(ns, xb, A1, B1, hbb)
                transpose_bf(hbb, hT, tt * 128)

            def proj(m, pz):
                nonlocal wi
                ws = wsl[wi % 3]
                wi += 1
                k.dma("sp", lambda e, ws=ws, m=m: e.dma_start(out=ws[:, :, :].rearrange("p a b -> p (a b)"), in_=cwin_b[m * 128:(m + 1) * 128, :]),
                      W=[ws])
                for kc in range(KC):
                    k.op("pe", lambda e, ws=ws, kc=kc, pz=pz: e.matmul(pz[:, :], ws[:, kc, :], hT[:, kc, :], start=(kc == 0), stop=(kc == KC - 1)),
                         R=[ws, hT], W=[pz])
            for jc in range(KC):
                proj(16 + jc, psA[0])
                proj(32 + jc, psA[1])
                proj(jc, psA[2])
                k.op("act", lambda e: e.copy(out=tmpf[:, :], in_=psA[0][:, :]), R=[psA[0]], W=[tmpf])
                k.op("dve", lambda e, jc=jc: e.tensor_tensor(out=zo[:, jc, :], in0=psA[1][:, :], in1=tmpf[:, :], op=ALU.mult),
                     R=[psA[1], tmpf], W=[zo])
                k.op("act", lambda e, jc=jc: e.copy(out=go[:, jc, :], in_=psA[2][:, :]), R=[psA[2]], W=[go])
            k.dma("act", lambda e, t0=t0: e.dma_start(out=zv[:, :, 1 + t0:1 + t0 + 512], in_=zo[:, :, :]), R=[zo])
            k.dma("act", lambda e, t0=t0: e.dma_start(out=gv[:, :, t0:t0 + 512], in_=go[:, :, :]), R=[go])
        k.barrier()
        pes.close()
        pes = ExitStack()
        cw = k.sb(pes, "cw", [128, KC, 3], F32)
        k.dma("sp", lambda e: e.dma_start(out=cw[:, :, :].rearrange("p a b -> p (a b)"), in_=cw_in), W=[cw])
        zi = [k.sb(pes, "zi%d" % j, [128, KC, 514], BF16) for j in range(2)]
        gi = [k.sb(pes, "gi%d" % j, [128, KC, 512], BF16) for j in range(2)]
        ca = k.sb(pes, "ca", [128, 512], F32)
        cb = k.sb(pes, "cb", [128, 512], F32)
        vo = [k.sb(pes, "vo%d" % j, [128, KC, 512], BF16) for j in range(2)]
        ov = oT.rearrange("(kc p) s -> p kc s", p=128)
        for c in range(NCH):
            t0 = c * 512
            z_, g_, v_ = zi[c % 2], gi[c % 2], vo[c % 2]
            k.dma("sp", lambda e, z_=z_, t0=t0: e.dma_start(out=z_[:, :, :], in_=zv[:, :, t0:t0 + 514]), W=[z_])
            k.dma("sp", lambda e, g_=g_, t0=t0: e.dma_start(out=g_[:, :, :], in_=gv[:, :, t0:t0 + 512]), W=[g_])
            for jc in range(KC):
                k.op("act", lambda e, z_=z_, jc=jc: e.activation(out=ca[:, :], in_=z_[:, jc, 0:512], func=AF.Copy, scale=cw[:, jc, 0:1]),
                     R=[z_, cw], W=[ca])
                k.op("dve", lambda e, z_=z_, jc=jc: e.scalar_tensor_tensor(out=cb[:, :], in0=z_[:, jc, 1:513], scalar=cw[:, jc, 1:2], in1=ca[:, :],
                                                                           op0=ALU.mult, op1=ALU.add), R=[z_, cw, ca], W=[cb])
                k.op("dve", lambda e, z_=z_, jc=jc: e.scalar_tensor_tensor(out=ca[:, :], in0=z_[:, jc, 2:514], scalar=cw[:, jc, 2:3], in1=cb[:, :],
                                                                           op0=ALU.mult, op1=ALU.add), R=[z_, cw, cb], W=[ca])
                k.op("pool", lambda e, g_=g_, v_=v_, jc=jc: e.tensor_tensor(out=v_[:, jc, :], in0=ca[:, :], in1=g_[:, jc, :], op=ALU.mult),
                     R=[ca, g_], W=[v_])
            k.dma("act", lambda e, v_=v_, t0=t0: e.dma_start(out=ov[:, :, t0:t0 + 512], in_=v_[:, :, :]), R=[v_])
        k.barrier()
        pes.close()

    STOP = cfg.get("STOP", 10 ** 9)
    step = [0]

    def go():
        step[0] += 1
        return step[0] <= STOP

    if go():
        cast_phase()
    if go():
        ada_phase()
    for l in range(2):
        for i in range(NBC):
            ges = ExitStack()
            rs = RouteState()
            rs.logits = k.sb(ges, "logits", [128, NTB, E], F32)
            rs.pos = k.sb(ges, "pos", [128, NTB, E], F32)
            rs.base = k.sb(ges, "base", [128, E], F32)
            rs.G = k.sb(ges, "G", [128, NTB, 4], F32)
            rs.dest = k.sb(ges, "dest", [128, NTB, 4], I32)
            rs.idxb = k.sb(ges, "idxb", [128, NBLK], I32)
            rs.idxd = k.sb(ges, "idxd", [128, NBLK], I32)
            k.op("dve", lambda e: e.memset(rs.base[:, :], 0.0), W=[rs.base])
            if l == 0:
                if go():
                    mla_latent_phase(i)
                if go():
                    attention_phase(i)
                if go():
                    mix_phase(0, i, wout_b, rs)
            else:
                if go():
                    conv_phase(i)
                if go():
                    mix_phase(1, i, cwout_b, rs)
            if go():
                moe_phase(l, i, rs)
            k.barrier()
            ges.close()
    k.barrier()
    es.close()
    return nc


def rope_tables(S):
    rows = S // GRID_W
    row = np.repeat(np.arange(rows), GRID_W).astype(np.float32)
    col = np.tile(np.arange(GRID_W), rows).astype(np.float32)
    half = ROPE // 2
    inv = (1.0 / (np.float32(THETA) ** (np.arange(0, half, 2, dtype=np.float32) / np.float32(half)))).astype(np.float32)
    ang_r = row[:, None] * inv[None, :]
    ang_c = col[:, None] * inv[None, :]
    cr, sr, cc, sc = np.cos(ang_r), np.sin(ang_r), np.cos(ang_c), np.sin(ang_c)
    cosT = np.concatenate([cr, cr, cc, cc], axis=1).T.astype(np.float32)
    sinT = np.concatenate([-sr, sr, -sc, sc], axis=1).T.astype(np.float32)
    return np.ascontiguousarray(cosT), np.ascontiguousarray(sinT)


ROPE_PERM = np.concatenate([np.arange(16, 32), np.arange(0, 16), np.arange(48, 64), np.arange(32, 48)])


def rep128(v):
    v = np.asarray(v, np.float32).reshape(1, -1)
    return np.ascontiguousarray(np.broadcast_to(v, (128, v.shape[1])))


def prep_shared(inp, cfg):
    S, NBC, E, F = cfg["S"], cfg["NBC"], cfg["E"], cfg["F"]
    NFC = F // 128
    A = S * TOPK
    NBLK = -(-(A + E * (BLK - 1)) // BLK)
    m = {}
    for l in range(2):
        m["adaw%d" % l] = np.ascontiguousarray(inp["ada_w"][l])
        m["adab%d" % l] = rep128(inp["ada_b"][l])
        m["gmix%d" % l] = rep128(inp["norm_mix_g"][l])
        m["gffn%d" % l] = rep128(inp["norm_ffn_g"][l])
        m["wr%d" % l] = np.ascontiguousarray(inp["router_w"][l])
        m["rb%d" % l] = rep128(inp["router_b"][l])
        wgu = inp["expert_w_gu"][l]
        wgu = wgu.reshape(E, KC, 128, 2, NFC, 128).transpose(4, 0, 2, 1, 3, 5)
        m["wgu%d" % l] = np.ascontiguousarray(wgu).reshape(NFC * E * 128, KC * 256)
        wdn = inp["expert_w_down"][l].reshape(E, NFC, 128, D).transpose(1, 0, 2, 3)
        m["wdn%d" % l] = np.ascontiguousarray(wdn).reshape(NFC * E * 128, D)
        bgu = inp["expert_b_gu"][l].reshape(E, 2, NFC, 128).transpose(0, 3, 1, 2)
        m["bgu%d" % l] = np.ascontiguousarray(bgu).reshape(E * 128, 2 * NFC)
        m["bdn%d" % l] = np.ascontiguousarray(inp["expert_b_down"][l])
    m["gfin"] = rep128(inp["final_norm_g"])
    w_in = inp["mla_w_in"][0]
    m["mla_win"] = np.ascontiguousarray(np.concatenate([w_in, w_in[:, 1024 + ROPE_PERM]], axis=1))
    m["gq"] = np.ascontiguousarray(inp["mla_q_norm_g"][0].reshape(4, 128).T)
    m["gkv"] = np.ascontiguousarray(inp["mla_kv_norm_g"][0].reshape(4, 128).T)
    wq = inp["mla_w_q_up"][0].reshape(QL, NH, 192)
    m["mla_wq"] = np.ascontiguousarray(np.concatenate([wq, wq[:, :, 128 + ROPE_PERM]], axis=2)).reshape(QL, NH * 256)
    m["mla_wkv"] = np.ascontiguousarray(inp["mla_w_kv_up"][0])
    m["mla_wout"] = np.ascontiguousarray(inp["mla_w_out"][0])
    cwin = inp["conv_w_in"][0].reshape(KC, 128, 48, 128).transpose(2, 1, 0, 3)
    m["conv_win"] = np.ascontiguousarray(cwin).reshape(48 * 128, KC * 128)
    m["conv_w"] = np.ascontiguousarray(inp["conv_w"][0].reshape(3, KC, 128).transpose(2, 1, 0)).reshape(128, KC * 3)
    m["conv_wout"] = np.ascontiguousarray(inp["conv_w_out"][0])
    m["ident"] = np.eye(128, dtype=np.float32)
    m["utri"] = np.triu(np.ones((128, 128), np.float32), 1)
    cosT, sinT = rope_tables(S)
    m["ropec"], m["ropes"] = cosT, sinT
    m["blkstart"] = rep128(np.arange(NBLK, dtype=np.float32) * BLK)
    m["pcol"] = np.arange(128, dtype=np.float32).reshape(128, 1)
    return m


def run(inp, cfg):
    S, NBC, NCORES = cfg["S"], cfg["NBC"], cfg["NCORES"]
    shared = prep_shared(inp, cfg)
    in_maps = []
    for c in range(NCORES):
        b0 = c * NBC
        m = dict(shared)
        m["x"] = np.ascontiguousarray(inp["x"][b0:b0 + NBC]).reshape(NBC * S, D)
        m["ctx"] = np.ascontiguousarray(inp["ctx"][b0:b0 + NBC]).reshape(NBC * CTX, D)
        cc = np.concatenate([inp["c"][b0:b0 + NBC], inp["c_ctx"][None, :]], axis=0)
        m["cT"] = np.ascontiguousarray(cc.reshape(NBC + 1, KC, 128).transpose(2, 0, 1)).reshape(128, (NBC + 1) * KC)
        in_maps.append(m)
    nc = build_nc(cfg)
    res = run_bass_kernel_spmd(nc, in_maps, core_ids=list(range(NCORES)))
    outs = [np.asarray(res.results[c]["out"]).reshape(NBC, S, D) for c in range(NCORES)]
    return np.concatenate(outs, axis=0).astype(np.float32)


def kernel(**inputs):
    inp = {k_: np.asarray(v) for k_, v in inputs.items()}
    return run(inp, FULL_CFG)
```
